# Optimizing a Trainium2 kernel written in Bass

```python
import jax, jax.numpy as jnp
from jax import lax
import numpy as np

D_MODEL = 2048
BATCH = 2
SEQ = 4096
DEPTH = 4

GRID_W = 64
CTX_LEN = 256
N_MIXERS = 3
HEAD_DIM = 128
A_Q_HEADS = 16
A_KV_HEADS = 4
A_GROUPS = A_Q_HEADS // A_KV_HEADS
A_Q_DIM = A_Q_HEADS * HEAD_DIM
A_KV_DIM = A_KV_HEADS * HEAD_DIM
Q_BLOCK = 128
ROPE_BASE = 10000.0
B_HEADS = D_MODEL // HEAD_DIM
WIN_R = 8
WIN_C = 16
POOL_WINDOWS = (2, 4, 8, 16)
POOL_GROUP = D_MODEL // len(POOL_WINDOWS)
PEER_HEADS = 8
PEER_QDIM = 256
PEER_NKEYS = 128
PEER_N = PEER_NKEYS * PEER_NKEYS
PEER_TOPK = 16
PEER_CHUNK = 128
N_MOD = 6
EPS = 1e-6
N_A = (DEPTH + 2) // 3
N_B = (DEPTH + 1) // 3
N_C = DEPTH // 3

kernel_name = "hybrid_dit_gqa_natten_pool_peer"


def rms_norm(x, w):
    x32 = x.astype(jnp.float32)
    y = x32 * lax.rsqrt(jnp.mean(x32 * x32, axis=-1, keepdims=True) + EPS)
    return (y * w.astype(jnp.float32)).astype(x.dtype)


def modulate(h, shift, scale):
    return h * (1.0 + scale) + shift


def softmax32(s, dtype):
    return jax.nn.softmax(s.astype(jnp.float32), axis=-1).astype(dtype)


def grid_angles(S):
    t = jnp.arange(S)
    row = (t // GRID_W).astype(jnp.float32)
    col = (t % GRID_W).astype(jnp.float32)
    half = HEAD_DIM // 2
    inv = ROPE_BASE ** (-jnp.arange(0, half, 2, dtype=jnp.float32) / half)
    return row[:, None] * inv[None, :], col[:, None] * inv[None, :]


def rope_rotate(x, ang):
    shape = (ang.shape[0],) + (1,) * (x.ndim - 3) + (ang.shape[1],)
    cos = jnp.cos(ang).reshape(shape)
    sin = jnp.sin(ang).reshape(shape)
    x32 = x.astype(jnp.float32)
    x1, x2 = jnp.split(x32, 2, axis=-1)
    return jnp.concatenate([x1 * cos - x2 * sin, x2 * cos + x1 * sin], axis=-1).astype(x.dtype)


def apply_rope_2d(x, ang_row, ang_col):
    half = HEAD_DIM // 2
    return jnp.concatenate([rope_rotate(x[..., :half], ang_row), rope_rotate(x[..., half:], ang_col)], axis=-1)


def gqa_axial_attention(h_ctx, h_lat, wqkv, q_gain, k_gain, wo, need_ctx_out):
    B, S, _ = h_lat.shape
    C = h_ctx.shape[1]
    scale = HEAD_DIM ** -0.5

    def split_q(q):
        return rms_norm(q.reshape(q.shape[:2] + (A_KV_HEADS, A_GROUPS, HEAD_DIM)), q_gain)

    def split_kv(kv):
        k = rms_norm(kv[..., :A_KV_DIM].reshape(kv.shape[:2] + (A_KV_HEADS, HEAD_DIM)), k_gain)
        v = kv[..., A_KV_DIM:].reshape(kv.shape[:2] + (A_KV_HEADS, HEAD_DIM))
        return k, v

    def attend(q, k, v):
        s = jnp.einsum('bqkgd,bskd->bkgqs', q, k)
        p = softmax32(s, v.dtype)
        return jnp.einsum('bkgqs,bskd->bqkgd', p, v)

    ang_r, ang_c = grid_angles(S)
    qkv = h_lat @ wqkv
    q_lat = apply_rope_2d(split_q(qkv[..., :A_Q_DIM]), ang_r, ang_c) * scale
    k_lat, v_lat = split_kv(qkv[..., A_Q_DIM:])
    k_lat = apply_rope_2d(k_lat, ang_r, ang_c)
    k_ctx, v_ctx = split_kv(h_ctx @ wqkv[:, A_Q_DIM:])
    k_all = jnp.concatenate([k_ctx, k_lat], axis=1)
    v_all = jnp.concatenate([v_ctx, v_lat], axis=1)

    nb = S // Q_BLOCK
    q_blocks = jnp.moveaxis(q_lat.reshape(B, nb, Q_BLOCK, A_KV_HEADS, A_GROUPS, HEAD_DIM), 1, 0)
    o = lax.map(lambda qb: attend(qb, k_all, v_all), q_blocks)
    o_lat = jnp.moveaxis(o, 0, 1).reshape(B, S, A_Q_DIM) @ wo
    o_ctx = None
    if need_ctx_out:
        q_ctx = split_q(h_ctx @ wqkv[:, :A_Q_DIM]) * scale
        o_ctx = attend(q_ctx, k_ctx, v_ctx).reshape(B, C, A_Q_DIM) @ wo
    return o_ctx, o_lat


def neighborhood_attention(h_ctx, h_lat, wqkv, rpb, wo, need_ctx_out):
    B, S, D = h_lat.shape
    C = h_ctx.shape[1]
    rows = S // GRID_W
    wr = min(WIN_R, rows)
    nwin = wr * WIN_C
    scale = HEAD_DIM ** -0.5

    def heads(t):
        return t.reshape(t.shape[:2] + (B_HEADS, HEAD_DIM))

    qkv = h_lat @ wqkv
    q_lat = heads(qkv[..., :D]) * scale
    k_grid = heads(qkv[..., D:2 * D]).reshape(B, rows, GRID_W, B_HEADS, HEAD_DIM)
    v_grid = heads(qkv[..., 2 * D:]).reshape(B, rows, GRID_W, B_HEADS, HEAD_DIM)
    kv_ctx = h_ctx @ wqkv[:, D:]
    k_ctx = heads(kv_ctx[..., :D])
    v_ctx = heads(kv_ctx[..., D:])

    cols = np.arange(GRID_W)
    col_start = np.clip(cols - WIN_C // 2, 0, GRID_W - WIN_C)
    key_cols = col_start[:, None] + np.arange(WIN_C)[None, :]
    col_rel = key_cols - cols[:, None] + (WIN_C - 1)

    def row_block(args):
        r, q_row = args
        r0 = jnp.clip(r - wr // 2, 0, rows - wr)
        kw = lax.dynamic_slice_in_dim(k_grid, r0, wr, axis=1)[:, :, key_cols]
        vw = lax.dynamic_slice_in_dim(v_grid, r0, wr, axis=1)[:, :, key_cols]
        rel_r = r0 + jnp.arange(wr) - r + (WIN_R - 1)
        bias = rpb[:, rel_r][:, :, col_rel]
        bias = jnp.transpose(bias, (0, 2, 1, 3)).reshape(B_HEADS, GRID_W, nwin)
        s_win = jnp.einsum('bqhd,brqjhd->bhqrj', q_row, kw).reshape(B, B_HEADS, GRID_W, nwin) + bias
        s_ctx = jnp.einsum('bqhd,bchd->bhqc', q_row, k_ctx)
        p = softmax32(jnp.concatenate([s_win, s_ctx], axis=-1), v_ctx.dtype)
        p_win = p[..., :nwin].reshape(B, B_HEADS, GRID_W, wr, WIN_C)
        return (jnp.einsum('bhqrj,brqjhd->bqhd', p_win, vw)
                + jnp.einsum('bhqc,bchd->bqhd', p[..., nwin:], v_ctx))

    q_rows = jnp.moveaxis(q_lat.reshape(B, rows, GRID_W, B_HEADS, HEAD_DIM), 1, 0)
    o = lax.map(row_block, (jnp.arange(rows), q_rows))
    o_lat = jnp.moveaxis(o, 0, 1).reshape(B, S, D) @ wo
    o_ctx = None
    if need_ctx_out:
        q_ctx = heads(h_ctx @ wqkv[:, :D]) * scale
        p = softmax32(jnp.einsum('bqhd,bkhd->bhqk', q_ctx, k_ctx), v_ctx.dtype)
        o_ctx = jnp.einsum('bhqk,bkhd->bqhd', p, v_ctx).reshape(B, C, D) @ wo
    return o_ctx, o_lat


def multiscale_pool_mixer(h_ctx, h_lat, w_grp, ls, need_ctx_out):
    def mix(h):
        B, L, D = h.shape
        h32 = h.astype(jnp.float32)
        cs = jnp.concatenate([jnp.zeros((B, 1, D), jnp.float32), jnp.cumsum(h32, axis=1)], axis=1)
        t = np.arange(L)
        outs = []
        for g, w in enumerate(POOL_WINDOWS):
            lo = np.maximum(t - w // 2, 0)
            hi = np.minimum(t + w - w // 2, L)
            inv_cnt = (1.0 / (hi - lo)).astype(np.float32)[None, :, None]
            sl = slice(g * POOL_GROUP, (g + 1) * POOL_GROUP)
            outs.append((cs[:, hi, sl] - cs[:, lo, sl]) * inv_cnt - h32[:, :, sl])
        y = jnp.stack(outs, axis=2).astype(h.dtype)
        return jnp.einsum('blgc,gce->blge', y, w_grp).reshape(B, L, D) * ls

    o_ctx = mix(h_ctx) if need_ctx_out else None
    return o_ctx, mix(h_lat)


def peer_ffn(h, wq, sub_keys, u, v):
    T, D = h.shape
    hq = PEER_QDIM // 2

    def chunk(xc):
        q = (xc @ wq).reshape(PEER_CHUNK, PEER_HEADS, 2, hq)
        s1 = jnp.einsum('thd,kd->thk', q[:, :, 0], sub_keys[0]).astype(jnp.float32)
        s2 = jnp.einsum('thd,kd->thk', q[:, :, 1], sub_keys[1]).astype(jnp.float32)
        v1, i1 = lax.top_k(s1, PEER_TOPK)
        v2, i2 = lax.top_k(s2, PEER_TOPK)
        cand = (v1[..., :, None] + v2[..., None, :]).reshape(PEER_CHUNK, PEER_HEADS, PEER_TOPK * PEER_TOPK)
        sc, ci = lax.top_k(cand, PEER_TOPK)
        e1 = jnp.take_along_axis(i1, ci // PEER_TOPK, axis=-1)
        e2 = jnp.take_along_axis(i2, ci % PEER_TOPK, axis=-1)
        idx = e1 * PEER_NKEYS + e2
        g = jax.nn.softmax(sc, axis=-1).astype(xc.dtype)
        act = jax.nn.gelu(jnp.einsum('td,thkd->thk', xc, u[idx]))
        return jnp.einsum('thk,thkd->td', g * act, v[idx])

    out = lax.map(chunk, h.reshape(T // PEER_CHUNK, PEER_CHUNK, D))
    return out.reshape(T, D)


def setup_inputs(seed: int = 0) -> dict:
    key = jax.random.key(seed)
    ks = jax.random.split(key, 21)
    f32 = jnp.float32
    D = D_MODEL

    def nrm(k, shape, std):
        return jax.random.normal(k, shape, f32) * std

    return {
        "x": nrm(ks[0], (BATCH, SEQ, D), 1.0),
        "c": nrm(ks[1], (BATCH, D), 1.0),
        "ctx": nrm(ks[2], (BATCH, CTX_LEN, D), 1.0),
        "c_ctx": nrm(ks[3], (D,), 1.0),
        "mod_w": nrm(ks[4], (DEPTH, D, N_MOD * D), 0.5 * D ** -0.5),
        "mod_b": nrm(ks[5], (DEPTH, N_MOD * D), 0.02),
        "norm_w": 1.0 + nrm(ks[6], (DEPTH, 2, D), 0.05),
        "final_norm_w": 1.0 + nrm(ks[7], (D,), 0.05),
        "a_wqkv": nrm(ks[8], (N_A, D, A_Q_DIM + 2 * A_KV_DIM), D ** -0.5),
        "a_q_gain": 1.0 + nrm(ks[9], (N_A, HEAD_DIM), 0.05),
        "a_k_gain": 1.0 + nrm(ks[10], (N_A, HEAD_DIM), 0.05),
        "a_wo": nrm(ks[11], (N_A, A_Q_DIM, D), A_Q_DIM ** -0.5),
        "b_wqkv": nrm(ks[12], (N_B, D, 3 * D), D ** -0.5),
        "b_rpb": nrm(ks[13], (N_B, B_HEADS, 2 * WIN_R - 1, 2 * WIN_C - 1), 0.1),
        "b_wo": nrm(ks[14], (N_B, D, D), D ** -0.5),
        "pool_w": nrm(ks[15], (N_C, len(POOL_WINDOWS), POOL_GROUP, POOL_GROUP), POOL_GROUP ** -0.5),
        "pool_scale": 0.5 + nrm(ks[16], (N_C, D), 0.05),
        "peer_wq": nrm(ks[17], (DEPTH, D, PEER_HEADS * PEER_QDIM), D ** -0.5),
        "peer_sub_keys": nrm(ks[18], (DEPTH, 2, PEER_NKEYS, PEER_QDIM // 2), (PEER_QDIM // 2) ** -0.5),
        "peer_u": nrm(ks[19], (DEPTH, PEER_N, D), D ** -0.5),
        "peer_v": nrm(ks[20], (DEPTH, PEER_N, D), 0.3),
    }


def reference(x, c, ctx, c_ctx, mod_w, mod_b, norm_w, final_norm_w, a_wqkv, a_q_gain, a_k_gain, a_wo,
              b_wqkv, b_rpb, b_wo, pool_w, pool_scale, peer_wq, peer_sub_keys, peer_u, peer_v):
    B, S, D = x.shape
    C = ctx.shape[1]
    xc = ctx
    sc_lat = jax.nn.silu(c)
    sc_ctx = jax.nn.silu(c_ctx)
    for i in range(DEPTH):
        last = i == DEPTH - 1
        kind, j = i % N_MIXERS, i // N_MIXERS
        m_lat = [m[:, None, :] for m in jnp.split(sc_lat @ mod_w[i] + mod_b[i], N_MOD, axis=-1)]
        h_lat = modulate(rms_norm(x, norm_w[i, 0]), m_lat[0], m_lat[1])
        h_ctx = None
        if not (last and kind == 2):
            m_ctx = jnp.split(sc_ctx @ mod_w[i] + mod_b[i], N_MOD, axis=-1)
            h_ctx = modulate(rms_norm(xc, norm_w[i, 0]), m_ctx[0], m_ctx[1])
        if kind == 0:
            o_ctx, o_lat = gqa_axial_attention(h_ctx, h_lat, a_wqkv[j], a_q_gain[j], a_k_gain[j], a_wo[j], not last)
        elif kind == 1:
            o_ctx, o_lat = neighborhood_attention(h_ctx, h_lat, b_wqkv[j], b_rpb[j], b_wo[j], not last)
        else:
            o_ctx, o_lat = multiscale_pool_mixer(h_ctx, h_lat, pool_w[j], pool_scale[j], not last)
        x = x + m_lat[2] * o_lat
        f_lat = modulate(rms_norm(x, norm_w[i, 1]), m_lat[3], m_lat[4]).reshape(B * S, D)
        if last:
            f = peer_ffn(f_lat, peer_wq[i], peer_sub_keys[i], peer_u[i], peer_v[i])
            x = x + m_lat[5] * f.reshape(B, S, D)
        else:
            xc = xc + m_ctx[2] * o_ctx
            f_ctx = modulate(rms_norm(xc, norm_w[i, 1]), m_ctx[3], m_ctx[4]).reshape(B * C, D)
            f = peer_ffn(jnp.concatenate([f_ctx, f_lat], axis=0), peer_wq[i], peer_sub_keys[i], peer_u[i], peer_v[i])
            xc = xc + m_ctx[5] * f[:B * C].reshape(B, C, D)
            x = x + m_lat[5] * f[B * C:].reshape(B, S, D)
    return rms_norm(x, final_norm_w)
```

```python
import contextlib
import numpy as np
import concourse.bass as bass
import concourse.mybir as mybir
from concourse.bass_utils import run_bass_kernel_spmd

F32 = mybir.dt.float32
BF16 = mybir.dt.bfloat16
U32 = mybir.dt.uint32
AF = mybir.ActivationFunctionType
ALU = mybir.AluOpType
AX = mybir.AxisListType
GELU = AF.Gelu_apprx_tanh

NC = 8
D = 2048
NT = 9
DEPTH = 4
N_DMA_SEMS = 10
NCC = 4
QUEUES = ("sp", "act", "pool")
COMPUTE = ("pe", "act", "dve", "pool")
G4 = [[0, 1, 2, 3], [4, 5, 6, 7]]
G2 = [[0, 4], [1, 5], [2, 6], [3, 7]]


class T:
    __slots__ = ("w", "r")

    def __init__(self):
        self.w = None
        self.r = []


class Prog:
    def __init__(self, nc):
        self.nc = nc
        self.ops = []
        self.cnt = {e: 0 for e in ("pe", "act", "dve", "pool", "sp")}
        self.dma_cnt = {q: [0] * N_DMA_SEMS for q in QUEUES}
        self.dma_rr = {q: 0 for q in QUEUES}
        self.dma_last = {q: [None] * N_DMA_SEMS for q in QUEUES}
        self.cc_cnt = [0] * NCC
        self.cc_last = [None] * NCC
        self.cc_rr = 0
        self.bar = set()

    def barrier(self):
        last = {}
        for oid, o in enumerate(self.ops):
            key = o["dma"][:2] if o["dma"] is not None else ("c", o["eng"])
            last[key] = oid
        self.bar = set(last.values())

    def _deps(self, r, w):
        deps = set(self.bar)
        for t in r:
            if t.w is not None:
                deps.add(t.w)
        for t in w:
            if t.w is not None:
                deps.add(t.w)
            deps.update(t.r)
        return deps

    def _mark(self, oid, r, w):
        for t in r:
            t.r.append(oid)
        for t in w:
            t.w = oid
            t.r = []

    def op(self, eng, fn, r=(), w=()):
        deps = self._deps(r, w)
        oid = len(self.ops)
        self.cnt[eng] += 1
        self.ops.append(dict(eng=eng, fn=fn, deps=deps, dma=None, idx=self.cnt[eng]))
        self._mark(oid, r, w)
        return oid

    def dma(self, q, fn, r=(), w=()):
        deps = self._deps(r, w)
        oid = len(self.ops)
        j = self.dma_rr[q]
        self.dma_rr[q] = (j + 1) % N_DMA_SEMS
        if self.dma_last[q][j] is not None:
            deps.add(self.dma_last[q][j])
        self.dma_cnt[q][j] += 1
        self.dma_last[q][j] = oid
        self.ops.append(dict(eng=q, fn=fn, deps=deps, dma=(q, j, 16 * self.dma_cnt[q][j], 16), idx=None))
        self._mark(oid, r, w)
        return oid

    def cc(self, fn, r=(), w=()):
        deps = self._deps(r, w)
        oid = len(self.ops)
        j = self.cc_rr
        self.cc_rr = (j + 1) % NCC
        if self.cc_last[j] is not None:
            deps.add(self.cc_last[j])
        self.cc_cnt[j] += 1
        self.cc_last[j] = oid
        self.ops.append(dict(eng="pool", fn=fn, deps=deps, dma=("cc", j, self.cc_cnt[j], 1), idx=None))
        self._mark(oid, r, w)
        return oid

    def emit(self):
        nc = self.nc
        with contextlib.ExitStack() as st:
            csem = {e: st.enter_context(nc.semaphore("c_" + e)) for e in COMPUTE}
            dsem = {q: [st.enter_context(nc.semaphore("d_%s%d" % (q, j))) for j in range(N_DMA_SEMS)] for q in QUEUES}
            dsem["cc"] = [st.enter_context(nc.semaphore("ccs%d" % j)) for j in range(NCC)]
            block = st.enter_context(nc.Block())
            ops = self.ops

            def target(o):
                if o["dma"] is not None:
                    q, j, v, _ = o["dma"]
                    return dsem[q][j], v, ("d", q, j)
                return csem[o["eng"]], o["idx"], ("c", o["eng"])

            def run(engname, eng):
                seen = {}
                for o in ops:
                    if o["eng"] != engname:
                        continue
                    for d in sorted(o["deps"]):
                        od = ops[d]
                        if od["dma"] is None and od["eng"] == "pe" and engname == "pe" and o["dma"] is None:
                            continue
                        sem, val, key = target(od)
                        if seen.get(key, 0) >= val:
                            continue
                        eng.wait_ge(sem, val)
                        seen[key] = val
                    ins = o["fn"](eng)
                    if o["dma"] is not None:
                        q, j, v, inc = o["dma"]
                        ins.then_inc(dsem[q][j], inc)
                    else:
                        ins.then_inc(csem[engname], 1)
                if engname == "sp":
                    for q in QUEUES:
                        for j in range(N_DMA_SEMS):
                            if self.dma_cnt[q][j]:
                                eng.wait_ge(dsem[q][j], 16 * self.dma_cnt[q][j])

            @block.tensor
            def _(e):
                run("pe", e)

            @block.scalar
            def _(e):
                run("act", e)

            @block.vector
            def _(e):
                run("dve", e)

            @block.gpsimd
            def _(e):
                run("pool", e)

            @block.sync
            def _(e):
                run("sp", e)


def cprime(c):
    return (c % 2) * 8 + c // 2


class K:
    def __init__(self, cfg):
        self.cfg = cfg
        self.nc = bass.Bass("TRN2", target_bir_lowering=False)
        self.P = Prog(self.nc)
        self.st = contextlib.ExitStack()
        self.inputs = {}
        self.uid = 0

    def inp(self, name, shape, dt=F32):
        t = self.nc.dram_tensor(name, list(shape), dt, kind="ExternalInput")
        self.inputs[name] = t
        return t

    def idram(self, name, shape, dt):
        return self.nc.dram_tensor(name, list(shape), dt, kind="Internal")

    def sb(self, name, shape, dt, st=None):
        self.uid += 1
        return (st or self.st).enter_context(self.nc.sbuf_tensor("%s_%d" % (name, self.uid), list(shape), dt))

    def psum(self, name, shape, dt):
        return self.st.enter_context(self.nc.psum_tensor(name, list(shape), dt))

    NPOOL = 4

    def _pool(self, dt):
        if not hasattr(self, "pools"):
            self.pools = {}
            self.pool_rr = {}
        key = "bf16" if dt == BF16 else "f32"
        if key not in self.pools:
            ne = (1 << 20) // (2 if dt == BF16 else 4)
            self.pools[key] = [(self.idram("pc_a_%s%d" % (key, i), [ne], dt), self.idram("pc_b_%s%d" % (key, i), [4 * ne], dt),
                                T(), T()) for i in range(self.NPOOL)]
            self.pool_rr[key] = 0
        i = self.pool_rr[key]
        self.pool_rr[key] = (i + 1) % self.NPOOL
        return self.pools[key][i]

    def gather_full(self, name, shard, rows, cols, dt=F32, rdeps=(), cast=False):
        P = self.P
        odt = BF16 if cast else dt
        full = self.idram(name + "_full", [4 * rows, cols], odt)
        tfull = T()
        rp = max(1, (1 << 20) // (cols * (2 if odt == BF16 else 4)))
        for pi, r0 in enumerate(range(0, rows, rp)):
            r1 = min(rows, r0 + rp)
            n = r1 - r0
            a0, a1, t0, t1 = self._pool(odt)
            s0 = a0.ap()[0:n * cols].rearrange("(r c) -> r c", c=cols)
            s1 = a1.ap()[0:4 * n * cols].rearrange("(r c) -> r c", c=cols)
            P.dma("pool" if cast else "sp", lambda e, r0=r0, r1=r1, s0=s0: e.dma_start(out=s0, in_=shard.ap()[r0:r1, :]),
                  r=list(rdeps), w=[t0])
            P.cc(lambda e, s0=s0, s1=s1: e.collective_compute("AllGather", ALU.bypass, replica_groups=G4, ins=[s0], outs=[s1]),
                 r=[t0], w=[t1])
            for rk in range(4):
                P.dma("sp", lambda e, rk=rk, r0=r0, n=n, s1=s1: e.dma_start(
                    out=full.ap()[rk * rows + r0: rk * rows + r0 + n, :], in_=s1[rk * n:(rk + 1) * n, :]),
                    r=[t1], w=[tfull])
        return full, tfull


def build(cfg):
    k = K(cfg)
    nc, P = k.nc, k.P
    layers = cfg["layers"]
    do_peer = cfg.get("peer", True)
    mixers = cfg.get("mixers", True)
    dbg = cfg.get("dbg", False)

    x_in = k.inp("x_c", [NT, 128, D])
    c_all = k.inp("c_all", [3, D])
    bsel = k.inp("bsel", [128, 2])
    modw = k.inp("modw", [DEPTH, D, 6, 256])
    modb = k.inp("modb", [1, DEPTH * 6 * 256])
    normw = k.inp("normw", [DEPTH * 2, D])
    fnw = k.inp("fnw", [1, D])
    NL = len(layers)
    if do_peer:
        wq_sh = k.inp("peer_wq", [NL, 512, D])
        skT_in = k.inp("skT", [DEPTH, 2, 128, 128])
        u_sh = k.inp("peer_u", [NL, 4096, D])
        v_sh = k.inp("peer_v", [NL, 4096, D])
    out = nc.dram_tensor("out", [8, 128, D], F32, kind="ExternalOutput")

    st = k.st
    with st:
        xres = k.sb("xres", [128, NT, D], F32)
        tx = [T() for _ in range(NT)]
        ident = k.sb("ident", [128, 128], BF16)
        identf = k.sb("identf", [128, 128], F32)
        tid = T()
        eps = k.sb("eps", [128, 1], F32)
        teps = T()
        bs = k.sb("bs", [128, 2], F32)
        tbs = T()
        ps = [k.psum("ps%d" % i, [128, 512], F32) for i in range(8)]
        tps = [T() for _ in range(8)]

        P.op("pool", lambda e: e.memset(ident[:], 0.0), w=[tid])
        P.op("pool", lambda e: e.affine_select(out=ident[:], in_=ident[:], pattern=[[-1, 128]], compare_op=ALU.not_equal,
                                               fill=1.0, base=0, channel_multiplier=1), r=[tid], w=[tid])
        P.op("pool", lambda e: e.memset(identf[:], 0.0), w=[tid])
        P.op("pool", lambda e: e.affine_select(out=identf[:], in_=identf[:], pattern=[[-1, 128]], compare_op=ALU.not_equal,
                                               fill=1.0, base=0, channel_multiplier=1), r=[tid], w=[tid])
        P.op("dve", lambda e: e.memset(eps[:], 1e-6), w=[teps])
        P.dma("sp", lambda e: e.dma_start(out=bs[:], in_=bsel.ap()), w=[tbs])
        for t in range(NT):
            P.dma("sp", lambda e, t=t: e.dma_start(out=xres[:, t, :], in_=x_in.ap()[t]), w=[tx[t]])

        wq_full, u_full, v_full = {}, {}, {}
        if do_peer:
            for li, l in enumerate(layers):
                wq_full[l] = k.gather_full("wq%d" % l, _sub(wq_sh, li), 512, D, cast=True)
                u_full[l] = k.gather_full("u%d" % l, _sub(u_sh, li), 4096, D, cast=True)
                v_full[l] = k.gather_full("v%d" % l, _sub(v_sh, li), 4096, D, cast=True)

        m_in = k.idram("m_in", [3, DEPTH * 6 * 256], F32)
        m_s1 = k.idram("m_s1", [12, DEPTH * 6 * 256], F32)
        m_all = k.idram("m_all", [24, DEPTH * 6 * 256], F32)
        tm_in, tm_s1, tm_all = T(), T(), T()
        MW = DEPTH * 6 * 256
        ph = contextlib.ExitStack()
        cs = k.sb("cs", [48, 128], F32, ph)
        tcs = T()
        sT = k.sb("sT", [128, 48], F32, ph)
        tsT = T()
        mwb = [k.sb("mwb%d" % i, [128, 16, 256], F32, ph) for i in range(2)]
        tmwb = [T(), T()]
        mbb = k.sb("mbb", [3, MW], F32, ph)
        tmbb = T()
        msb = k.sb("msb", [3, MW], F32, ph)
        tmsb = T()
        for r in range(3):
            P.dma("sp", lambda e, r=r: e.dma_start(out=cs[r * 16:(r + 1) * 16, :],
                                                   in_=c_all.ap()[r].rearrange("(c p) -> c p", p=128)), w=[tcs])
        P.op("act", lambda e: e.activation(out=cs[:], in_=cs[:], func=AF.Silu), r=[tcs], w=[tcs])
        P.op("pe", lambda e: e.transpose(ps[0][:, 0:48], cs[:], identf[0:48, 0:48]), r=[tcs, tid], w=[tps[0]])
        P.op("dve", lambda e: e.tensor_copy(out=sT[:], in_=ps[0][:, 0:48]), r=[tps[0]], w=[tsT])
        P.dma("sp", lambda e: e.dma_start(out=mbb[:], in_=modb.ap().partition_broadcast(3)), w=[tmbb])
        sT3 = sT[:].rearrange("p (r c) -> p c r", r=3)
        g = 0
        for l in range(DEPTH):
            for w6 in range(6):
                b = g % 2
                P.dma("sp", lambda e, l=l, w6=w6, b=b: e.dma_start(
                    out=mwb[b][:], in_=modw.ap()[l, :, w6, :].rearrange("(k p) n -> p k n", p=128)), w=[tmwb[b]])
                pp = 1 + (g % 2)
                for kk in range(16):
                    P.op("pe", lambda e, kk=kk, b=b, pp=pp: e.matmul(ps[pp][0:3, 0:256], lhsT=sT3[:, kk, :], rhs=mwb[b][:, kk, :],
                                                                   start=(kk == 0), stop=(kk == 15)),
                         r=[tsT, tmwb[b]], w=[tps[pp]])
                P.op("dve", lambda e, g=g, pp=pp: e.tensor_tensor(out=msb[:, g * 256:(g + 1) * 256], in0=ps[pp][0:3, 0:256],
                                                                  in1=mbb[:, g * 256:(g + 1) * 256], op=ALU.add),
                     r=[tps[pp], tmbb], w=[tmsb])
                g += 1
        P.dma("sp", lambda e: e.dma_start(out=m_in.ap(), in_=msb[:]), r=[tmsb], w=[tm_in])
        P.cc(lambda e: e.collective_compute("AllGather", ALU.bypass, replica_groups=G4, ins=[m_in.ap()], outs=[m_s1.ap()]),
             r=[tm_in], w=[tm_s1])
        P.cc(lambda e: e.collective_compute("AllGather", ALU.bypass, replica_groups=G2, ins=[m_s1.ap()], outs=[m_all.ap()]),
             r=[tm_s1], w=[tm_all])
        ph.close()
        P.barrier()
        hT = k.sb("hT", [128, 16, NT * 128], BF16)
        thT = [T() for _ in range(NT)]

        def mvec_ap(l, row, w6, jh=None, bcast=None):
            off = row * MW + (l * 6 + w6) * 256
            if bcast:
                return bass.AP(m_all, off, [[0, bcast], [3 * MW, 8], [1, 256]])
            return bass.AP(m_all, off + jh * 128, [[3 * MW, 8], [1, 128]])

        vrow = k.sb("vrow", [128, 2, 128], F32)
        tvrow = T()
        nrow = k.sb("nrow", [32, 128], F32)
        tnrow = T()
        vT = k.sb("vT", [128, 2, 128], F32)
        tvT = T()
        nT = k.sb("nT", [128, 32], F32)
        tnT = T()
        AB = k.sb("AB", [128, 8, 16], F32)
        tAB = T()

        def layer_vectors(l):
            vi = 0
            for row in (0, 1):
                for w6 in (0, 1, 3, 4):
                    for jh in (0, 1):
                        P.dma("sp", lambda e, row=row, w6=w6, jh=jh, vi=vi: e.dma_start(
                            out=vrow[vi * 16 + jh * 8: vi * 16 + jh * 8 + 8, 0, :], in_=mvec_ap(l, row, w6, jh=jh)),
                            r=[tm_all], w=[tvrow])
                    vi += 1
            vi = 0
            for w6 in (0, 1, 3, 4):
                for jh in (0, 1):
                    P.dma("sp", lambda e, w6=w6, jh=jh, vi=vi: e.dma_start(
                        out=vrow[vi * 16 + jh * 8: vi * 16 + jh * 8 + 8, 1, :], in_=mvec_ap(l, 2, w6, jh=jh)),
                        r=[tm_all], w=[tvrow])
                vi += 1
            for n2 in (0, 1):
                for jh in (0, 1):
                    P.dma("sp", lambda e, n2=n2, jh=jh: e.dma_start(
                        out=nrow[n2 * 16 + jh * 8: n2 * 16 + jh * 8 + 8, :],
                        in_=bass.AP(normw, (l * 2 + n2) * D + jh * 128, [[256, 8], [1, 128]])), w=[tnrow])
            P.op("pe", lambda e: e.transpose(ps[0][:, 0:128], vrow[:, 0, :], identf[:]), r=[tvrow, tid], w=[tps[0]])
            P.op("dve", lambda e: e.tensor_copy(out=vT[:, 0, :], in_=ps[0][:, 0:128]), r=[tps[0]], w=[tvT])
            P.op("pe", lambda e: e.transpose(ps[0][:, 0:64], vrow[0:64, 1, :], identf[0:64, 0:64]), r=[tvrow, tid], w=[tps[0]])
            P.op("dve", lambda e: e.tensor_copy(out=vT[:, 1, 0:64], in_=ps[0][:, 0:64]), r=[tps[0]], w=[tvT])
            P.op("pe", lambda e: e.transpose(ps[0][:, 0:32], nrow[:], identf[0:32, 0:32]), r=[tnrow, tid], w=[tps[0]])
            P.op("dve", lambda e: e.tensor_copy(out=nT[:], in_=ps[0][:, 0:32]), r=[tps[0]], w=[tnT])
            P.op("dve", lambda e: e.tensor_scalar(out=vT[:, 0, 0:64], in0=vT[:, 0, 0:64], scalar1=bs[:, 0:1], scalar2=None,
                                                  op0=ALU.mult), r=[tvT, tbs], w=[tvT])
            P.op("dve", lambda e: e.scalar_tensor_tensor(out=vT[:, 0, 0:64], in0=vT[:, 0, 64:128], scalar=bs[:, 1:2],
                                                         in1=vT[:, 0, 0:64], op0=ALU.mult, op1=ALU.add), r=[tvT, tbs], w=[tvT])
            for n2 in (0, 1):
                for grp in (0, 1):
                    ai = n2 * 4 + grp * 2
                    sh = vT[:, grp, (2 * n2) * 16:(2 * n2) * 16 + 16]
                    sc = vT[:, grp, (2 * n2 + 1) * 16:(2 * n2 + 1) * 16 + 16]
                    P.op("dve", lambda e, ai=ai, sc=sc, n2=n2: e.scalar_tensor_tensor(
                        out=AB[:, ai, :], in0=sc, scalar=1.0, in1=nT[:, n2 * 16:(n2 + 1) * 16], op0=ALU.add, op1=ALU.mult),
                        r=[tvT, tnT], w=[tAB])
                    P.op("dve", lambda e, ai=ai, sh=sh: e.tensor_copy(out=AB[:, ai + 1, :], in_=sh), r=[tvT], w=[tAB])

        def load_gates(l, w6, ph):
            g0 = k.sb("g0", [128, D], F32, ph)
            gl = k.sb("gl", [128, D], F32, ph)
            gc = k.sb("gc", [128, D], F32, ph)
            tg0, tgl, tgc = T(), T(), T()
            P.dma("sp", lambda e: e.dma_start(out=g0[:].rearrange("p (r j) -> p r j", r=8), in_=mvec_ap(l, 0, w6, bcast=128)),
                  r=[tm_all], w=[tg0])
            P.dma("sp", lambda e: e.dma_start(out=gl[:].rearrange("p (r j) -> p r j", r=8), in_=mvec_ap(l, 1, w6, bcast=128)),
                  r=[tm_all], w=[tgl])
            P.dma("sp", lambda e: e.dma_start(out=gc[:].rearrange("p (r j) -> p r j", r=8), in_=mvec_ap(l, 2, w6, bcast=128)),
                  r=[tm_all], w=[tgc])
            P.op("pool", lambda e: e.tensor_scalar(out=gl[:], in0=gl[:], scalar1=bs[:, 1:2], scalar2=None, op0=ALU.mult),
                 r=[tgl, tbs], w=[tgl])
            P.op("dve", lambda e: e.scalar_tensor_tensor(out=gl[:], in0=g0[:], scalar=bs[:, 0:1], in1=gl[:], op0=ALU.mult,
                                                         op1=ALU.add), r=[tg0, tgl, tbs], w=[tgl])
            return gl, tgl, gc, tgc

        xn = [k.sb("xn%d" % i, [128, D], BF16) for i in range(2)]
        txn = [T(), T()]
        ss = k.sb("ss", [128, 2], F32)
        tss = [T(), T()]

        def norm_mod(n2, tiles):
            for i, t in enumerate(tiles):
                b = i % 2
                P.op("act", lambda e, t=t, b=b: e.activation(out=xn[b][:], in_=xres[:, t, :], func=AF.Square,
                                                             accum_out=ss[:, b:b + 1]), r=[tx[t]], w=[txn[b], tss[b]])
                P.op("act", lambda e, b=b: e.activation(out=ss[:, b:b + 1], in_=ss[:, b:b + 1], func=AF.Sqrt, bias=eps[:],
                                                        scale=1.0 / D), r=[tss[b], teps], w=[tss[b]])
                P.op("dve", lambda e, b=b: e.reciprocal(out=ss[:, b:b + 1], in_=ss[:, b:b + 1]), r=[tss[b]], w=[tss[b]])
                P.op("dve", lambda e, t=t, b=b: e.tensor_scalar(out=xn[b][:], in0=xres[:, t, :], scalar1=ss[:, b:b + 1],
                                                                scalar2=None, op0=ALU.mult), r=[tx[t], tss[b]], w=[txn[b]])
                ai = n2 * 4 + (2 if t == 8 else 0)
                for q4 in range(4):
                    pp = 2 + (q4 % 2)
                    pview = ps[pp][:].bitcast(BF16)
                    for j in range(4):
                        c = q4 * 4 + j
                        P.op("pe", lambda e, c=c, j=j, b=b, pview=pview: e.transpose(pview[:, j * 128:(j + 1) * 128],
                                                                                    xn[b][:, c * 128:(c + 1) * 128], ident[:]),
                             r=[txn[b], tid], w=[tps[pp]])
                    for j in range(4):
                        c = q4 * 4 + j
                        P.op("dve", lambda e, c=c, j=j, t=t, ai=ai, pview=pview: e.tensor_scalar(
                            out=hT[:, c, t * 128:(t + 1) * 128], in0=pview[:, j * 128:(j + 1) * 128],
                            scalar1=AB[:, ai, cprime(c):cprime(c) + 1], scalar2=AB[:, ai + 1, cprime(c):cprime(c) + 1],
                            op0=ALU.mult, op1=ALU.add), r=[tps[pp], tAB], w=[thT[t]])

        NTOK = NT * 128
        Gd = k.idram("Gd", [NT, 128, 16384], BF16)
        tGd = [T() for _ in range(NT)]
        EC = 256
        NEC = 16384 // EC
        KB = 8
        NB = 128 // KB
        BE = KB * 128

        def peer(l, tiles):
            wqf, twqf = wq_full[l]
            uf, tuf = u_full[l]
            vf, tvf = v_full[l]
            ph = contextlib.ExitStack()
            qT = k.sb("qT", [128, 16, 128], BF16, ph)
            tqT = T()
            wqb = [k.sb("wqb", [128, 16, 128], BF16, ph) for i in range(2)]
            twqb = [T(), T()]
            skT = k.sb("skT", [128, 2, 128], BF16, ph)
            tskT = T()
            sall = k.sb("sall", [128, 16, 128], F32, ph)
            tsall = T()
            tmpm = k.sb("tmpm", [128, 256], F32, ph)
            ttmpm = T()
            vtop = k.sb("vtop", [128, 16, 16], F32, ph)
            tvtop = T()
            cand = k.sb("cand", [128, 8, 256], F32, ph)
            tcand = T()
            sc = k.sb("sc", [128, 8, 16], F32, ph)
            tsc = T()
            sm = k.sb("sm", [128, 4, 8], F32, ph)
            tsm = T()
            dd = k.sb("dd", [128, 8, 16], F32, ph)
            tdd = T()
            Dq = [k.sb("Dq", [128, BE], F32, ph) for i in range(2)]
            tDq = [T(), T()]
            Eq = [k.sb("Eq", [128, BE], BF16, ph) for i in range(2)]
            tEq = [T(), T()]
            Gq = [k.sb("Gq", [128, BE], BF16, ph) for i in range(2)]
            tGq = [T(), T()]
            Gb = [k.sb("Gb", [128, BE], BF16, ph) for i in range(2)]
            tGb = [T(), T()]
            for h in range(2):
                P.dma("pool", lambda e, h=h: e.dma_start(out=skT[:, h, :], in_=skT_in.ap()[l, h]), w=[tskT])
            for t in tiles:
                for j in range(16):
                    b = j % 2
                    pp = 4 + j % 2
                    P.dma("sp", lambda e, j=j, b=b: e.dma_start(
                        out=wqb[b][:], in_=wqf.ap()[:, j * 128:(j + 1) * 128].rearrange("(k p) n -> p k n", p=128)),
                        r=[twqf], w=[twqb[b]])
                    for kk in range(16):
                        P.op("pe", lambda e, kk=kk, b=b, t=t, pp=pp: e.matmul(
                            ps[pp][:, 0:128], lhsT=wqb[b][:, kk, :], rhs=hT[:, kk, t * 128:(t + 1) * 128],
                            start=(kk == 0), stop=(kk == 15)), r=[twqb[b], thT[t]], w=[tps[pp]])
                    P.op("act", lambda e, j=j, pp=pp: e.copy(out=qT[:, j, :], in_=ps[pp][:, 0:128]), r=[tps[pp]], w=[tqT])
                for q4 in range(4):
                    pp = q4 % 2
                    for jj in range(4):
                        j = q4 * 4 + jj
                        P.op("pe", lambda e, j=j, jj=jj, pp=pp: e.matmul(
                            ps[pp][:, jj * 128:(jj + 1) * 128], lhsT=qT[:, j, :], rhs=skT[:, j % 2, :],
                            start=True, stop=True), r=[tqT, tskT], w=[tps[pp]])
                    P.op("act", lambda e, q4=q4, pp=pp: e.copy(out=sall[:, q4 * 4:(q4 + 1) * 4, :].rearrange("p a b -> p (a b)"),
                                                               in_=ps[pp][:]), r=[tps[pp]], w=[tsall])
                for j in range(16):
                    P.op("dve", lambda e, j=j: e.max(out=vtop[:, j, 0:8], in_=sall[:, j, :]), r=[tsall], w=[tvtop])
                    P.op("dve", lambda e, j=j: e.match_replace(out=tmpm[:, 0:128], in_to_replace=vtop[:, j, 0:8],
                                                               in_values=sall[:, j, :], imm_value=-1e30),
                         r=[tsall, tvtop], w=[ttmpm])
                    P.op("dve", lambda e, j=j: e.max(out=vtop[:, j, 8:16], in_=tmpm[:, 0:128]), r=[ttmpm], w=[tvtop])
                vt4 = vtop[:].rearrange("p (h two) a -> p h two a", two=2)
                P.op("dve", lambda e, vt4=vt4: e.tensor_tensor(
                    out=cand[:].rearrange("p h (a b) -> p h a b", a=16),
                    in0=vt4[:, :, 0, :].unsqueeze(3).to_broadcast([128, 8, 16, 16]),
                    in1=vt4[:, :, 1, :].unsqueeze(2).to_broadcast([128, 8, 16, 16]), op=ALU.add), r=[tvtop], w=[tcand])
                for h in range(8):
                    P.op("dve", lambda e, h=h: e.max(out=sc[:, h, 0:8], in_=cand[:, h, :]), r=[tcand], w=[tsc])
                    P.op("dve", lambda e, h=h: e.match_replace(out=tmpm[:], in_to_replace=sc[:, h, 0:8], in_values=cand[:, h, :],
                                                               imm_value=-1e30), r=[tcand, tsc], w=[ttmpm])
                    P.op("dve", lambda e, h=h: e.max(out=sc[:, h, 8:16], in_=tmpm[:]), r=[ttmpm], w=[tsc])
                P.op("dve", lambda e: e.tensor_scalar(out=sm[:, 0, :], in0=sc[:, :, 15], scalar1=-1.0, scalar2=None, op0=ALU.mult),
                     r=[tsc], w=[tsm])
                P.op("dve", lambda e: e.tensor_tensor(out=dd[:], in0=sc[:], in1=sm[:, 0, :].unsqueeze(2).to_broadcast([128, 8, 16]),
                                                      op=ALU.add), r=[tsc, tsm], w=[tdd])
                P.op("act", lambda e: e.activation(out=dd[:], in_=dd[:], func=AF.Exp), r=[tdd], w=[tdd])
                P.op("dve", lambda e: e.tensor_reduce(out=sm[:, 1, :], in_=dd[:], axis=AX.X, op=ALU.add), r=[tdd], w=[tsm])
                P.op("act", lambda e: e.activation(out=sm[:, 2, :], in_=sm[:, 1, :], func=AF.Ln), r=[tsm], w=[tsm])
                P.op("dve", lambda e: e.tensor_scalar(out=sm[:, 2, :], in0=sm[:, 2, :], scalar1=-1.0, scalar2=None, op0=ALU.mult),
                     r=[tsm], w=[tsm])
                it = 0
                for qq in range(NB):
                    gb = qq % 2
                    for h in range(8):
                        b = it % 2
                        it += 1
                        P.op("dve", lambda e, h=h, qq=qq, b=b: e.scalar_tensor_tensor(
                            out=Dq[b][:].rearrange("p (a c) -> p a c", a=KB),
                            in0=sall[:, 2 * h, qq * KB:(qq + 1) * KB].unsqueeze(2).to_broadcast([128, KB, 128]),
                            scalar=sm[:, 0, h:h + 1],
                            in1=sall[:, 2 * h + 1, :].unsqueeze(1).to_broadcast([128, KB, 128]),
                            op0=ALU.add, op1=ALU.add), r=[tsall, tsm], w=[tDq[b]])
                        P.op("act", lambda e, h=h, b=b: e.activation(out=Eq[b][:], in_=Dq[b][:], func=AF.Exp,
                                                                     bias=sm[:, 2, h:h + 1], scale=1.0),
                             r=[tDq[b], tsm], w=[tEq[b]])
                        if h == 0:
                            P.op("dve", lambda e, b=b, gb=gb: e.scalar_tensor_tensor(
                                out=Gb[gb][:], in0=Dq[b][:], scalar=-1e-5, in1=Eq[b][:], op0=ALU.is_ge, op1=ALU.mult),
                                r=[tDq[b], tEq[b]], w=[tGb[gb]])
                        else:
                            P.op("dve", lambda e, b=b: e.scalar_tensor_tensor(
                                out=Gq[b][:], in0=Dq[b][:], scalar=-1e-5, in1=Eq[b][:], op0=ALU.is_ge, op1=ALU.mult),
                                r=[tDq[b], tEq[b]], w=[tGq[b]])
                            P.op("pool", lambda e, b=b, gb=gb: e.tensor_tensor(out=Gb[gb][:], in0=Gb[gb][:], in1=Gq[b][:],
                                                                             op=ALU.add), r=[tGq[b], tGb[gb]], w=[tGb[gb]])
                    P.dma("sp", lambda e, t=t, qq=qq, gb=gb: e.dma_start(out=Gd.ap()[t, :, qq * BE:(qq + 1) * BE], in_=Gb[gb][:]),
                          r=[tGb[gb]], w=[tGd[t]])
            ph.close()
            P.barrier()
            ph = contextlib.ExitStack()
            usb = k.sb("usb", [128, 2, D], BF16, ph)
            tusb = T()
            vsb = [k.sb("vsb", [128, 2, D], BF16, ph) for i in range(2)]
            tvsb = [T(), T()]
            uT = k.sb("uT", [128, 16, EC], BF16, ph)
            tuT = T()
            gsb = [k.sb("gsb", [128, NT, EC], BF16, ph) for i in range(2)]
            tgsb = [T(), T()]
            asb = [k.sb("asb", [128, EC], BF16, ph) for i in range(2)]
            tasb = [T(), T()]
            wsb = [k.sb("wsb", [128, EC], BF16, ph) for i in range(2)]
            twsb = [T(), T()]
            wT = [k.sb("wT", [128, 2, 128], BF16, ph) for i in range(2)]
            twT = [T(), T()]
            acc = [k.sb("acc", [128, 512], F32, ph) for i in range(2)]
            tacc = [T(), T()]
            gl, tgl, gc, tgc = load_gates(l, 5, ph)
            ia = 0
            for ec in range(NEC):
                b = ec % 2
                e0 = ec * EC
                P.dma("sp", lambda e, e0=e0: e.dma_start(
                    out=usb[:], in_=uf.ap()[e0:e0 + EC, :].rearrange("(s p) f -> p s f", p=128)), r=[tuf], w=[tusb])
                P.dma("sp", lambda e, b=b, e0=e0: e.dma_start(
                    out=vsb[b][:], in_=vf.ap()[e0:e0 + EC, :].rearrange("(s p) f -> p s f", p=128)), r=[tvf], w=[tvsb[b]])
                P.dma("sp", lambda e, b=b, e0=e0: e.dma_start(
                    out=gsb[b][:], in_=Gd.ap()[:, :, e0:e0 + EC].rearrange("t p e -> p t e")), r=tGd, w=[tgsb[b]])
                it = 0
                for s in range(2):
                    for k4 in range(4):
                        pp = it % 2
                        it += 1
                        pview = ps[pp][:].bitcast(BF16)
                        for jj in range(4):
                            kk = k4 * 4 + jj
                            P.op("pe", lambda e, s=s, kk=kk, jj=jj, pview=pview: e.transpose(
                                pview[:, jj * 128:(jj + 1) * 128], usb[:, s, kk * 128:(kk + 1) * 128], ident[:]),
                                r=[tusb, tid], w=[tps[pp]])
                        P.op("act", lambda e, s=s, k4=k4, pview=pview: e.copy(
                            out=uT[:, k4 * 4:(k4 + 1) * 4, s * 128:(s + 1) * 128],
                            in_=pview[:, 0:512].rearrange("p (j n) -> p j n", j=4)), r=[tps[pp]], w=[tuT])
                for i, t in enumerate(tiles):
                    b2 = i % 2
                    for kk in range(16):
                        P.op("pe", lambda e, kk=kk, t=t: e.matmul(ps[2][:, 0:EC], lhsT=hT[:, kk, t * 128:(t + 1) * 128],
                                                                 rhs=uT[:, kk, :], start=(kk == 0), stop=(kk == 15)),
                             r=[thT[t], tuT], w=[tps[2]])
                    P.op("act", lambda e, b2=b2: e.activation(out=asb[b2][:], in_=ps[2][:, 0:EC], func=GELU),
                         r=[tps[2]], w=[tasb[b2]])
                    P.op("pool", lambda e, b2=b2, b=b, t=t: e.tensor_tensor(out=wsb[b2][:], in0=asb[b2][:], in1=gsb[b][:, t, :],
                                                                          op=ALU.mult), r=[tasb[b2], tgsb[b]], w=[twsb[b2]])
                    pview = ps[3][:].bitcast(BF16)
                    for s in range(2):
                        P.op("pe", lambda e, s=s, b2=b2, pview=pview: e.transpose(pview[:, s * 128:(s + 1) * 128],
                                                                                wsb[b2][:, s * 128:(s + 1) * 128], ident[:]),
                             r=[twsb[b2], tid], w=[tps[3]])
                    P.op("act", lambda e, b2=b2, pview=pview: e.copy(out=wT[b2][:].rearrange("p s n -> p (s n)"), in_=pview[:, 0:256]),
                         r=[tps[3]], w=[twT[b2]])
                    gt, tg = (gc, tgc) if t == 8 else (gl, tgl)
                    for fc in range(4):
                        for s in range(2):
                            P.op("pe", lambda e, fc=fc, s=s, b=b, b2=b2: e.matmul(
                                ps[4 + fc][:], lhsT=wT[b2][:, s, :], rhs=vsb[b][:, s, fc * 512:(fc + 1) * 512],
                                start=(s == 0), stop=(s == 1)), r=[twT[b2], tvsb[b]], w=[tps[4 + fc]])
                    for fc in range(4):
                        a2 = ia % 2
                        ia += 1
                        P.op("dve", lambda e, fc=fc, a2=a2, gt=gt: e.tensor_tensor(
                            out=acc[a2][:], in0=ps[4 + fc][:], in1=gt[:, fc * 512:(fc + 1) * 512], op=ALU.mult),
                            r=[tps[4 + fc], tg], w=[tacc[a2]])
                        P.op("pool", lambda e, t=t, fc=fc, a2=a2: e.tensor_tensor(
                            out=xres[:, t, fc * 512:(fc + 1) * 512], in0=xres[:, t, fc * 512:(fc + 1) * 512], in1=acc[a2][:],
                            op=ALU.add), r=[tacc[a2], tx[t]], w=[tx[t]])
            ph.close()
            P.barrier()

        def proj(Wf, tWf, col0, ncols, tiles, consume, ph, bw=256, nk=16, krow0=0, src=None, tsrc=None, kofs=0):
            src = hT if src is None else src
            tsrc = thT if tsrc is None else tsrc
            wb = [k.sb("wblk", [128, nk, bw], BF16, ph) for i in range(2)]
            twb = [T(), T()]
            it = 0
            for bi, c0 in enumerate(range(col0, col0 + ncols, bw)):
                b = bi % 2
                P.dma("sp" if Wf.ap().dtype == BF16 else "pool", lambda e, b=b, c0=c0: e.dma_start(
                    out=wb[b][:], in_=Wf.ap()[krow0:krow0 + nk * 128, c0:c0 + bw].rearrange("(k p) n -> p k n", p=128)),
                    r=[tWf], w=[twb[b]])
                for t in tiles:
                    pp = 6 + it % 2
                    it += 1
                    for kk in range(nk):
                        P.op("pe", lambda e, kk=kk, b=b, t=t, pp=pp: e.matmul(
                            ps[pp][:, 0:bw], lhsT=src[:, kofs + kk, t * 128:(t + 1) * 128], rhs=wb[b][:, kk, :],
                            start=(kk == 0), stop=(kk == nk - 1)), r=[tsrc[t], twb[b]], w=[tps[pp]])
                    consume(t, c0 // bw, ps[pp][:, 0:bw], tps[pp])

        def out_proj_residual(l, Wf, tWf, tiles, ph, gates):
            gl, tgl, gc, tgc = gates
            tmp = [k.sb("optmp", [128, 256], F32, ph) for i in range(2)]
            ttmp = [T(), T()]
            cnt = [0]

            def consume(t, bi, pap, tpp):
                a2 = cnt[0] % 2
                cnt[0] += 1
                gt, tg = (gc, tgc) if t == 8 else (gl, tgl)
                c0 = bi * 256
                P.op("dve", lambda e: e.tensor_tensor(out=tmp[a2][:], in0=pap, in1=gt[:, c0:c0 + 256], op=ALU.mult),
                     r=[tpp, tg], w=[ttmp[a2]])
                P.op("pool", lambda e: e.tensor_tensor(out=xres[:, t, c0:c0 + 256], in0=xres[:, t, c0:c0 + 256], in1=tmp[a2][:],
                                                       op=ALU.add), r=[ttmp[a2], tx[t]], w=[tx[t]])
            proj(Wf, tWf, 0, D, tiles, consume, ph)

        if mixers and any(l % 3 == 0 for l in layers):
            nA = sorted(set(l // 3 for l in layers if l % 3 == 0))
            awqkv_sh = k.inp("a_wqkv", [len(nA), 512, 3072])
            awo_sh = k.inp("a_wo", [len(nA), 512, D])
            again = k.inp("a_gain", [2, 2, 128])
            rope_in = k.inp("rope", [8, 128, 256])
            awqkv_full, awo_full = {}, {}
            for ji, j in enumerate(nA):
                awqkv_full[j] = k.gather_full("awqkv%d" % j, _sub(awqkv_sh, ji), 512, 3072)
                awo_full[j] = k.gather_full("awo%d" % j, _sub(awo_sh, ji), 512, D, cast=True)
            kvl_in = k.idram("kvl_in", [1024, 1024], BF16)
            kvc_in = k.idram("kvc_in", [64, 1024], BF16)
        SCALE = 128 ** -0.5

        def mixer_a(l, tiles):
            j = l // 3
            last = (l == DEPTH - 1)
            Wf, tWf = awqkv_full[j]
            ph0 = contextlib.ExitStack()
            qT = k.sb("qTa", [128, 16, NT * 128], BF16, ph0)
            tqT = [T() for _ in range(NT)]
            ph = contextlib.ExitStack()
            gq = k.sb("gq", [128, 2, 128], F32, ph)
            tgq = T()
            for i2 in range(2):
                P.dma("sp", lambda e, i2=i2: e.dma_start(out=gq[:, i2, :], in_=again.ap()[j, i2:i2 + 1, :].partition_broadcast(128)),
                      w=[tgq])
            rp_ = [k.sb("ropeb", [128, 256], F32, ph) for i in range(2)]
            trp = [T(), T()]
            sq = [k.sb("sq", [128, 256], F32, ph) for i in range(2)]
            tsq = [T(), T()]
            s2 = [k.sb("s2", [128, 2], F32, ph) for i in range(2)]
            ts2 = [T(), T()]
            qn = [k.sb("qn", [128, 256], F32, ph) for i in range(2)]
            tqn = [T(), T()]
            qc = [k.sb("qc", [128, 256], F32, ph) for i in range(2)]
            tqc = [T(), T()]
            qf = [k.sb("qf", [128, 256], BF16, ph) for i in range(2)]
            tqf = [T(), T()]
            tkvl, tkvc = T(), T()
            cnt = [0]
            ropetile = [None, None]

            def consume(t, bi, pap, tpp):
                b = cnt[0] % 2
                cnt[0] += 1
                c0 = bi * 256
                isq, isk, isv = c0 < 2048, 2048 <= c0 < 2560, c0 >= 2560
                if isv:
                    P.op("act", lambda e: e.copy(out=qf[b][:], in_=pap), r=[tpp], w=[tqf[b]])
                else:
                    gi = 0 if isq else 1
                    P.op("act", lambda e: e.activation(out=sq[b][:], in_=pap, func=AF.Square), r=[tpp], w=[tsq[b]])
                    P.op("dve", lambda e: e.tensor_reduce(out=s2[b][:], in_=sq[b][:].rearrange("p (h d) -> p h d", h=2), axis=AX.X,
                                                          op=ALU.add), r=[tsq[b]], w=[ts2[b]])
                    P.op("act", lambda e: e.activation(out=s2[b][:], in_=s2[b][:], func=AF.Sqrt, bias=eps[:], scale=1.0 / 128),
                         r=[ts2[b], teps], w=[ts2[b]])
                    P.op("dve", lambda e: e.reciprocal(out=s2[b][:], in_=s2[b][:]), r=[ts2[b]], w=[ts2[b]])
                    P.op("dve", lambda e: e.tensor_tensor(out=qn[b][:].rearrange("p (h d) -> p h d", h=2),
                                                          in0=pap.rearrange("p (h d) -> p h d", h=2),
                                                          in1=s2[b][:].unsqueeze(2).to_broadcast([128, 2, 128]), op=ALU.mult),
                         r=[tpp, ts2[b]], w=[tqn[b]])
                    if t == 8:
                        P.op("pool", lambda e: e.tensor_tensor(out=qf[b][:].rearrange("p (h d) -> p h d", h=2),
                                                               in0=qn[b][:].rearrange("p (h d) -> p h d", h=2),
                                                               in1=gq[:, gi, :].unsqueeze(1).to_broadcast([128, 2, 128]), op=ALU.mult),
                             r=[tqn[b], tgq], w=[tqf[b]])
                    else:
                        P.op("pool", lambda e: e.tensor_tensor(out=qn[b][:].rearrange("p (h d) -> p h d", h=2),
                                                               in0=qn[b][:].rearrange("p (h d) -> p h d", h=2),
                                                               in1=gq[:, gi, :].unsqueeze(1).to_broadcast([128, 2, 128]), op=ALU.mult),
                             r=[tqn[b], tgq], w=[tqn[b]])
                        rb = t % 2
                        if ropetile[rb] != t:
                            ropetile[rb] = t
                            P.dma("sp", lambda e: e.dma_start(out=rp_[rb][:], in_=rope_in.ap()[t]), w=[trp[rb]])
                        cosv = rp_[rb][:, 0:128]
                        sinv = rp_[rb][:, 128:256].rearrange("p (a b f) -> p a b f", a=2, b=2)
                        q5 = qn[b][:].rearrange("p (h a b f) -> p h a b f", h=2, a=2, b=2)
                        c5 = qc[b][:].rearrange("p (h a b f) -> p h a b f", h=2, a=2, b=2)
                        for hh in range(2):
                            P.op("dve", lambda e, hh=hh: e.tensor_tensor(
                                out=c5[:, :, :, hh, :], in0=q5[:, :, :, 1 - hh, :],
                                in1=sinv[:, :, hh, :].unsqueeze(1).to_broadcast([128, 2, 2, 32]), op=ALU.mult),
                                r=[tqn[b], trp[rb]], w=[tqc[b]])
                        P.op("pool", lambda e: e.tensor_tensor(out=qn[b][:].rearrange("p (h d) -> p h d", h=2),
                                                               in0=qn[b][:].rearrange("p (h d) -> p h d", h=2),
                                                               in1=cosv.unsqueeze(1).to_broadcast([128, 2, 128]), op=ALU.mult),
                             r=[tqn[b], trp[rb]], w=[tqn[b]])
                        P.op("pool", lambda e: e.tensor_tensor(out=qf[b][:], in0=qn[b][:], in1=qc[b][:], op=ALU.add),
                             r=[tqn[b], tqc[b]], w=[tqf[b]])
                if isq:
                    pview = ps[2 + bi % 2][:].bitcast(BF16)
                    tp_ = tps[2 + bi % 2]
                    for hh in range(2):
                        P.op("pe", lambda e, hh=hh: e.transpose(pview[:, hh * 128:(hh + 1) * 128], qf[b][:, hh * 128:(hh + 1) * 128],
                                                                ident[:]), r=[tqf[b], tid], w=[tp_])
                    P.op("act", lambda e: e.copy(out=qT[:, 2 * bi:2 * bi + 2, t * 128:(t + 1) * 128],
                                                 in_=pview[:, 0:256].rearrange("p (h n) -> p h n", h=2)), r=[tp_], w=[tqT[t]])
                else:
                    cc0 = c0 - 2048
                    if t == 8:
                        P.dma("sp", lambda e: e.dma_start(out=kvc_in.ap()[:, cc0:cc0 + 256], in_=qf[b][0:64, :]),
                              r=[tqf[b]], w=[tkvc])
                    else:
                        P.dma("sp", lambda e: e.dma_start(out=kvl_in.ap()[t * 128:(t + 1) * 128, cc0:cc0 + 256], in_=qf[b][:]),
                              r=[tqf[b]], w=[tkvl])

            qtiles = tiles
            proj(Wf, tWf, 0, 2048, qtiles, consume, ph)
            proj(Wf, tWf, 2048, 1024, list(range(NT)), consume, ph)
            ph.close()
            P.barrier()
            kvl_all, tkvl_all = k.gather_full("kvl%d" % l, _V(kvl_in.ap()), 1024, 1024, BF16, rdeps=[tkvl])
            kvc_all, tkvc_all = k.gather_full("kvc%d" % l, _V(kvc_in.ap()), 64, 1024, BF16, rdeps=[tkvc])
            ph = contextlib.ExitStack()
            NKC = 34
            kT = k.sb("kTa", [128, NKC * 128], BF16, ph)
            tkT = T()
            vv = k.sb("vva", [128, NKC, 129], BF16, ph)
            tvv = T()
            kin = [k.sb("kin", [128, 4, 128], BF16, ph) for i in range(2)]
            tkin = [T(), T()]
            pT = [k.sb("pTa", [128, 512], BF16, ph) for i in range(3)]
            tpT = [T(), T(), T()]
            rs = k.sb("rsa", [128, 4], F32, ph)
            trs = T()
            otok = [k.sb("otok", [128, 512], BF16, ph) for i in range(2)]
            totok = [T(), T()]
            P.op("pool", lambda e: e.memset(vv[:, :, 128:129], 1.0), w=[tvv])

            def keyrows(c4):
                if c4 * 4 * 128 < 256:
                    return kvc_all, tkvc_all, c4 * 512
                return kvl_all, tkvl_all, c4 * 512 - 256
            ipc = [0]

            def do_group(g):
                ip = ipc[0]
                srcs = [(kvc_all, tkvc_all, 0, 2)] + [(kvl_all, tkvl_all, r0, 4) for r0 in range(0, 4096, 512)]
                kc = 0
                for (srcT, tsrcT, r0, nch) in srcs:
                    b = ip % 2
                    ip += 1
                    P.dma("sp", lambda e, b=b, srcT=srcT, r0=r0, nch=nch: e.dma_start(
                        out=kin[b][:, 0:nch, :],
                        in_=srcT.ap()[r0:r0 + nch * 128, g * 128:(g + 1) * 128].rearrange("(c p) d -> p c d", p=128)),
                        r=[tsrcT], w=[tkin[b]])
                    pview = ps[b][:].bitcast(BF16)
                    for cc in range(nch):
                        P.op("pe", lambda e, b=b, cc=cc, pview=pview: e.transpose(pview[:, cc * 128:(cc + 1) * 128], kin[b][:, cc, :],
                                                                                ident[:]), r=[tkin[b], tid], w=[tps[b]])
                    P.op("act", lambda e, kc=kc, nch=nch, pview=pview: e.copy(out=kT[:, kc * 128:(kc + nch) * 128],
                                                                            in_=pview[:, 0:nch * 128]), r=[tps[b]], w=[tkT])
                    P.dma("sp", lambda e, srcT=srcT, r0=r0, nch=nch, kc=kc: e.dma_start(
                        out=vv[:, kc:kc + nch, 0:128],
                        in_=srcT.ap()[r0:r0 + nch * 128, 512 + g * 128:512 + (g + 1) * 128].rearrange("(c p) d -> p c d", p=128)),
                        r=[tsrcT], w=[tvv])
                    kc += nch
                for ti, t in enumerate(tiles):
                    chunks = [0, 1] if t == 8 else list(range(NKC))
                    for ci, kc in enumerate(chunks):
                        sp_ = ci % 2
                        pb = ci % 3
                        P.op("pe", lambda e, kc=kc, t=t, sp_=sp_: e.matmul(
                            ps[sp_][:], lhsT=kT[:, kc * 128:(kc + 1) * 128], rhs=qT[:, 4 * g:4 * g + 4, t * 128:(t + 1) * 128],
                            start=True, stop=True), r=[tkT, tqT[t]], w=[tps[sp_]])
                        P.op("act", lambda e, sp_=sp_, pb=pb: e.activation(out=pT[pb][:], in_=ps[sp_][:], func=AF.Exp, scale=SCALE),
                             r=[tps[sp_]], w=[tpT[pb]])
                        for hh in range(4):
                            po = ps[2 + hh // 2]
                            off = (hh % 2) * 129
                            P.op("pe", lambda e, hh=hh, kc=kc, pb=pb, po=po, off=off, ci=ci, n=len(chunks): e.matmul(
                                po[:, off:off + 129], lhsT=pT[pb][:, hh * 128:(hh + 1) * 128], rhs=vv[:, kc, :],
                                start=(ci == 0), stop=(ci == n - 1)), r=[tpT[pb], tvv], w=[tps[2 + hh // 2]])
                    ob = ti % 2
                    for hh in range(4):
                        po = ps[2 + hh // 2]
                        off = (hh % 2) * 129
                        P.op("dve", lambda e, hh=hh, po=po, off=off: e.reciprocal(out=rs[:, hh:hh + 1], in_=po[:, off + 128:off + 129]),
                             r=[tps[2 + hh // 2]], w=[trs])
                        P.op("dve", lambda e, hh=hh, po=po, off=off, ob=ob: e.tensor_scalar(
                            out=otok[ob][:, hh * 128:(hh + 1) * 128], in0=po[:, off:off + 128], scalar1=rs[:, hh:hh + 1],
                            scalar2=None, op0=ALU.mult), r=[tps[2 + hh // 2], trs], w=[totok[ob]])
                    pview = ps[4 + ti % 2][:].bitcast(BF16)
                    tp_ = tps[4 + ti % 2]
                    for hh in range(4):
                        P.op("pe", lambda e, hh=hh, ob=ob, pview=pview: e.transpose(pview[:, hh * 128:(hh + 1) * 128],
                                                                                  otok[ob][:, hh * 128:(hh + 1) * 128], ident[:]),
                             r=[totok[ob], tid], w=[tp_])
                    P.op("act", lambda e, t=t, pview=pview: e.copy(out=hT[:, 4 * g:4 * g + 4, t * 128:(t + 1) * 128],
                                                                   in_=pview[:, 0:512].rearrange("p (h n) -> p h n", h=4)),
                         r=[tp_], w=[thT[t]])
                ipc[0] = ip
            for g in range(4):
                do_group(g)
            ph.close()
            ph0.close()
            P.barrier()
            ph = contextlib.ExitStack()
            gates = load_gates(l, 2, ph)
            out_proj_residual(l, awo_full[j][0], awo_full[j][1], tiles, ph, gates)
            ph.close()
            P.barrier()

        if mixers and any(l % 3 == 2 for l in layers):
            poolw_sh = k.inp("pool_w", [512, 512])
            pools_in = k.inp("pool_scale", [1, D])
            poolM_in = k.inp("poolM", [NT, 128, 12, 128])
            poolH_in = k.inp("poolH", [NT, 64, 4, 128])
            sel_in = k.inp("poolSel", [256, 64])
            poolw_full = k.gather_full("poolw", poolw_sh, 512, 512)
            xe_in = k.idram("xe_in", [64, D], BF16)

        def mixer_c(l, tiles):
            ph = contextlib.ExitStack()
            xna = k.sb("xna", [128, NT, D], BF16, ph)
            txna = [T() for _ in range(NT)]
            txe = T()
            for i, t in enumerate(range(NT)):
                b = i % 2
                P.op("act", lambda e, t=t, b=b: e.activation(out=xna[:, t, :], in_=xres[:, t, :], func=AF.Square,
                                                             accum_out=ss[:, b:b + 1]), r=[tx[t]], w=[txna[t], tss[b]])
                P.op("act", lambda e, b=b: e.activation(out=ss[:, b:b + 1], in_=ss[:, b:b + 1], func=AF.Sqrt, bias=eps[:],
                                                        scale=1.0 / D), r=[tss[b], teps], w=[tss[b]])
                P.op("dve", lambda e, b=b: e.reciprocal(out=ss[:, b:b + 1], in_=ss[:, b:b + 1]), r=[tss[b]], w=[tss[b]])
                P.op("dve", lambda e, t=t, b=b: e.tensor_scalar(out=xna[:, t, :], in0=xres[:, t, :], scalar1=ss[:, b:b + 1],
                                                                scalar2=None, op0=ALU.mult), r=[tx[t], tss[b]], w=[txna[t]])
            for (r0, p0, t) in ((0, 0, 0), (16, 112, 7), (32, 0, 8), (48, 48, 8)):
                P.dma("sp", lambda e, r0=r0, p0=p0, t=t: e.dma_start(out=xe_in.ap()[r0:r0 + 16, :], in_=xna[p0:p0 + 16, t, :]),
                      r=[txna[t]], w=[txe])
            xe_all, txe_all = k.gather_full("xe%d" % l, _V(xe_in.ap()), 64, D, BF16, rdeps=[txe])
            xes = k.sb("xes", [128, 2, D], BF16, ph)
            txes = T()
            sels = k.sb("sels", [128, 2, 64], BF16, ph)
            tsels = T()
            hal = k.sb("hal", [64, D], BF16, ph)
            thal = T()
            P.dma("sp", lambda e: e.dma_start(out=xes[:], in_=xe_all.ap().rearrange("(c p) f -> p c f", p=128)), r=[txe_all], w=[txes])
            P.dma("pool", lambda e: e.dma_start(out=sels[:], in_=sel_in.ap().rearrange("(c p) f -> p c f", p=128)), w=[tsels])
            for fb in range(4):
                pp = fb % 2
                for c in range(2):
                    P.op("pe", lambda e, fb=fb, c=c, pp=pp: e.matmul(ps[pp][0:64, :], lhsT=sels[:, c, :],
                                                                    rhs=xes[:, c, fb * 512:(fb + 1) * 512], start=(c == 0), stop=(c == 1)),
                         r=[tsels, txes], w=[tps[pp]])
                P.op("act", lambda e, fb=fb, pp=pp: e.copy(out=hal[:, fb * 512:(fb + 1) * 512], in_=ps[pp][0:64, :]), r=[tps[pp]], w=[thal])
            Mt = [k.sb("Mt", [128, 12, 128], BF16, ph) for i in range(2)]
            tMt = [T(), T()]
            Mh = [k.sb("Mh", [64, 4, 128], BF16, ph) for i in range(2)]
            tMh = [T(), T()]
            it = 0
            for i, t in enumerate(tiles):
                b = i % 2
                P.dma("pool", lambda e, t=t, b=b: e.dma_start(out=Mt[b][:], in_=poolM_in.ap()[t]), w=[tMt[b]])
                P.dma("pool", lambda e, t=t, b=b: e.dma_start(out=Mh[b][:], in_=poolH_in.ap()[t]), w=[tMh[b]])
                ai = 2 if t == 8 else 0
                for c in range(16):
                    g = c // 4
                    pp = 2 + it % 4
                    it += 1
                    srcs = [(xna[:, t, c * 128:(c + 1) * 128], Mt[b][:, g, :], [txna[t], tMt[b]])]
                    if 1 <= t <= 7:
                        srcs.append((xna[:, t - 1, c * 128:(c + 1) * 128], Mt[b][:, 4 + g, :], [txna[t - 1], tMt[b]]))
                    if t <= 6:
                        srcs.append((xna[:, t + 1, c * 128:(c + 1) * 128], Mt[b][:, 8 + g, :], [txna[t + 1], tMt[b]]))
                    if t in (0, 7):
                        srcs.append((hal[0:32, c * 128:(c + 1) * 128], Mh[b][0:32, g, :], [thal, tMh[b]]))
                    if t == 8:
                        srcs.append((hal[32:64, c * 128:(c + 1) * 128], Mh[b][32:64, g, :], [thal, tMh[b]]))
                    for si, (lh, rh, deps) in enumerate(srcs):
                        P.op("pe", lambda e, lh=lh, rh=rh, pp=pp, si=si, n=len(srcs): e.matmul(
                            ps[pp][:, 0:128], lhsT=lh, rhs=rh, start=(si == 0), stop=(si == n - 1)), r=deps, w=[tps[pp]])
                    P.op("dve", lambda e, c=c, t=t, ai=ai, pp=pp: e.tensor_scalar(
                        out=hT[:, c, t * 128:(t + 1) * 128], in0=ps[pp][:, 0:128], scalar1=AB[:, ai, cprime(c):cprime(c) + 1],
                        scalar2=None, op0=ALU.mult), r=[tps[pp], tAB], w=[thT[t]])
            ph.close()
            P.barrier()
            ph = contextlib.ExitStack()
            gl, tgl, gc, tgc = load_gates(l, 2, ph)
            lsb = k.sb("lsb", [128, D], F32, ph)
            tlsb = T()
            P.dma("sp", lambda e: e.dma_start(out=lsb[:], in_=pools_in.ap().partition_broadcast(128)), w=[tlsb])
            P.op("dve", lambda e: e.tensor_tensor(out=gl[:], in0=gl[:], in1=lsb[:], op=ALU.mult), r=[tgl, tlsb], w=[tgl])
            P.op("pool", lambda e: e.tensor_tensor(out=gc[:], in0=gc[:], in1=lsb[:], op=ALU.mult), r=[tgc, tlsb], w=[tgc])
            tmp = [k.sb("pctmp", [128, 256], F32, ph) for i in range(2)]
            ttmp = [T(), T()]
            cnt = [0]
            for g in range(4):
                def consume(t, bi, pap, tpp, g=g):
                    a2 = cnt[0] % 2
                    cnt[0] += 1
                    gt, tg = (gc, tgc) if t == 8 else (gl, tgl)
                    c0 = g * 512 + bi * 256
                    P.op("dve", lambda e: e.tensor_tensor(out=tmp[a2][:], in0=pap, in1=gt[:, c0:c0 + 256], op=ALU.mult),
                         r=[tpp, tg], w=[ttmp[a2]])
                    P.op("pool", lambda e: e.tensor_tensor(out=xres[:, t, c0:c0 + 256], in0=xres[:, t, c0:c0 + 256], in1=tmp[a2][:],
                                                           op=ALU.add), r=[ttmp[a2], tx[t]], w=[tx[t]])
                proj(poolw_full[0], poolw_full[1], 0, 512, tiles, consume, ph, nk=4, krow0=g * 512, kofs=4 * g)
            ph.close()
            P.barrier()

        if mixers and any(l % 3 == 1 for l in layers):
            bwqkv_sh = k.inp("b_wqkv", [512, 6144])
            bwo_sh = k.inp("b_wo", [512, D])
            biasT_in = k.inp("biasT", [5, 16, 8, 128, 128])
            idxB_in = k.inp("idxB", [128, 15], U32)
            bwqkv_full = k.gather_full("bwqkv", bwqkv_sh, 512, 6144)
            bwo_full = k.gather_full("bwo", bwo_sh, 512, D, cast=True)
            kvbl_in = k.idram("kvbl_in", [1024, 4096], BF16)
            kvbc_in = k.idram("kvbc_in", [64, 4096], BF16)
            win_kv = k.idram("win_kv", [1920, 4096], BF16)
        SLOT = [0, 1, 2, 2, 2, 2, 3, 4]

        def mixer_b(l, tiles):
            Wf, tWf = bwqkv_full
            ph0 = contextlib.ExitStack()
            qT = k.sb("qTb", [128, 16, NT * 128], BF16, ph0)
            tqT = [T() for _ in range(NT)]
            ph = contextlib.ExitStack()
            qf = [k.sb("qfb", [128, 256], BF16, ph) for i in range(2)]
            tqf = [T(), T()]
            tkvl, tkvc = T(), T()
            cnt = [0]

            def consume(t, bi, pap, tpp):
                b = cnt[0] % 2
                cnt[0] += 1
                c0 = bi * 256
                P.op("act", lambda e: e.copy(out=qf[b][:], in_=pap), r=[tpp], w=[tqf[b]])
                if c0 < 2048:
                    pview = ps[2 + bi % 2][:].bitcast(BF16)
                    tp_ = tps[2 + bi % 2]
                    for hh in range(2):
                        P.op("pe", lambda e, hh=hh: e.transpose(pview[:, hh * 128:(hh + 1) * 128], qf[b][:, hh * 128:(hh + 1) * 128],
                                                                ident[:]), r=[tqf[b], tid], w=[tp_])
                    P.op("act", lambda e: e.copy(out=qT[:, 2 * bi:2 * bi + 2, t * 128:(t + 1) * 128],
                                                 in_=pview[:, 0:256].rearrange("p (h n) -> p h n", h=2)), r=[tp_], w=[tqT[t]])
                else:
                    cc0 = c0 - 2048
                    if t == 8:
                        P.dma("sp", lambda e: e.dma_start(out=kvbc_in.ap()[:, cc0:cc0 + 256], in_=qf[b][0:64, :]), r=[tqf[b]], w=[tkvc])
                    else:
                        P.dma("sp", lambda e: e.dma_start(out=kvbl_in.ap()[t * 128:(t + 1) * 128, cc0:cc0 + 256], in_=qf[b][:]),
                              r=[tqf[b]], w=[tkvl])
            proj(Wf, tWf, 0, 6144, list(range(NT)), consume, ph)
            ph.close()
            P.barrier()
            kvl_all, tkvl_all = k.gather_full("kvbl%d" % l, _V(kvbl_in.ap()), 1024, 4096, BF16, rdeps=[tkvl])
            kvc_all, tkvc_all = k.gather_full("kvbc%d" % l, _V(kvbc_in.ap()), 64, 4096, BF16, rdeps=[tkvc])
            ph = contextlib.ExitStack()
            idxs = k.sb("idxs", [128, 15], U32, ph)
            tidx = T()
            wbuf = [k.sb("wbuf", [128, 4096], BF16, ph) for i in range(2)]
            twbuf = [T(), T()]
            twin = T()
            P.dma("sp", lambda e: e.dma_start(out=idxs[:], in_=idxB_in.ap()), w=[tidx])
            for wc in range(15):
                b = wc % 2
                P.dma("pool", lambda e, wc=wc, b=b: e.indirect_dma_start(
                    out=wbuf[b][:], out_offset=None, in_=kvl_all.ap(),
                    in_offset=bass.IndirectOffsetOnAxis(ap=idxs[:, wc:wc + 1], axis=0)), r=[tidx, tkvl_all], w=[twbuf[b]])
                P.dma("sp", lambda e, wc=wc, b=b: e.dma_start(out=win_kv.ap()[wc * 128:(wc + 1) * 128, :], in_=wbuf[b][:]),
                      r=[twbuf[b]], w=[twin])
            ph.close()
            P.barrier()
            ph = contextlib.ExitStack()
            NKC = 17
            kT = k.sb("kTb", [128, NKC * 128], BF16, ph)
            tkT = T()
            vv = k.sb("vvb", [128, NKC, 129], BF16, ph)
            tvv = T()
            kin = [k.sb("kinb", [128, 4, 128], BF16, ph) for i in range(2)]
            tkin = [T(), T()]
            bt = [k.sb("btb", [128, 4, 128], F32, ph) for i in range(2)]
            tbt = [T(), T()]
            sbb = [k.sb("sbb", [128, 512], F32, ph) for i in range(2)]
            tsbb = [T(), T()]
            pT = [k.sb("pTb", [128, 512], BF16, ph) for i in range(3)]
            tpT = [T(), T(), T()]
            rs = k.sb("rsb", [128, 1], F32, ph)
            trs = T()
            otok = [k.sb("otokb", [128, 128], BF16, ph) for i in range(2)]
            totok = [T(), T()]
            P.op("pool", lambda e: e.memset(vv[:, :, 128:129], 1.0), w=[tvv])
            ctr = dict(ip=0, ig=0, it=0)

            def do_head(h):
                srcs = [(win_kv, twin, r0, min(4, 15 - r0 // 128)) for r0 in range(0, 1920, 512)] + [(kvc_all, tkvc_all, 0, 2)]
                kc = 0
                for (srcT, tsrcT, r0, nch) in srcs:
                    b = ctr["ip"] % 2
                    ctr["ip"] += 1
                    P.dma("sp", lambda e, b=b, srcT=srcT, r0=r0, nch=nch: e.dma_start(
                        out=kin[b][:, 0:nch, :],
                        in_=srcT.ap()[r0:r0 + nch * 128, h * 128:(h + 1) * 128].rearrange("(c p) d -> p c d", p=128)),
                        r=[tsrcT], w=[tkin[b]])
                    pview = ps[b][:].bitcast(BF16)
                    for cc in range(nch):
                        P.op("pe", lambda e, b=b, cc=cc, pview=pview: e.transpose(pview[:, cc * 128:(cc + 1) * 128], kin[b][:, cc, :],
                                                                                ident[:]), r=[tkin[b], tid], w=[tps[b]])
                    P.op("act", lambda e, kc=kc, nch=nch, pview=pview: e.copy(out=kT[:, kc * 128:(kc + nch) * 128],
                                                                            in_=pview[:, 0:nch * 128]), r=[tps[b]], w=[tkT])
                    P.dma("sp", lambda e, srcT=srcT, r0=r0, nch=nch, kc=kc: e.dma_start(
                        out=vv[:, kc:kc + nch, 0:128],
                        in_=srcT.ap()[r0:r0 + nch * 128, 2048 + h * 128:2048 + (h + 1) * 128].rearrange("(c p) d -> p c d", p=128)),
                        r=[tsrcT], w=[tvv])
                    kc += nch
                for t in tiles:
                    if t == 8:
                        groups = [([15, 16], None)]
                    else:
                        groups = [([t, t + 1, t + 2, t + 3], 0), ([t + 4, t + 5, t + 6, t + 7], 4), ([15, 16], None)]
                    nall = sum(len(g_[0]) for g_ in groups)
                    done = 0
                    for (chs, boff) in groups:
                        ig = ctr["ig"]
                        ctr["ig"] += 1
                        sp_ = ig % 2
                        pb = ig % 3
                        n = len(chs)
                        for jj, kc in enumerate(chs):
                            P.op("pe", lambda e, jj=jj, kc=kc, t=t, sp_=sp_: e.matmul(
                                ps[sp_][:, jj * 128:(jj + 1) * 128], lhsT=kT[:, kc * 128:(kc + 1) * 128],
                                rhs=qT[:, h, t * 128:(t + 1) * 128], start=True, stop=True), r=[tkT, tqT[t]], w=[tps[sp_]])
                        if boff is not None:
                            bb = ig % 2
                            P.dma("sp", lambda e, t=t, boff=boff, bb=bb: e.dma_start(
                                out=bt[bb][:], in_=biasT_in.ap()[SLOT[t], h, boff:boff + 4].rearrange("c k q -> k c q")), w=[tbt[bb]])
                            P.op("dve", lambda e, sp_=sp_, bb=bb: e.scalar_tensor_tensor(
                                out=sbb[bb][:], in0=ps[sp_][:], scalar=SCALE, in1=bt[bb][:].rearrange("k c q -> k (c q)"),
                                op0=ALU.mult, op1=ALU.add), r=[tps[sp_], tbt[bb]], w=[tsbb[bb]])
                            P.op("act", lambda e, bb=bb, pb=pb: e.activation(out=pT[pb][:], in_=sbb[bb][:], func=AF.Exp),
                                 r=[tsbb[bb]], w=[tpT[pb]])
                        else:
                            P.op("act", lambda e, sp_=sp_, pb=pb, n=n: e.activation(out=pT[pb][:, 0:n * 128], in_=ps[sp_][:, 0:n * 128],
                                                                                  func=AF.Exp, scale=SCALE), r=[tps[sp_]], w=[tpT[pb]])
                        for jj, kc in enumerate(chs):
                            P.op("pe", lambda e, jj=jj, kc=kc, pb=pb, first=(done == 0), lastc=(done == nall - 1): e.matmul(
                                ps[2][:, 0:129], lhsT=pT[pb][:, jj * 128:(jj + 1) * 128], rhs=vv[:, kc, :],
                                start=first, stop=lastc), r=[tpT[pb], tvv], w=[tps[2]])
                            done += 1
                    ob = ctr["it"] % 2
                    ctr["it"] += 1
                    P.op("dve", lambda e: e.reciprocal(out=rs[:], in_=ps[2][:, 128:129]), r=[tps[2]], w=[trs])
                    P.op("dve", lambda e, ob=ob: e.tensor_scalar(out=otok[ob][:], in0=ps[2][:, 0:128], scalar1=rs[:, 0:1], scalar2=None,
                                                                 op0=ALU.mult), r=[tps[2], trs], w=[totok[ob]])
                    pview = ps[4 + ob][:].bitcast(BF16)
                    P.op("pe", lambda e, ob=ob, pview=pview: e.transpose(pview[:, 0:128], otok[ob][:], ident[:]),
                         r=[totok[ob], tid], w=[tps[4 + ob]])
                    P.op("act", lambda e, t=t, ob=ob, pview=pview: e.copy(out=hT[:, h, t * 128:(t + 1) * 128], in_=pview[:, 0:128]),
                         r=[tps[4 + ob]], w=[thT[t]])
            for h in range(16):
                do_head(h)
            ph.close()
            ph0.close()
            P.barrier()
            ph = contextlib.ExitStack()
            gates = load_gates(l, 2, ph)
            out_proj_residual(l, bwo_full[0], bwo_full[1], tiles, ph, gates)
            ph.close()
            P.barrier()

        for l in layers:
            last = (l == DEPTH - 1)
            tiles = list(range(8)) if last else list(range(NT))
            layer_vectors(l)
            if mixers:
                if l % 3 == 0:
                    norm_mod(0, list(range(NT)))
                    mixer_a(l, tiles)
                elif l % 3 == 1:
                    norm_mod(0, list(range(NT)))
                    mixer_b(l, tiles)
                else:
                    mixer_c(l, tiles)
            if do_peer:
                norm_mod(1, tiles)
                peer(l, tiles)

        fw = k.sb("fw", [128, D], F32)
        tfw = T()
        P.dma("sp", lambda e: e.dma_start(out=fw[:], in_=fnw.ap().partition_broadcast(128)), w=[tfw])
        ob = [k.sb("ob%d" % i, [128, D], F32) for i in range(2)]
        tob = [T(), T()]
        for t in range(8):
            b = t % 2
            P.op("act", lambda e, t=t, b=b: e.activation(out=xn[b][:], in_=xres[:, t, :], func=AF.Square, accum_out=ss[:, b:b + 1]),
                 r=[tx[t]], w=[txn[b], tss[b]])
            P.op("act", lambda e, b=b: e.activation(out=ss[:, b:b + 1], in_=ss[:, b:b + 1], func=AF.Sqrt, bias=eps[:], scale=1.0 / D),
                 r=[tss[b], teps], w=[tss[b]])
            P.op("dve", lambda e, b=b: e.reciprocal(out=ss[:, b:b + 1], in_=ss[:, b:b + 1]), r=[tss[b]], w=[tss[b]])
            P.op("dve", lambda e, t=t, b=b: e.scalar_tensor_tensor(out=ob[b][:], in0=xres[:, t, :], scalar=ss[:, b:b + 1], in1=fw[:],
                                                                   op0=ALU.mult, op1=ALU.mult), r=[tx[t], tss[b], tfw], w=[tob[b]])
            P.dma("sp", lambda e, t=t, b=b: e.dma_start(out=out.ap()[t], in_=ob[b][:]), r=[tob[b]])
        P.emit()
    return nc


class _V:
    def __init__(self, a):
        self._a = a

    def ap(self):
        return self._a


def _sub(t, l):
    class V:
        def __init__(self, a):
            self._a = a

        def ap(self):
            return self._a
    return V(t.ap()[l])


def host_inputs(inp, cfg):
    f = lambda a: np.ascontiguousarray(np.asarray(a, dtype=np.float32))
    x, c, ctx, c_ctx = f(inp["x"]), f(inp["c"]), f(inp["ctx"]), f(inp["c_ctx"])
    mod_w, mod_b = f(inp["mod_w"]), f(inp["mod_b"])
    maps = []
    mw5 = mod_w.reshape(DEPTH, D, 6, 8, 256)
    mb4 = mod_b.reshape(DEPTH, 6, 8, 256)
    skT = np.ascontiguousarray(f(inp["peer_sub_keys"]).transpose(0, 1, 3, 2))
    for cid in range(NC):
        b, kq = cid // 4, cid % 4
        xc = np.zeros((NT, 128, D), np.float32)
        xc[:8] = x[b, kq * 1024:(kq + 1) * 1024].reshape(8, 128, D)
        xc[8, :64] = ctx[b, kq * 64:(kq + 1) * 64]
        bsel = np.zeros((128, 2), np.float32)
        bsel[:, b] = 1.0
        m = {
            "x_c": xc,
            "c_all": np.stack([c[0], c[1], c_ctx]),
            "bsel": bsel,
            "modw": np.ascontiguousarray(mw5[:, :, :, cid, :]),
            "modb": np.ascontiguousarray(mb4[:, :, cid, :]).reshape(1, -1),
            "normw": f(inp["norm_w"]).reshape(DEPTH * 2, D),
            "fnw": f(inp["final_norm_w"]).reshape(1, D),
            "peer_wq": np.ascontiguousarray(f(inp["peer_wq"])[cfg["layers"], kq * 512:(kq + 1) * 512]),
            "skT": skT,
            "peer_u": np.ascontiguousarray(inp["peer_u"][cfg["layers"], kq * 4096:(kq + 1) * 4096], dtype=np.float32),
            "peer_v": np.ascontiguousarray(inp["peer_v"][cfg["layers"], kq * 4096:(kq + 1) * 4096], dtype=np.float32),
        }
        lays = cfg["layers"]
        if cfg.get("mixers", True) and any(l % 3 == 0 for l in lays):
            nA = sorted(set(l // 3 for l in lays if l % 3 == 0))
            m["a_wqkv"] = np.ascontiguousarray(f(inp["a_wqkv"])[nA, kq * 512:(kq + 1) * 512])
            m["a_wo"] = np.ascontiguousarray(f(inp["a_wo"])[nA, kq * 512:(kq + 1) * 512])
            m["a_gain"] = np.ascontiguousarray(np.stack([f(inp["a_q_gain"]), f(inp["a_k_gain"])], axis=1))
            m["rope"] = rope_tables(kq)
        if cfg.get("mixers", True) and any(l % 3 == 2 for l in lays):
            m["pool_w"] = np.ascontiguousarray(f(inp["pool_w"])[0, kq])
            m["pool_scale"] = f(inp["pool_scale"]).reshape(1, D)
            pm, phh, sel = pool_consts(kq)
            m["poolM"], m["poolH"], m["poolSel"] = pm, phh, sel
        if cfg.get("mixers", True) and any(l % 3 == 1 for l in lays):
            m["b_wqkv"] = np.ascontiguousarray(f(inp["b_wqkv"])[0, kq * 512:(kq + 1) * 512])
            m["b_wo"] = np.ascontiguousarray(f(inp["b_wo"])[0, kq * 512:(kq + 1) * 512])
            m["biasT"], m["idxB"] = nbr_consts(kq, f(inp["b_rpb"])[0])
        if not cfg.get("peer", True):
            for nm in ("peer_wq", "skT", "peer_u", "peer_v"):
                m.pop(nm)
        maps.append(m)
    return maps


def rope_tables(kq):
    tok = np.arange(kq * 1024, (kq + 1) * 1024)
    row = (tok // 64).astype(np.float32)
    col = (tok % 64).astype(np.float32)
    inv = (10000.0 ** (-np.arange(0, 64, 2, dtype=np.float32) / 64)).astype(np.float32)
    ar = row[:, None] * inv[None, :]
    ac = col[:, None] * inv[None, :]
    cos = np.concatenate([np.cos(ar), np.cos(ar), np.cos(ac), np.cos(ac)], axis=1)
    sin = np.concatenate([-np.sin(ar), np.sin(ar), -np.sin(ac), np.sin(ac)], axis=1)
    return np.ascontiguousarray(np.concatenate([cos, sin], axis=1).astype(np.float32).reshape(8, 128, 256))


CFG = dict(layers=[0, 1, 2, 3], peer=True, mixers=True)


def run(inp, cfg):
    nc = build(cfg)
    maps = host_inputs(inp, cfg)
    res = run_bass_kernel_spmd(nc, maps, core_ids=list(range(NC)))
    outp = np.zeros((2, 4096, D), np.float32)
    for cid in range(NC):
        b, kq = cid // 4, cid % 4
        outp[b, kq * 1024:(kq + 1) * 1024] = res.results[cid]["out"].reshape(1024, D)
    return outp


def kernel(**inputs):
    return run(inputs, CFG)


def pool_consts(kq):
    wins = (2, 4, 8, 16)

    def coef(tau, spos, w, L):
        lo = np.maximum(tau - w // 2, 0)
        hi = np.minimum(tau + w - w // 2, L)
        inwin = (spos[:, None] >= lo[None, :]) & (spos[:, None] < hi[None, :]) & (spos[:, None] >= 0) & (spos[:, None] < L)
        m = inwin / (hi - lo)[None, :].astype(np.float64)
        m = m - (spos[:, None] == tau[None, :])
        return m.astype(np.float32)
    PM = np.zeros((NT, 128, 12, 128), np.float32)
    PH = np.zeros((NT, 64, 4, 128), np.float32)
    for t in range(8):
        base = kq * 1024 + t * 128
        tau = base + np.arange(128)
        for g, w in enumerate(wins):
            PM[t, :, g, :] = coef(tau, base + np.arange(128), w, 4096)
            if t >= 1:
                PM[t, :, 4 + g, :] = coef(tau, base - 128 + np.arange(128), w, 4096)
            if t <= 6:
                PM[t, :, 8 + g, :] = coef(tau, base + 128 + np.arange(128), w, 4096)
            if t == 0:
                PH[t, 0:16, g, :] = coef(tau, base - 16 + np.arange(16), w, 4096)
            if t == 7:
                PH[t, 16:32, g, :] = coef(tau, base + 128 + np.arange(16), w, 4096)
    base = kq * 64
    tau = base + np.arange(128)
    valid = (np.arange(128) < 64)
    for g, w in enumerate(wins):
        m0 = coef(tau, base + np.arange(128), w, 256)
        m0[64:, :] = 0.0
        m0[:, ~valid] = 0.0
        PM[8, :, g, :] = m0
        mp = coef(tau, base - 16 + np.arange(16), w, 256)
        mn = coef(tau, base + 64 + np.arange(16), w, 256)
        mp[:, ~valid] = 0.0
        mn[:, ~valid] = 0.0
        PH[8, 32:48, g, :] = mp
        PH[8, 48:64, g, :] = mn
    sel = np.zeros((256, 64), np.float32)
    for i in range(16):
        if kq > 0:
            sel[(kq - 1) * 64 + 16 + i, i] = 1.0
            sel[(kq - 1) * 64 + 48 + i, 32 + i] = 1.0
        if kq < 3:
            sel[(kq + 1) * 64 + i, 16 + i] = 1.0
            sel[(kq + 1) * 64 + 32 + i, 48 + i] = 1.0
    return PM, PH, sel


def nbr_consts(kq, rpb):
    NEG = -30000.0
    out = np.full((5, 16, 8, 128, 128), NEG, np.float32)
    qr = np.arange(128) // 64
    qc = np.arange(128) % 64
    for slot, t in enumerate((0, 1, 2, 6, 7)):
        R0 = 16 * kq + 2 * t
        r = R0 + qr
        r0 = np.clip(r - 4, 0, 56)
        cs = np.clip(qc - 8, 0, 48)
        for jj in range(8):
            krow = (R0 - 7 + 2 * jj) + np.arange(128) // 64
            kcol = np.arange(128) % 64
            valid = ((krow[:, None] >= r0[None, :]) & (krow[:, None] < r0[None, :] + 8) &
                     (kcol[:, None] >= cs[None, :]) & (kcol[:, None] < cs[None, :] + 16) &
                     (krow[:, None] >= 0) & (krow[:, None] < 64))
            rr = np.clip(krow[:, None] - r[None, :] + 7, 0, 14)
            rc = np.clip(kcol[:, None] - qc[None, :] + 15, 0, 30)
            vals = rpb[:, rr, rc]
            out[slot, :, jj] = np.where(valid[None], vals, NEG)
    n = np.arange(1920)
    grow = np.clip(16 * kq - 7 + n // 64, 0, 63)
    idx = (grow * 64 + n % 64).astype(np.uint32).reshape(15, 128).T
    return out, np.ascontiguousarray(idx)
```

```python
import contextlib
import numpy as np
import concourse.bass as bass
import concourse.mybir as mybir
from concourse.bass_utils import run_bass_kernel_spmd

F32 = mybir.dt.float32
BF16 = mybir.dt.bfloat16
U32 = mybir.dt.uint32
AF = mybir.ActivationFunctionType
ALU = mybir.AluOpType
AX = mybir.AxisListType
GELU = AF.Gelu_apprx_tanh

NC = 8
D = 2048
NT = 9
DEPTH = 4
N_DMA_SEMS = 10
NCC = 4
QUEUES = ("sp", "act", "pool")
COMPUTE = ("pe", "act", "dve", "pool")
G4 = [[0, 1, 2, 3], [4, 5, 6, 7]]
G2 = [[0, 4], [1, 5], [2, 6], [3, 7]]


class T:
    __slots__ = ("w", "r")

    def __init__(self):
        self.w = None
        self.r = []


class Prog:
    def __init__(self, nc):
        self.nc = nc
        self.ops = []
        self.cnt = {e: 0 for e in ("pe", "act", "dve", "pool", "sp")}
        self.dma_cnt = {q: [0] * N_DMA_SEMS for q in QUEUES}
        self.dma_rr = {q: 0 for q in QUEUES}
        self.dma_last = {q: [None] * N_DMA_SEMS for q in QUEUES}
        self.cc_cnt = [0] * NCC
        self.cc_last = [None] * NCC
        self.cc_rr = 0
        self.bar = set()

    def barrier(self):
        last = {}
        for oid, o in enumerate(self.ops):
            key = o["dma"][:2] if o["dma"] is not None else ("c", o["eng"])
            last[key] = oid
        self.bar = set(last.values())

    def _deps(self, r, w):
        deps = set(self.bar)
        for t in r:
            if t.w is not None:
                deps.add(t.w)
        for t in w:
            if t.w is not None:
                deps.add(t.w)
            deps.update(t.r)
        return deps

    def _mark(self, oid, r, w):
        for t in r:
            t.r.append(oid)
        for t in w:
            t.w = oid
            t.r = []

    def op(self, eng, fn, r=(), w=()):
        deps = self._deps(r, w)
        oid = len(self.ops)
        self.cnt[eng] += 1
        self.ops.append(dict(eng=eng, fn=fn, deps=deps, dma=None, idx=self.cnt[eng]))
        self._mark(oid, r, w)
        return oid

    def dma(self, q, fn, r=(), w=()):
        deps = self._deps(r, w)
        oid = len(self.ops)
        j = self.dma_rr[q]
        self.dma_rr[q] = (j + 1) % N_DMA_SEMS
        if self.dma_last[q][j] is not None:
            deps.add(self.dma_last[q][j])
        self.dma_cnt[q][j] += 1
        self.dma_last[q][j] = oid
        self.ops.append(dict(eng=q, fn=fn, deps=deps, dma=(q, j, 16 * self.dma_cnt[q][j], 16), idx=None))
        self._mark(oid, r, w)
        return oid

    def cc(self, fn, r=(), w=()):
        deps = self._deps(r, w)
        oid = len(self.ops)
        j = self.cc_rr
        self.cc_rr = (j + 1) % NCC
        if self.cc_last[j] is not None:
            deps.add(self.cc_last[j])
        self.cc_cnt[j] += 1
        self.cc_last[j] = oid
        self.ops.append(dict(eng="pool", fn=fn, deps=deps, dma=("cc", j, self.cc_cnt[j], 1), idx=None))
        self._mark(oid, r, w)
        return oid

    def emit(self):
        nc = self.nc
        with contextlib.ExitStack() as st:
            csem = {e: st.enter_context(nc.semaphore("c_" + e)) for e in COMPUTE}
            dsem = {q: [st.enter_context(nc.semaphore("d_%s%d" % (q, j))) for j in range(N_DMA_SEMS)] for q in QUEUES}
            dsem["cc"] = [st.enter_context(nc.semaphore("ccs%d" % j)) for j in range(NCC)]
            block = st.enter_context(nc.Block())
            ops = self.ops

            def target(o):
                if o["dma"] is not None:
                    q, j, v, _ = o["dma"]
                    return dsem[q][j], v, ("d", q, j)
                return csem[o["eng"]], o["idx"], ("c", o["eng"])

            def run(engname, eng):
                seen = {}
                for o in ops:
                    if o["eng"] != engname:
                        continue
                    for d in sorted(o["deps"]):
                        od = ops[d]
                        if od["dma"] is None and od["eng"] == "pe" and engname == "pe" and o["dma"] is None:
                            continue
                        sem, val, key = target(od)
                        if seen.get(key, 0) >= val:
                            continue
                        eng.wait_ge(sem, val)
                        seen[key] = val
                    ins = o["fn"](eng)
                    if o["dma"] is not None:
                        q, j, v, inc = o["dma"]
                        ins.then_inc(dsem[q][j], inc)
                    else:
                        ins.then_inc(csem[engname], 1)
                if engname == "sp":
                    for q in QUEUES:
                        for j in range(N_DMA_SEMS):
                            if self.dma_cnt[q][j]:
                                eng.wait_ge(dsem[q][j], 16 * self.dma_cnt[q][j])

            @block.tensor
            def _(e):
                run("pe", e)

            @block.scalar
            def _(e):
                run("act", e)

            @block.vector
            def _(e):
                run("dve", e)

            @block.gpsimd
            def _(e):
                run("pool", e)

            @block.sync
            def _(e):
                run("sp", e)


def cprime(c):
    return (c % 2) * 8 + c // 2


class K:
    def __init__(self, cfg):
        self.cfg = cfg
        self.nc = bass.Bass("TRN2", target_bir_lowering=False)
        self.P = Prog(self.nc)
        self.st = contextlib.ExitStack()
        self.inputs = {}
        self.uid = 0

    def inp(self, name, shape, dt=F32):
        t = self.nc.dram_tensor(name, list(shape), dt, kind="ExternalInput")
        self.inputs[name] = t
        return t

    def idram(self, name, shape, dt):
        return self.nc.dram_tensor(name, list(shape), dt, kind="Internal")

    def sb(self, name, shape, dt, st=None):
        self.uid += 1
        return (st or self.st).enter_context(self.nc.sbuf_tensor("%s_%d" % (name, self.uid), list(shape), dt))

    def psum(self, name, shape, dt):
        return self.st.enter_context(self.nc.psum_tensor(name, list(shape), dt))

    NPOOL = 4

    def _pool(self, dt):
        if not hasattr(self, "pools"):
            self.pools = {}
            self.pool_rr = {}
        key = "bf16" if dt == BF16 else "f32"
        if key not in self.pools:
            ne = (1 << 20) // (2 if dt == BF16 else 4)
            self.pools[key] = [(self.idram("pc_a_%s%d" % (key, i), [ne], dt), self.idram("pc_b_%s%d" % (key, i), [4 * ne], dt),
                                T(), T()) for i in range(self.NPOOL)]
            self.pool_rr[key] = 0
        i = self.pool_rr[key]
        self.pool_rr[key] = (i + 1) % self.NPOOL
        return self.pools[key][i]

    def gather_full(self, name, shard, rows, cols, dt=F32, rdeps=(), cast=False):
        P = self.P
        odt = BF16 if cast else dt
        full = self.idram(name + "_full", [4 * rows, cols], odt)
        tfull = T()
        rp = max(1, (1 << 20) // (cols * (2 if odt == BF16 else 4)))
        for pi, r0 in enumerate(range(0, rows, rp)):
            r1 = min(rows, r0 + rp)
            n = r1 - r0
            a0, a1, t0, t1 = self._pool(odt)
            s0 = a0.ap()[0:n * cols].rearrange("(r c) -> r c", c=cols)
            s1 = a1.ap()[0:4 * n * cols].rearrange("(r c) -> r c", c=cols)
            P.dma("pool" if cast else "sp", lambda e, r0=r0, r1=r1, s0=s0: e.dma_start(out=s0, in_=shard.ap()[r0:r1, :]),
                  r=list(rdeps), w=[t0])
            P.cc(lambda e, s0=s0, s1=s1: e.collective_compute("AllGather", ALU.bypass, replica_groups=G4, ins=[s0], outs=[s1]),
                 r=[t0], w=[t1])
            for rk in range(4):
                P.dma("sp", lambda e, rk=rk, r0=r0, n=n, s1=s1: e.dma_start(
                    out=full.ap()[rk * rows + r0: rk * rows + r0 + n, :], in_=s1[rk * n:(rk + 1) * n, :]),
                    r=[t1], w=[tfull])
        return full, tfull


def build(cfg):
    k = K(cfg)
    nc, P = k.nc, k.P
    layers = cfg["layers"]
    do_peer = cfg.get("peer", True)
    mixers = cfg.get("mixers", True)
    dbg = cfg.get("dbg", False)

    x_in = k.inp("x_c", [NT, 128, D])
    c_all = k.inp("c_all", [3, D])
    bsel = k.inp("bsel", [128, 2])
    modw = k.inp("modw", [DEPTH, D, 6, 256])
    modb = k.inp("modb", [1, DEPTH * 6 * 256])
    normw = k.inp("normw", [DEPTH * 2, D])
    fnw = k.inp("fnw", [1, D])
    NL = len(layers)
    if do_peer:
        wq_sh = k.inp("peer_wq", [NL, 512, D])
        skT_in = k.inp("skT", [DEPTH, 2, 128, 128])
        u_sh = k.inp("peer_u", [NL, 4096, D])
        v_sh = k.inp("peer_v", [NL, 4096, D])
    out = nc.dram_tensor("out", [8, 128, D], F32, kind="ExternalOutput")

    st = k.st
    with st:
        xres = k.sb("xres", [128, NT, D], F32)
        tx = [T() for _ in range(NT)]
        ident = k.sb("ident", [128, 128], BF16)
        identf = k.sb("identf", [128, 128], F32)
        tid = T()
        eps = k.sb("eps", [128, 1], F32)
        teps = T()
        bs = k.sb("bs", [128, 2], F32)
        tbs = T()
        ps = [k.psum("ps%d" % i, [128, 512], F32) for i in range(8)]
        tps = [T() for _ in range(8)]

        P.op("pool", lambda e: e.memset(ident[:], 0.0), w=[tid])
        P.op("pool", lambda e: e.affine_select(out=ident[:], in_=ident[:], pattern=[[-1, 128]], compare_op=ALU.not_equal,
                                               fill=1.0, base=0, channel_multiplier=1), r=[tid], w=[tid])
        P.op("pool", lambda e: e.memset(identf[:], 0.0), w=[tid])
        P.op("pool", lambda e: e.affine_select(out=identf[:], in_=identf[:], pattern=[[-1, 128]], compare_op=ALU.not_equal,
                                               fill=1.0, base=0, channel_multiplier=1), r=[tid], w=[tid])
        P.op("dve", lambda e: e.memset(eps[:], 1e-6), w=[teps])
        P.dma("sp", lambda e: e.dma_start(out=bs[:], in_=bsel.ap()), w=[tbs])
        for t in range(NT):
            P.dma("sp", lambda e, t=t: e.dma_start(out=xres[:, t, :], in_=x_in.ap()[t]), w=[tx[t]])

        wq_full, u_full, v_full = {}, {}, {}
        if do_peer:
            for li, l in enumerate(layers):
                wq_full[l] = k.gather_full("wq%d" % l, _sub(wq_sh, li), 512, D, cast=True)
                u_full[l] = k.gather_full("u%d" % l, _sub(u_sh, li), 4096, D, cast=True)
                v_full[l] = k.gather_full("v%d" % l, _sub(v_sh, li), 4096, D, cast=True)

        m_in = k.idram("m_in", [3, DEPTH * 6 * 256], F32)
        m_s1 = k.idram("m_s1", [12, DEPTH * 6 * 256], F32)
        m_all = k.idram("m_all", [24, DEPTH * 6 * 256], F32)
        tm_in, tm_s1, tm_all = T(), T(), T()
        MW = DEPTH * 6 * 256
        ph = contextlib.ExitStack()
        cs = k.sb("cs", [48, 128], F32, ph)
        tcs = T()
        sT = k.sb("sT", [128, 48], F32, ph)
        tsT = T()
        mwb = [k.sb("mwb%d" % i, [128, 16, 256], F32, ph) for i in range(2)]
        tmwb = [T(), T()]
        mbb = k.sb("mbb", [3, MW], F32, ph)
        tmbb = T()
        msb = k.sb("msb", [3, MW], F32, ph)
        tmsb = T()
        for r in range(3):
            P.dma("sp", lambda e, r=r: e.dma_start(out=cs[r * 16:(r + 1) * 16, :],
                                                   in_=c_all.ap()[r].rearrange("(c p) -> c p", p=128)), w=[tcs])
        P.op("act", lambda e: e.activation(out=cs[:], in_=cs[:], func=AF.Silu), r=[tcs], w=[tcs])
        P.op("pe", lambda e: e.transpose(ps[0][:, 0:48], cs[:], identf[0:48, 0:48]), r=[tcs, tid], w=[tps[0]])
        P.op("dve", lambda e: e.tensor_copy(out=sT[:], in_=ps[0][:, 0:48]), r=[tps[0]], w=[tsT])
        P.dma("sp", lambda e: e.dma_start(out=mbb[:], in_=modb.ap().partition_broadcast(3)), w=[tmbb])
        sT3 = sT[:].rearrange("p (r c) -> p c r", r=3)
        g = 0
        for l in range(DEPTH):
            for w6 in range(6):
                b = g % 2
                P.dma("sp", lambda e, l=l, w6=w6, b=b: e.dma_start(
                    out=mwb[b][:], in_=modw.ap()[l, :, w6, :].rearrange("(k p) n -> p k n", p=128)), w=[tmwb[b]])
                pp = 1 + (g % 2)
                for kk in range(16):
                    P.op("pe", lambda e, kk=kk, b=b, pp=pp: e.matmul(ps[pp][0:3, 0:256], lhsT=sT3[:, kk, :], rhs=mwb[b][:, kk, :],
                                                                   start=(kk == 0), stop=(kk == 15)),
                         r=[tsT, tmwb[b]], w=[tps[pp]])
                P.op("dve", lambda e, g=g, pp=pp: e.tensor_tensor(out=msb[:, g * 256:(g + 1) * 256], in0=ps[pp][0:3, 0:256],
                                                                  in1=mbb[:, g * 256:(g + 1) * 256], op=ALU.add),
                     r=[tps[pp], tmbb], w=[tmsb])
                g += 1
        P.dma("sp", lambda e: e.dma_start(out=m_in.ap(), in_=msb[:]), r=[tmsb], w=[tm_in])
        P.cc(lambda e: e.collective_compute("AllGather", ALU.bypass, replica_groups=G4, ins=[m_in.ap()], outs=[m_s1.ap()]),
             r=[tm_in], w=[tm_s1])
        P.cc(lambda e: e.collective_compute("AllGather", ALU.bypass, replica_groups=G2, ins=[m_s1.ap()], outs=[m_all.ap()]),
             r=[tm_s1], w=[tm_all])
        ph.close()
        P.barrier()
        hT = k.sb("hT", [128, 16, NT * 128], BF16)
        thT = [T() for _ in range(NT)]

        def mvec_ap(l, row, w6, jh=None, bcast=None):
            off = row * MW + (l * 6 + w6) * 256
            if bcast:
                return bass.AP(m_all, off, [[0, bcast], [3 * MW, 8], [1, 256]])
            return bass.AP(m_all, off + jh * 128, [[3 * MW, 8], [1, 128]])

        vrow = k.sb("vrow", [128, 2, 128], F32)
        tvrow = T()
        nrow = k.sb("nrow", [32, 128], F32)
        tnrow = T()
        vT = k.sb("vT", [128, 2, 128], F32)
        tvT = T()
        nT = k.sb("nT", [128, 32], F32)
        tnT = T()
        AB = k.sb("AB", [128, 8, 16], F32)
        tAB = T()

        def layer_vectors(l):
            vi = 0
            for row in (0, 1):
                for w6 in (0, 1, 3, 4):
                    for jh in (0, 1):
                        P.dma("sp", lambda e, row=row, w6=w6, jh=jh, vi=vi: e.dma_start(
                            out=vrow[vi * 16 + jh * 8: vi * 16 + jh * 8 + 8, 0, :], in_=mvec_ap(l, row, w6, jh=jh)),
                            r=[tm_all], w=[tvrow])
                    vi += 1
            vi = 0
            for w6 in (0, 1, 3, 4):
                for jh in (0, 1):
                    P.dma("sp", lambda e, w6=w6, jh=jh, vi=vi: e.dma_start(
                        out=vrow[vi * 16 + jh * 8: vi * 16 + jh * 8 + 8, 1, :], in_=mvec_ap(l, 2, w6, jh=jh)),
                        r=[tm_all], w=[tvrow])
                vi += 1
            for n2 in (0, 1):
                for jh in (0, 1):
                    P.dma("sp", lambda e, n2=n2, jh=jh: e.dma_start(
                        out=nrow[n2 * 16 + jh * 8: n2 * 16 + jh * 8 + 8, :],
                        in_=bass.AP(normw, (l * 2 + n2) * D + jh * 128, [[256, 8], [1, 128]])), w=[tnrow])
            P.op("pe", lambda e: e.transpose(ps[0][:, 0:128], vrow[:, 0, :], identf[:]), r=[tvrow, tid], w=[tps[0]])
            P.op("dve", lambda e: e.tensor_copy(out=vT[:, 0, :], in_=ps[0][:, 0:128]), r=[tps[0]], w=[tvT])
            P.op("pe", lambda e: e.transpose(ps[0][:, 0:64], vrow[0:64, 1, :], identf[0:64, 0:64]), r=[tvrow, tid], w=[tps[0]])
            P.op("dve", lambda e: e.tensor_copy(out=vT[:, 1, 0:64], in_=ps[0][:, 0:64]), r=[tps[0]], w=[tvT])
            P.op("pe", lambda e: e.transpose(ps[0][:, 0:32], nrow[:], identf[0:32, 0:32]), r=[tnrow, tid], w=[tps[0]])
            P.op("dve", lambda e: e.tensor_copy(out=nT[:], in_=ps[0][:, 0:32]), r=[tps[0]], w=[tnT])
            P.op("dve", lambda e: e.tensor_scalar(out=vT[:, 0, 0:64], in0=vT[:, 0, 0:64], scalar1=bs[:, 0:1], scalar2=None,
                                                  op0=ALU.mult), r=[tvT, tbs], w=[tvT])
            P.op("dve", lambda e: e.scalar_tensor_tensor(out=vT[:, 0, 0:64], in0=vT[:, 0, 64:128], scalar=bs[:, 1:2],
                                                         in1=vT[:, 0, 0:64], op0=ALU.mult, op1=ALU.add), r=[tvT, tbs], w=[tvT])
            for n2 in (0, 1):
                for grp in (0, 1):
                    ai = n2 * 4 + grp * 2
                    sh = vT[:, grp, (2 * n2) * 16:(2 * n2) * 16 + 16]
                    sc = vT[:, grp, (2 * n2 + 1) * 16:(2 * n2 + 1) * 16 + 16]
                    P.op("dve", lambda e, ai=ai, sc=sc, n2=n2: e.scalar_tensor_tensor(
                        out=AB[:, ai, :], in0=sc, scalar=1.0, in1=nT[:, n2 * 16:(n2 + 1) * 16], op0=ALU.add, op1=ALU.mult),
                        r=[tvT, tnT], w=[tAB])
                    P.op("dve", lambda e, ai=ai, sh=sh: e.tensor_copy(out=AB[:, ai + 1, :], in_=sh), r=[tvT], w=[tAB])

        def load_gates(l, w6, ph):
            g0 = k.sb("g0", [128, D], F32, ph)
            gl = k.sb("gl", [128, D], F32, ph)
            gc = k.sb("gc", [128, D], F32, ph)
            tg0, tgl, tgc = T(), T(), T()
            P.dma("sp", lambda e: e.dma_start(out=g0[:].rearrange("p (r j) -> p r j", r=8), in_=mvec_ap(l, 0, w6, bcast=128)),
                  r=[tm_all], w=[tg0])
            P.dma("sp", lambda e: e.dma_start(out=gl[:].rearrange("p (r j) -> p r j", r=8), in_=mvec_ap(l, 1, w6, bcast=128)),
                  r=[tm_all], w=[tgl])
            P.dma("sp", lambda e: e.dma_start(out=gc[:].rearrange("p (r j) -> p r j", r=8), in_=mvec_ap(l, 2, w6, bcast=128)),
                  r=[tm_all], w=[tgc])
            P.op("pool", lambda e: e.tensor_scalar(out=gl[:], in0=gl[:], scalar1=bs[:, 1:2], scalar2=None, op0=ALU.mult),
                 r=[tgl, tbs], w=[tgl])
            P.op("dve", lambda e: e.scalar_tensor_tensor(out=gl[:], in0=g0[:], scalar=bs[:, 0:1], in1=gl[:], op0=ALU.mult,
                                                         op1=ALU.add), r=[tg0, tgl, tbs], w=[tgl])
            return gl, tgl, gc, tgc

        xn = [k.sb("xn%d" % i, [128, D], BF16) for i in range(2)]
        txn = [T(), T()]
        ss = k.sb("ss", [128, 2], F32)
        tss = [T(), T()]

        def norm_mod(n2, tiles):
            for i, t in enumerate(tiles):
                b = i % 2
                P.op("act", lambda e, t=t, b=b: e.activation(out=xn[b][:], in_=xres[:, t, :], func=AF.Square,
                                                             accum_out=ss[:, b:b + 1]), r=[tx[t]], w=[txn[b], tss[b]])
                P.op("act", lambda e, b=b: e.activation(out=ss[:, b:b + 1], in_=ss[:, b:b + 1], func=AF.Sqrt, bias=eps[:],
                                                        scale=1.0 / D), r=[tss[b], teps], w=[tss[b]])
                P.op("dve", lambda e, b=b: e.reciprocal(out=ss[:, b:b + 1], in_=ss[:, b:b + 1]), r=[tss[b]], w=[tss[b]])
                P.op("dve", lambda e, t=t, b=b: e.tensor_scalar(out=xn[b][:], in0=xres[:, t, :], scalar1=ss[:, b:b + 1],
                                                                scalar2=None, op0=ALU.mult), r=[tx[t], tss[b]], w=[txn[b]])
                ai = n2 * 4 + (2 if t == 8 else 0)
                for q4 in range(4):
                    pp = 2 + (q4 % 2)
                    pview = ps[pp][:].bitcast(BF16)
                    for j in range(4):
                        c = q4 * 4 + j
                        P.op("pe", lambda e, c=c, j=j, b=b, pview=pview: e.transpose(pview[:, j * 128:(j + 1) * 128],
                                                                                    xn[b][:, c * 128:(c + 1) * 128], ident[:]),
                             r=[txn[b], tid], w=[tps[pp]])
                    for j in range(4):
                        c = q4 * 4 + j
                        P.op("dve", lambda e, c=c, j=j, t=t, ai=ai, pview=pview: e.tensor_scalar(
                            out=hT[:, c, t * 128:(t + 1) * 128], in0=pview[:, j * 128:(j + 1) * 128],
                            scalar1=AB[:, ai, cprime(c):cprime(c) + 1], scalar2=AB[:, ai + 1, cprime(c):cprime(c) + 1],
                            op0=ALU.mult, op1=ALU.add), r=[tps[pp], tAB], w=[thT[t]])

        NTOK = NT * 128
        Gd = k.idram("Gd", [NT, 128, 16384], BF16)
        tGd = [T() for _ in range(NT)]
        EC = 256
        NEC = 16384 // EC
        KB = 8
        NB = 128 // KB
        BE = KB * 128

        def peer(l, tiles):
            wqf, twqf = wq_full[l]
            uf, tuf = u_full[l]
            vf, tvf = v_full[l]
            ph = contextlib.ExitStack()
            qT = k.sb("qT", [128, 16, 128], BF16, ph)
            tqT = T()
            wqb = [k.sb("wqb", [128, 16, 128], BF16, ph) for i in range(2)]
            twqb = [T(), T()]
            skT = k.sb("skT", [128, 2, 128], BF16, ph)
            tskT = T()
            sall = k.sb("sall", [128, 16, 128], F32, ph)
            tsall = T()
            tmpm = k.sb("tmpm", [128, 256], F32, ph)
            ttmpm = T()
            vtop = k.sb("vtop", [128, 16, 16], F32, ph)
            tvtop = T()
            cand = k.sb("cand", [128, 8, 256], F32, ph)
            tcand = T()
            sc = k.sb("sc", [128, 8, 16], F32, ph)
            tsc = T()
            sm = k.sb("sm", [128, 4, 8], F32, ph)
            tsm = T()
            dd = k.sb("dd", [128, 8, 16], F32, ph)
            tdd = T()
            Dq = [k.sb("Dq", [128, BE], F32, ph) for i in range(2)]
            tDq = [T(), T()]
            Eq = [k.sb("Eq", [128, BE], BF16, ph) for i in range(2)]
            tEq = [T(), T()]
            Gq = [k.sb("Gq", [128, BE], BF16, ph) for i in range(2)]
            tGq = [T(), T()]
            Gb = [k.sb("Gb", [128, BE], BF16, ph) for i in range(2)]
            tGb = [T(), T()]
            for h in range(2):
                P.dma("pool", lambda e, h=h: e.dma_start(out=skT[:, h, :], in_=skT_in.ap()[l, h]), w=[tskT])
            for t in tiles:
                for j in range(16):
                    b = j % 2
                    pp = 4 + j % 2
                    P.dma("sp", lambda e, j=j, b=b: e.dma_start(
                        out=wqb[b][:], in_=wqf.ap()[:, j * 128:(j + 1) * 128].rearrange("(k p) n -> p k n", p=128)),
                        r=[twqf], w=[twqb[b]])
                    for kk in range(16):
                        P.op("pe", lambda e, kk=kk, b=b, t=t, pp=pp: e.matmul(
                            ps[pp][:, 0:128], lhsT=wqb[b][:, kk, :], rhs=hT[:, kk, t * 128:(t + 1) * 128],
                            start=(kk == 0), stop=(kk == 15)), r=[twqb[b], thT[t]], w=[tps[pp]])
                    P.op("act", lambda e, j=j, pp=pp: e.copy(out=qT[:, j, :], in_=ps[pp][:, 0:128]), r=[tps[pp]], w=[tqT])
                for q4 in range(4):
                    pp = q4 % 2
                    for jj in range(4):
                        j = q4 * 4 + jj
                        P.op("pe", lambda e, j=j, jj=jj, pp=pp: e.matmul(
                            ps[pp][:, jj * 128:(jj + 1) * 128], lhsT=qT[:, j, :], rhs=skT[:, j % 2, :],
                            start=True, stop=True), r=[tqT, tskT], w=[tps[pp]])
                    P.op("act", lambda e, q4=q4, pp=pp: e.copy(out=sall[:, q4 * 4:(q4 + 1) * 4, :].rearrange("p a b -> p (a b)"),
                                                               in_=ps[pp][:]), r=[tps[pp]], w=[tsall])
                for j in range(16):
                    P.op("dve", lambda e, j=j: e.max(out=vtop[:, j, 0:8], in_=sall[:, j, :]), r=[tsall], w=[tvtop])
                    P.op("dve", lambda e, j=j: e.match_replace(out=tmpm[:, 0:128], in_to_replace=vtop[:, j, 0:8],
                                                               in_values=sall[:, j, :], imm_value=-1e30),
                         r=[tsall, tvtop], w=[ttmpm])
                    P.op("dve", lambda e, j=j: e.max(out=vtop[:, j, 8:16], in_=tmpm[:, 0:128]), r=[ttmpm], w=[tvtop])
                vt4 = vtop[:].rearrange("p (h two) a -> p h two a", two=2)
                P.op("dve", lambda e, vt4=vt4: e.tensor_tensor(
                    out=cand[:].rearrange("p h (a b) -> p h a b", a=16),
                    in0=vt4[:, :, 0, :].unsqueeze(3).to_broadcast([128, 8, 16, 16]),
                    in1=vt4[:, :, 1, :].unsqueeze(2).to_broadcast([128, 8, 16, 16]), op=ALU.add), r=[tvtop], w=[tcand])
                for h in range(8):
                    P.op("dve", lambda e, h=h: e.max(out=sc[:, h, 0:8], in_=cand[:, h, :]), r=[tcand], w=[tsc])
                    P.op("dve", lambda e, h=h: e.match_replace(out=tmpm[:], in_to_replace=sc[:, h, 0:8], in_values=cand[:, h, :],
                                                               imm_value=-1e30), r=[tcand, tsc], w=[ttmpm])
                    P.op("dve", lambda e, h=h: e.max(out=sc[:, h, 8:16], in_=tmpm[:]), r=[ttmpm], w=[tsc])
                P.op("dve", lambda e: e.tensor_scalar(out=sm[:, 0, :], in0=sc[:, :, 15], scalar1=-1.0, scalar2=None, op0=ALU.mult),
                     r=[tsc], w=[tsm])
                P.op("dve", lambda e: e.tensor_tensor(out=dd[:], in0=sc[:], in1=sm[:, 0, :].unsqueeze(2).to_broadcast([128, 8, 16]),
                                                      op=ALU.add), r=[tsc, tsm], w=[tdd])
                P.op("act", lambda e: e.activation(out=dd[:], in_=dd[:], func=AF.Exp), r=[tdd], w=[tdd])
                P.op("dve", lambda e: e.tensor_reduce(out=sm[:, 1, :], in_=dd[:], axis=AX.X, op=ALU.add), r=[tdd], w=[tsm])
                P.op("act", lambda e: e.activation(out=sm[:, 2, :], in_=sm[:, 1, :], func=AF.Ln), r=[tsm], w=[tsm])
                P.op("dve", lambda e: e.tensor_scalar(out=sm[:, 2, :], in0=sm[:, 2, :], scalar1=-1.0, scalar2=None, op0=ALU.mult),
                     r=[tsm], w=[tsm])
                it = 0
                for qq in range(NB):
                    gb = qq % 2
                    for h in range(8):
                        b = it % 2
                        it += 1
                        P.op("dve", lambda e, h=h, qq=qq, b=b: e.scalar_tensor_tensor(
                            out=Dq[b][:].rearrange("p (a c) -> p a c", a=KB),
                            in0=sall[:, 2 * h, qq * KB:(qq + 1) * KB].unsqueeze(2).to_broadcast([128, KB, 128]),
                            scalar=sm[:, 0, h:h + 1],
                            in1=sall[:, 2 * h + 1, :].unsqueeze(1).to_broadcast([128, KB, 128]),
                            op0=ALU.add, op1=ALU.add), r=[tsall, tsm], w=[tDq[b]])
                        P.op("act", lambda e, h=h, b=b: e.activation(out=Eq[b][:], in_=Dq[b][:], func=AF.Exp,
                                                                     bias=sm[:, 2, h:h + 1], scale=1.0),
                             r=[tDq[b], tsm], w=[tEq[b]])
                        if h == 0:
                            P.op("dve", lambda e, b=b, gb=gb: e.scalar_tensor_tensor(
                                out=Gb[gb][:], in0=Dq[b][:], scalar=-1e-5, in1=Eq[b][:], op0=ALU.is_ge, op1=ALU.mult),
                                r=[tDq[b], tEq[b]], w=[tGb[gb]])
                        else:
                            P.op("dve", lambda e, b=b: e.scalar_tensor_tensor(
                                out=Gq[b][:], in0=Dq[b][:], scalar=-1e-5, in1=Eq[b][:], op0=ALU.is_ge, op1=ALU.mult),
                                r=[tDq[b], tEq[b]], w=[tGq[b]])
                            P.op("pool", lambda e, b=b, gb=gb: e.tensor_tensor(out=Gb[gb][:], in0=Gb[gb][:], in1=Gq[b][:],
                                                                             op=ALU.add), r=[tGq[b], tGb[gb]], w=[tGb[gb]])
                    P.dma("sp", lambda e, t=t, qq=qq, gb=gb: e.dma_start(out=Gd.ap()[t, :, qq * BE:(qq + 1) * BE], in_=Gb[gb][:]),
                          r=[tGb[gb]], w=[tGd[t]])
            ph.close()
            P.barrier()
            ph = contextlib.ExitStack()
            usb = k.sb("usb", [128, 2, D], BF16, ph)
            tusb = T()
            vsb = [k.sb("vsb", [128, 2, D], BF16, ph) for i in range(2)]
            tvsb = [T(), T()]
            uT = k.sb("uT", [128, 16, EC], BF16, ph)
            tuT = T()
            gsb = [k.sb("gsb", [128, NT, EC], BF16, ph) for i in range(2)]
            tgsb = [T(), T()]
            asb = [k.sb("asb", [128, EC], BF16, ph) for i in range(2)]
            tasb = [T(), T()]
            wsb = [k.sb("wsb", [128, EC], BF16, ph) for i in range(2)]
            twsb = [T(), T()]
            wT = [k.sb("wT", [128, 2, 128], BF16, ph) for i in range(2)]
            twT = [T(), T()]
            acc = [k.sb("acc", [128, 512], F32, ph) for i in range(2)]
            tacc = [T(), T()]
            gl, tgl, gc, tgc = load_gates(l, 5, ph)
            ia = 0
            for ec in range(NEC):
                b = ec % 2
                e0 = ec * EC
                P.dma("sp", lambda e, e0=e0: e.dma_start(
                    out=usb[:], in_=uf.ap()[e0:e0 + EC, :].rearrange("(s p) f -> p s f", p=128)), r=[tuf], w=[tusb])
                P.dma("sp", lambda e, b=b, e0=e0: e.dma_start(
                    out=vsb[b][:], in_=vf.ap()[e0:e0 + EC, :].rearrange("(s p) f -> p s f", p=128)), r=[tvf], w=[tvsb[b]])
                P.dma("sp", lambda e, b=b, e0=e0: e.dma_start(
                    out=gsb[b][:], in_=Gd.ap()[:, :, e0:e0 + EC].rearrange("t p e -> p t e")), r=tGd, w=[tgsb[b]])
                it = 0
                for s in range(2):
                    for k4 in range(4):
                        pp = it % 2
                        it += 1
                        pview = ps[pp][:].bitcast(BF16)
                        for jj in range(4):
                            kk = k4 * 4 + jj
                            P.op("pe", lambda e, s=s, kk=kk, jj=jj, pview=pview: e.transpose(
                                pview[:, jj * 128:(jj + 1) * 128], usb[:, s, kk * 128:(kk + 1) * 128], ident[:]),
                                r=[tusb, tid], w=[tps[pp]])
                        P.op("act", lambda e, s=s, k4=k4, pview=pview: e.copy(
                            out=uT[:, k4 * 4:(k4 + 1) * 4, s * 128:(s + 1) * 128],
                            in_=pview[:, 0:512].rearrange("p (j n) -> p j n", j=4)), r=[tps[pp]], w=[tuT])
                def act_mm(i, b=b):
                    t = tiles[i]
                    pa = 2 + i % 2
                    for kk in range(16):
                        P.op("pe", lambda e, kk=kk, t=t, pa=pa: e.matmul(ps[pa][:, 0:EC], lhsT=hT[:, kk, t * 128:(t + 1) * 128],
                                                                        rhs=uT[:, kk, :], start=(kk == 0), stop=(kk == 15)),
                             r=[thT[t], tuT], w=[tps[pa]])
                act_mm(0)
                for i, t in enumerate(tiles):
                    b2 = i % 2
                    pa = 2 + i % 2
                    P.op("act", lambda e, b2=b2, pa=pa: e.activation(out=asb[b2][:], in_=ps[pa][:, 0:EC], func=GELU),
                         r=[tps[pa]], w=[tasb[b2]])
                    P.op("pool", lambda e, b2=b2, b=b, t=t: e.tensor_tensor(out=wsb[b2][:], in0=asb[b2][:], in1=gsb[b][:, t, :],
                                                                          op=ALU.mult), r=[tasb[b2], tgsb[b]], w=[twsb[b2]])
                    if i + 1 < len(tiles):
                        act_mm(i + 1)
                    pw = i % 2
                    pview = ps[pw][:].bitcast(BF16)
                    for s in range(2):
                        P.op("pe", lambda e, s=s, b2=b2, pview=pview, pw=pw: e.transpose(pview[:, s * 128:(s + 1) * 128],
                                                                                       wsb[b2][:, s * 128:(s + 1) * 128], ident[:]),
                             r=[twsb[b2], tid], w=[tps[pw]])
                    P.op("act", lambda e, b2=b2, pview=pview, pw=pw: e.copy(out=wT[b2][:].rearrange("p s n -> p (s n)"),
                                                                          in_=pview[:, 0:256]), r=[tps[pw]], w=[twT[b2]])
                    gt, tg = (gc, tgc) if t == 8 else (gl, tgl)
                    for fc in range(4):
                        for s in range(2):
                            P.op("pe", lambda e, fc=fc, s=s, b=b, b2=b2: e.matmul(
                                ps[4 + fc][:], lhsT=wT[b2][:, s, :], rhs=vsb[b][:, s, fc * 512:(fc + 1) * 512],
                                start=(s == 0), stop=(s == 1)), r=[twT[b2], tvsb[b]], w=[tps[4 + fc]])
                    for fc in range(4):
                        a2 = ia % 2
                        ia += 1
                        P.op("dve", lambda e, fc=fc, a2=a2, gt=gt: e.tensor_tensor(
                            out=acc[a2][:], in0=ps[4 + fc][:], in1=gt[:, fc * 512:(fc + 1) * 512], op=ALU.mult),
                            r=[tps[4 + fc], tg], w=[tacc[a2]])
                        P.op("pool", lambda e, t=t, fc=fc, a2=a2: e.tensor_tensor(
                            out=xres[:, t, fc * 512:(fc + 1) * 512], in0=xres[:, t, fc * 512:(fc + 1) * 512], in1=acc[a2][:],
                            op=ALU.add), r=[tacc[a2], tx[t]], w=[tx[t]])
            ph.close()
            P.barrier()

        def proj(Wf, tWf, col0, ncols, tiles, consume, ph, bw=256, nk=16, krow0=0, src=None, tsrc=None, kofs=0):
            src = hT if src is None else src
            tsrc = thT if tsrc is None else tsrc
            wb = [k.sb("wblk", [128, nk, bw], BF16, ph) for i in range(2)]
            twb = [T(), T()]
            it = 0
            for bi, c0 in enumerate(range(col0, col0 + ncols, bw)):
                b = bi % 2
                P.dma("sp" if Wf.ap().dtype == BF16 else "pool", lambda e, b=b, c0=c0: e.dma_start(
                    out=wb[b][:], in_=Wf.ap()[krow0:krow0 + nk * 128, c0:c0 + bw].rearrange("(k p) n -> p k n", p=128)),
                    r=[tWf], w=[twb[b]])
                for t in tiles:
                    pp = 6 + it % 2
                    it += 1
                    for kk in range(nk):
                        P.op("pe", lambda e, kk=kk, b=b, t=t, pp=pp: e.matmul(
                            ps[pp][:, 0:bw], lhsT=src[:, kofs + kk, t * 128:(t + 1) * 128], rhs=wb[b][:, kk, :],
                            start=(kk == 0), stop=(kk == nk - 1)), r=[tsrc[t], twb[b]], w=[tps[pp]])
                    consume(t, c0 // bw, ps[pp][:, 0:bw], tps[pp])

        def out_proj_residual(l, Wf, tWf, tiles, ph, gates):
            gl, tgl, gc, tgc = gates
            tmp = [k.sb("optmp", [128, 256], F32, ph) for i in range(2)]
            ttmp = [T(), T()]
            cnt = [0]

            def consume(t, bi, pap, tpp):
                a2 = cnt[0] % 2
                cnt[0] += 1
                gt, tg = (gc, tgc) if t == 8 else (gl, tgl)
                c0 = bi * 256
                P.op("dve", lambda e: e.tensor_tensor(out=tmp[a2][:], in0=pap, in1=gt[:, c0:c0 + 256], op=ALU.mult),
                     r=[tpp, tg], w=[ttmp[a2]])
                P.op("pool", lambda e: e.tensor_tensor(out=xres[:, t, c0:c0 + 256], in0=xres[:, t, c0:c0 + 256], in1=tmp[a2][:],
                                                       op=ALU.add), r=[ttmp[a2], tx[t]], w=[tx[t]])
            proj(Wf, tWf, 0, D, tiles, consume, ph)

        if mixers and any(l % 3 == 0 for l in layers):
            nA = sorted(set(l // 3 for l in layers if l % 3 == 0))
            awqkv_sh = k.inp("a_wqkv", [len(nA), 512, 3072])
            awo_sh = k.inp("a_wo", [len(nA), 512, D])
            again = k.inp("a_gain", [2, 2, 128])
            rope_in = k.inp("rope", [8, 128, 256])
            awqkv_full, awo_full = {}, {}
            for ji, j in enumerate(nA):
                awqkv_full[j] = k.gather_full("awqkv%d" % j, _sub(awqkv_sh, ji), 512, 3072)
                awo_full[j] = k.gather_full("awo%d" % j, _sub(awo_sh, ji), 512, D, cast=True)
            kvl_in = k.idram("kvl_in", [1024, 1024], BF16)
            kvc_in = k.idram("kvc_in", [64, 1024], BF16)
        SCALE = 128 ** -0.5

        def mixer_a(l, tiles):
            j = l // 3
            last = (l == DEPTH - 1)
            Wf, tWf = awqkv_full[j]
            ph0 = contextlib.ExitStack()
            qT = k.sb("qTa", [128, 16, NT * 128], BF16, ph0)
            tqT = [T() for _ in range(NT)]
            ph = contextlib.ExitStack()
            gq = k.sb("gq", [128, 2, 128], F32, ph)
            tgq = T()
            for i2 in range(2):
                P.dma("sp", lambda e, i2=i2: e.dma_start(out=gq[:, i2, :], in_=again.ap()[j, i2:i2 + 1, :].partition_broadcast(128)),
                      w=[tgq])
            rp_ = [k.sb("ropeb", [128, 256], F32, ph) for i in range(2)]
            trp = [T(), T()]
            sq = [k.sb("sq", [128, 256], F32, ph) for i in range(2)]
            tsq = [T(), T()]
            s2 = [k.sb("s2", [128, 2], F32, ph) for i in range(2)]
            ts2 = [T(), T()]
            qn = [k.sb("qn", [128, 256], F32, ph) for i in range(2)]
            tqn = [T(), T()]
            qc = [k.sb("qc", [128, 256], F32, ph) for i in range(2)]
            tqc = [T(), T()]
            qf = [k.sb("qf", [128, 256], BF16, ph) for i in range(2)]
            tqf = [T(), T()]
            tkvl, tkvc = T(), T()
            cnt = [0]
            ropetile = [None, None]

            def consume(t, bi, pap, tpp):
                b = cnt[0] % 2
                cnt[0] += 1
                c0 = bi * 256
                isq, isk, isv = c0 < 2048, 2048 <= c0 < 2560, c0 >= 2560
                if isv:
                    P.op("act", lambda e: e.copy(out=qf[b][:], in_=pap), r=[tpp], w=[tqf[b]])
                else:
                    gi = 0 if isq else 1
                    P.op("act", lambda e: e.activation(out=sq[b][:], in_=pap, func=AF.Square), r=[tpp], w=[tsq[b]])
                    P.op("dve", lambda e: e.tensor_reduce(out=s2[b][:], in_=sq[b][:].rearrange("p (h d) -> p h d", h=2), axis=AX.X,
                                                          op=ALU.add), r=[tsq[b]], w=[ts2[b]])
                    P.op("act", lambda e: e.activation(out=s2[b][:], in_=s2[b][:], func=AF.Sqrt, bias=eps[:], scale=1.0 / 128),
                         r=[ts2[b], teps], w=[ts2[b]])
                    P.op("dve", lambda e: e.reciprocal(out=s2[b][:], in_=s2[b][:]), r=[ts2[b]], w=[ts2[b]])
                    P.op("dve", lambda e: e.tensor_tensor(out=qn[b][:].rearrange("p (h d) -> p h d", h=2),
                                                          in0=pap.rearrange("p (h d) -> p h d", h=2),
                                                          in1=s2[b][:].unsqueeze(2).to_broadcast([128, 2, 128]), op=ALU.mult),
                         r=[tpp, ts2[b]], w=[tqn[b]])
                    if t == 8:
                        P.op("pool", lambda e: e.tensor_tensor(out=qf[b][:].rearrange("p (h d) -> p h d", h=2),
                                                               in0=qn[b][:].rearrange("p (h d) -> p h d", h=2),
                                                               in1=gq[:, gi, :].unsqueeze(1).to_broadcast([128, 2, 128]), op=ALU.mult),
                             r=[tqn[b], tgq], w=[tqf[b]])
                    else:
                        P.op("pool", lambda e: e.tensor_tensor(out=qn[b][:].rearrange("p (h d) -> p h d", h=2),
                                                               in0=qn[b][:].rearrange("p (h d) -> p h d", h=2),
                                                               in1=gq[:, gi, :].unsqueeze(1).to_broadcast([128, 2, 128]), op=ALU.mult),
                             r=[tqn[b], tgq], w=[tqn[b]])
                        rb = t % 2
                        if ropetile[rb] != t:
                            ropetile[rb] = t
                            P.dma("sp", lambda e: e.dma_start(out=rp_[rb][:], in_=rope_in.ap()[t]), w=[trp[rb]])
                        cosv = rp_[rb][:, 0:128]
                        sinv = rp_[rb][:, 128:256].rearrange("p (a b f) -> p a b f", a=2, b=2)
                        q5 = qn[b][:].rearrange("p (h a b f) -> p h a b f", h=2, a=2, b=2)
                        c5 = qc[b][:].rearrange("p (h a b f) -> p h a b f", h=2, a=2, b=2)
                        for hh in range(2):
                            P.op("dve", lambda e, hh=hh: e.tensor_tensor(
                                out=c5[:, :, :, hh, :], in0=q5[:, :, :, 1 - hh, :],
                                in1=sinv[:, :, hh, :].unsqueeze(1).to_broadcast([128, 2, 2, 32]), op=ALU.mult),
                                r=[tqn[b], trp[rb]], w=[tqc[b]])
                        P.op("pool", lambda e: e.tensor_tensor(out=qn[b][:].rearrange("p (h d) -> p h d", h=2),
                                                               in0=qn[b][:].rearrange("p (h d) -> p h d", h=2),
                                                               in1=cosv.unsqueeze(1).to_broadcast([128, 2, 128]), op=ALU.mult),
                             r=[tqn[b], trp[rb]], w=[tqn[b]])
                        P.op("pool", lambda e: e.tensor_tensor(out=qf[b][:], in0=qn[b][:], in1=qc[b][:], op=ALU.add),
                             r=[tqn[b], tqc[b]], w=[tqf[b]])
                if isq:
                    pview = ps[2 + bi % 2][:].bitcast(BF16)
                    tp_ = tps[2 + bi % 2]
                    for hh in range(2):
                        P.op("pe", lambda e, hh=hh: e.transpose(pview[:, hh * 128:(hh + 1) * 128], qf[b][:, hh * 128:(hh + 1) * 128],
                                                                ident[:]), r=[tqf[b], tid], w=[tp_])
                    P.op("act", lambda e: e.copy(out=qT[:, 2 * bi:2 * bi + 2, t * 128:(t + 1) * 128],
                                                 in_=pview[:, 0:256].rearrange("p (h n) -> p h n", h=2)), r=[tp_], w=[tqT[t]])
                else:
                    cc0 = c0 - 2048
                    if t == 8:
                        P.dma("sp", lambda e: e.dma_start(out=kvc_in.ap()[:, cc0:cc0 + 256], in_=qf[b][0:64, :]),
                              r=[tqf[b]], w=[tkvc])
                    else:
                        P.dma("sp", lambda e: e.dma_start(out=kvl_in.ap()[t * 128:(t + 1) * 128, cc0:cc0 + 256], in_=qf[b][:]),
                              r=[tqf[b]], w=[tkvl])

            qtiles = tiles
            proj(Wf, tWf, 0, 2048, qtiles, consume, ph)
            proj(Wf, tWf, 2048, 1024, list(range(NT)), consume, ph)
            ph.close()
            P.barrier()
            kvl_all, tkvl_all = k.gather_full("kvl%d" % l, _V(kvl_in.ap()), 1024, 1024, BF16, rdeps=[tkvl])
            kvc_all, tkvc_all = k.gather_full("kvc%d" % l, _V(kvc_in.ap()), 64, 1024, BF16, rdeps=[tkvc])
            ph = contextlib.ExitStack()
            NKC = 34
            kT = k.sb("kTa", [128, NKC * 128], BF16, ph)
            tkT = T()
            vv = k.sb("vva", [128, NKC, 129], BF16, ph)
            tvv = T()
            kin = [k.sb("kin", [128, 4, 128], BF16, ph) for i in range(2)]
            tkin = [T(), T()]
            pT = [k.sb("pTa", [128, 512], BF16, ph) for i in range(3)]
            tpT = [T(), T(), T()]
            rs = k.sb("rsa", [128, 4], F32, ph)
            trs = T()
            otok = [k.sb("otok", [128, 512], BF16, ph) for i in range(2)]
            totok = [T(), T()]
            P.op("pool", lambda e: e.memset(vv[:, :, 128:129], 1.0), w=[tvv])

            def keyrows(c4):
                if c4 * 4 * 128 < 256:
                    return kvc_all, tkvc_all, c4 * 512
                return kvl_all, tkvl_all, c4 * 512 - 256
            ipc = [0]

            def do_group(g):
                ip = ipc[0]
                srcs = [(kvc_all, tkvc_all, 0, 2)] + [(kvl_all, tkvl_all, r0, 4) for r0 in range(0, 4096, 512)]
                kc = 0
                for (srcT, tsrcT, r0, nch) in srcs:
                    b = ip % 2
                    ip += 1
                    P.dma("sp", lambda e, b=b, srcT=srcT, r0=r0, nch=nch: e.dma_start(
                        out=kin[b][:, 0:nch, :],
                        in_=srcT.ap()[r0:r0 + nch * 128, g * 128:(g + 1) * 128].rearrange("(c p) d -> p c d", p=128)),
                        r=[tsrcT], w=[tkin[b]])
                    pview = ps[b][:].bitcast(BF16)
                    for cc in range(nch):
                        P.op("pe", lambda e, b=b, cc=cc, pview=pview: e.transpose(pview[:, cc * 128:(cc + 1) * 128], kin[b][:, cc, :],
                                                                                ident[:]), r=[tkin[b], tid], w=[tps[b]])
                    P.op("act", lambda e, kc=kc, nch=nch, pview=pview: e.copy(out=kT[:, kc * 128:(kc + nch) * 128],
                                                                            in_=pview[:, 0:nch * 128]), r=[tps[b]], w=[tkT])
                    P.dma("sp", lambda e, srcT=srcT, r0=r0, nch=nch, kc=kc: e.dma_start(
                        out=vv[:, kc:kc + nch, 0:128],
                        in_=srcT.ap()[r0:r0 + nch * 128, 512 + g * 128:512 + (g + 1) * 128].rearrange("(c p) d -> p c d", p=128)),
                        r=[tsrcT], w=[tvv])
                    kc += nch
                for ti, t in enumerate(tiles):
                    chunks = [0, 1] if t == 8 else list(range(NKC))
                    def s_mm(ci, t=t, chunks=chunks):
                        kc = chunks[ci]
                        sp_ = ci % 2
                        P.op("pe", lambda e, kc=kc, t=t, sp_=sp_: e.matmul(
                            ps[sp_][:], lhsT=kT[:, kc * 128:(kc + 1) * 128], rhs=qT[:, 4 * g:4 * g + 4, t * 128:(t + 1) * 128],
                            start=True, stop=True), r=[tkT, tqT[t]], w=[tps[sp_]])
                    s_mm(0)
                    for ci, kc in enumerate(chunks):
                        sp_ = ci % 2
                        pb = ci % 3
                        if ci + 1 < len(chunks):
                            s_mm(ci + 1)
                        P.op("act", lambda e, sp_=sp_, pb=pb: e.activation(out=pT[pb][:], in_=ps[sp_][:], func=AF.Exp, scale=SCALE),
                             r=[tps[sp_]], w=[tpT[pb]])
                        for hh in range(4):
                            po = ps[2 + hh // 2]
                            off = (hh % 2) * 129
                            P.op("pe", lambda e, hh=hh, kc=kc, pb=pb, po=po, off=off, ci=ci, n=len(chunks): e.matmul(
                                po[:, off:off + 129], lhsT=pT[pb][:, hh * 128:(hh + 1) * 128], rhs=vv[:, kc, :],
                                start=(ci == 0), stop=(ci == n - 1)), r=[tpT[pb], tvv], w=[tps[2 + hh // 2]])
                    ob = ti % 2
                    for hh in range(4):
                        po = ps[2 + hh // 2]
                        off = (hh % 2) * 129
                        P.op("dve", lambda e, hh=hh, po=po, off=off: e.reciprocal(out=rs[:, hh:hh + 1], in_=po[:, off + 128:off + 129]),
                             r=[tps[2 + hh // 2]], w=[trs])
                        P.op("dve", lambda e, hh=hh, po=po, off=off, ob=ob: e.tensor_scalar(
                            out=otok[ob][:, hh * 128:(hh + 1) * 128], in0=po[:, off:off + 128], scalar1=rs[:, hh:hh + 1],
                            scalar2=None, op0=ALU.mult), r=[tps[2 + hh // 2], trs], w=[totok[ob]])
                    pview = ps[4 + ti % 2][:].bitcast(BF16)
                    tp_ = tps[4 + ti % 2]
                    for hh in range(4):
                        P.op("pe", lambda e, hh=hh, ob=ob, pview=pview: e.transpose(pview[:, hh * 128:(hh + 1) * 128],
                                                                                  otok[ob][:, hh * 128:(hh + 1) * 128], ident[:]),
                             r=[totok[ob], tid], w=[tp_])
                    P.op("act", lambda e, t=t, pview=pview: e.copy(out=hT[:, 4 * g:4 * g + 4, t * 128:(t + 1) * 128],
                                                                   in_=pview[:, 0:512].rearrange("p (h n) -> p h n", h=4)),
                         r=[tp_], w=[thT[t]])
                ipc[0] = ip
            for g in range(4):
                do_group(g)
            ph.close()
            ph0.close()
            P.barrier()
            ph = contextlib.ExitStack()
            gates = load_gates(l, 2, ph)
            out_proj_residual(l, awo_full[j][0], awo_full[j][1], tiles, ph, gates)
            ph.close()
            P.barrier()

        if mixers and any(l % 3 == 2 for l in layers):
            poolw_sh = k.inp("pool_w", [512, 512])
            pools_in = k.inp("pool_scale", [1, D])
            poolM_in = k.inp("poolM", [NT, 128, 12, 128])
            poolH_in = k.inp("poolH", [NT, 64, 4, 128])
            sel_in = k.inp("poolSel", [256, 64])
            poolw_full = k.gather_full("poolw", poolw_sh, 512, 512)
            xe_in = k.idram("xe_in", [64, D], BF16)

        def mixer_c(l, tiles):
            ph = contextlib.ExitStack()
            xna = k.sb("xna", [128, NT, D], BF16, ph)
            txna = [T() for _ in range(NT)]
            txe = T()
            for i, t in enumerate(range(NT)):
                b = i % 2
                P.op("act", lambda e, t=t, b=b: e.activation(out=xna[:, t, :], in_=xres[:, t, :], func=AF.Square,
                                                             accum_out=ss[:, b:b + 1]), r=[tx[t]], w=[txna[t], tss[b]])
                P.op("act", lambda e, b=b: e.activation(out=ss[:, b:b + 1], in_=ss[:, b:b + 1], func=AF.Sqrt, bias=eps[:],
                                                        scale=1.0 / D), r=[tss[b], teps], w=[tss[b]])
                P.op("dve", lambda e, b=b: e.reciprocal(out=ss[:, b:b + 1], in_=ss[:, b:b + 1]), r=[tss[b]], w=[tss[b]])
                P.op("dve", lambda e, t=t, b=b: e.tensor_scalar(out=xna[:, t, :], in0=xres[:, t, :], scalar1=ss[:, b:b + 1],
                                                                scalar2=None, op0=ALU.mult), r=[tx[t], tss[b]], w=[txna[t]])
            for (r0, p0, t) in ((0, 0, 0), (16, 112, 7), (32, 0, 8), (48, 48, 8)):
                P.dma("sp", lambda e, r0=r0, p0=p0, t=t: e.dma_start(out=xe_in.ap()[r0:r0 + 16, :], in_=xna[p0:p0 + 16, t, :]),
                      r=[txna[t]], w=[txe])
            xe_all, txe_all = k.gather_full("xe%d" % l, _V(xe_in.ap()), 64, D, BF16, rdeps=[txe])
            xes = k.sb("xes", [128, 2, D], BF16, ph)
            txes = T()
            sels = k.sb("sels", [128, 2, 64], BF16, ph)
            tsels = T()
            hal = k.sb("hal", [64, D], BF16, ph)
            thal = T()
            P.dma("sp", lambda e: e.dma_start(out=xes[:], in_=xe_all.ap().rearrange("(c p) f -> p c f", p=128)), r=[txe_all], w=[txes])
            P.dma("pool", lambda e: e.dma_start(out=sels[:], in_=sel_in.ap().rearrange("(c p) f -> p c f", p=128)), w=[tsels])
            for fb in range(4):
                pp = fb % 2
                for c in range(2):
                    P.op("pe", lambda e, fb=fb, c=c, pp=pp: e.matmul(ps[pp][0:64, :], lhsT=sels[:, c, :],
                                                                    rhs=xes[:, c, fb * 512:(fb + 1) * 512], start=(c == 0), stop=(c == 1)),
                         r=[tsels, txes], w=[tps[pp]])
                P.op("act", lambda e, fb=fb, pp=pp: e.copy(out=hal[:, fb * 512:(fb + 1) * 512], in_=ps[pp][0:64, :]), r=[tps[pp]], w=[thal])
            Mt = [k.sb("Mt", [128, 12, 128], BF16, ph) for i in range(2)]
            tMt = [T(), T()]
            Mh = [k.sb("Mh", [64, 4, 128], BF16, ph) for i in range(2)]
            tMh = [T(), T()]
            it = 0
            for i, t in enumerate(tiles):
                b = i % 2
                P.dma("pool", lambda e, t=t, b=b: e.dma_start(out=Mt[b][:], in_=poolM_in.ap()[t]), w=[tMt[b]])
                P.dma("pool", lambda e, t=t, b=b: e.dma_start(out=Mh[b][:], in_=poolH_in.ap()[t]), w=[tMh[b]])
                ai = 2 if t == 8 else 0
                for c in range(16):
                    g = c // 4
                    pp = 2 + it % 4
                    it += 1
                    srcs = [(xna[:, t, c * 128:(c + 1) * 128], Mt[b][:, g, :], [txna[t], tMt[b]])]
                    if 1 <= t <= 7:
                        srcs.append((xna[:, t - 1, c * 128:(c + 1) * 128], Mt[b][:, 4 + g, :], [txna[t - 1], tMt[b]]))
                    if t <= 6:
                        srcs.append((xna[:, t + 1, c * 128:(c + 1) * 128], Mt[b][:, 8 + g, :], [txna[t + 1], tMt[b]]))
                    if t in (0, 7):
                        srcs.append((hal[0:32, c * 128:(c + 1) * 128], Mh[b][0:32, g, :], [thal, tMh[b]]))
                    if t == 8:
                        srcs.append((hal[32:64, c * 128:(c + 1) * 128], Mh[b][32:64, g, :], [thal, tMh[b]]))
                    for si, (lh, rh, deps) in enumerate(srcs):
                        P.op("pe", lambda e, lh=lh, rh=rh, pp=pp, si=si, n=len(srcs): e.matmul(
                            ps[pp][:, 0:128], lhsT=lh, rhs=rh, start=(si == 0), stop=(si == n - 1)), r=deps, w=[tps[pp]])
                    P.op("dve", lambda e, c=c, t=t, ai=ai, pp=pp: e.tensor_scalar(
                        out=hT[:, c, t * 128:(t + 1) * 128], in0=ps[pp][:, 0:128], scalar1=AB[:, ai, cprime(c):cprime(c) + 1],
                        scalar2=None, op0=ALU.mult), r=[tps[pp], tAB], w=[thT[t]])
            ph.close()
            P.barrier()
            ph = contextlib.ExitStack()
            gl, tgl, gc, tgc = load_gates(l, 2, ph)
            lsb = k.sb("lsb", [128, D], F32, ph)
            tlsb = T()
            P.dma("sp", lambda e: e.dma_start(out=lsb[:], in_=pools_in.ap().partition_broadcast(128)), w=[tlsb])
            P.op("dve", lambda e: e.tensor_tensor(out=gl[:], in0=gl[:], in1=lsb[:], op=ALU.mult), r=[tgl, tlsb], w=[tgl])
            P.op("pool", lambda e: e.tensor_tensor(out=gc[:], in0=gc[:], in1=lsb[:], op=ALU.mult), r=[tgc, tlsb], w=[tgc])
            tmp = [k.sb("pctmp", [128, 256], F32, ph) for i in range(2)]
            ttmp = [T(), T()]
            cnt = [0]
            for g in range(4):
                def consume(t, bi, pap, tpp, g=g):
                    a2 = cnt[0] % 2
                    cnt[0] += 1
                    gt, tg = (gc, tgc) if t == 8 else (gl, tgl)
                    c0 = g * 512 + bi * 256
                    P.op("dve", lambda e: e.tensor_tensor(out=tmp[a2][:], in0=pap, in1=gt[:, c0:c0 + 256], op=ALU.mult),
                         r=[tpp, tg], w=[ttmp[a2]])
                    P.op("pool", lambda e: e.tensor_tensor(out=xres[:, t, c0:c0 + 256], in0=xres[:, t, c0:c0 + 256], in1=tmp[a2][:],
                                                           op=ALU.add), r=[ttmp[a2], tx[t]], w=[tx[t]])
                proj(poolw_full[0], poolw_full[1], 0, 512, tiles, consume, ph, nk=4, krow0=g * 512, kofs=4 * g)
            ph.close()
            P.barrier()

        if mixers and any(l % 3 == 1 for l in layers):
            bwqkv_sh = k.inp("b_wqkv", [512, 6144])
            bwo_sh = k.inp("b_wo", [512, D])
            biasT_in = k.inp("biasT", [5, 16, 8, 128, 128])
            idxB_in = k.inp("idxB", [128, 15], U32)
            bwqkv_full = k.gather_full("bwqkv", bwqkv_sh, 512, 6144)
            bwo_full = k.gather_full("bwo", bwo_sh, 512, D, cast=True)
            kvbl_in = k.idram("kvbl_in", [1024, 4096], BF16)
            kvbc_in = k.idram("kvbc_in", [64, 4096], BF16)
            win_kv = k.idram("win_kv", [1920, 4096], BF16)
        SLOT = [0, 1, 2, 2, 2, 2, 3, 4]

        def mixer_b(l, tiles):
            Wf, tWf = bwqkv_full
            ph0 = contextlib.ExitStack()
            qT = k.sb("qTb", [128, 16, NT * 128], BF16, ph0)
            tqT = [T() for _ in range(NT)]
            ph = contextlib.ExitStack()
            qf = [k.sb("qfb", [128, 256], BF16, ph) for i in range(2)]
            tqf = [T(), T()]
            tkvl, tkvc = T(), T()
            cnt = [0]

            def consume(t, bi, pap, tpp):
                b = cnt[0] % 2
                cnt[0] += 1
                c0 = bi * 256
                P.op("act", lambda e: e.copy(out=qf[b][:], in_=pap), r=[tpp], w=[tqf[b]])
                if c0 < 2048:
                    pview = ps[2 + bi % 2][:].bitcast(BF16)
                    tp_ = tps[2 + bi % 2]
                    for hh in range(2):
                        P.op("pe", lambda e, hh=hh: e.transpose(pview[:, hh * 128:(hh + 1) * 128], qf[b][:, hh * 128:(hh + 1) * 128],
                                                                ident[:]), r=[tqf[b], tid], w=[tp_])
                    P.op("act", lambda e: e.copy(out=qT[:, 2 * bi:2 * bi + 2, t * 128:(t + 1) * 128],
                                                 in_=pview[:, 0:256].rearrange("p (h n) -> p h n", h=2)), r=[tp_], w=[tqT[t]])
                else:
                    cc0 = c0 - 2048
                    if t == 8:
                        P.dma("sp", lambda e: e.dma_start(out=kvbc_in.ap()[:, cc0:cc0 + 256], in_=qf[b][0:64, :]), r=[tqf[b]], w=[tkvc])
                    else:
                        P.dma("sp", lambda e: e.dma_start(out=kvbl_in.ap()[t * 128:(t + 1) * 128, cc0:cc0 + 256], in_=qf[b][:]),
                              r=[tqf[b]], w=[tkvl])
            proj(Wf, tWf, 0, 6144, list(range(NT)), consume, ph)
            ph.close()
            P.barrier()
            kvl_all, tkvl_all = k.gather_full("kvbl%d" % l, _V(kvbl_in.ap()), 1024, 4096, BF16, rdeps=[tkvl])
            kvc_all, tkvc_all = k.gather_full("kvbc%d" % l, _V(kvbc_in.ap()), 64, 4096, BF16, rdeps=[tkvc])
            ph = contextlib.ExitStack()
            idxs = k.sb("idxs", [128, 15], U32, ph)
            tidx = T()
            wbuf = [k.sb("wbuf", [128, 4096], BF16, ph) for i in range(2)]
            twbuf = [T(), T()]
            twin = T()
            P.dma("sp", lambda e: e.dma_start(out=idxs[:], in_=idxB_in.ap()), w=[tidx])
            for wc in range(15):
                b = wc % 2
                P.dma("pool", lambda e, wc=wc, b=b: e.indirect_dma_start(
                    out=wbuf[b][:], out_offset=None, in_=kvl_all.ap(),
                    in_offset=bass.IndirectOffsetOnAxis(ap=idxs[:, wc:wc + 1], axis=0)), r=[tidx, tkvl_all], w=[twbuf[b]])
                P.dma("sp", lambda e, wc=wc, b=b: e.dma_start(out=win_kv.ap()[wc * 128:(wc + 1) * 128, :], in_=wbuf[b][:]),
                      r=[twbuf[b]], w=[twin])
            ph.close()
            P.barrier()
            ph = contextlib.ExitStack()
            NKC = 17
            kT = k.sb("kTb", [128, NKC * 128], BF16, ph)
            tkT = T()
            vv = k.sb("vvb", [128, NKC, 129], BF16, ph)
            tvv = T()
            kin = [k.sb("kinb", [128, 4, 128], BF16, ph) for i in range(2)]
            tkin = [T(), T()]
            bt = [k.sb("btb", [128, 4, 128], F32, ph) for i in range(2)]
            tbt = [T(), T()]
            sbb = [k.sb("sbb", [128, 512], F32, ph) for i in range(2)]
            tsbb = [T(), T()]
            pT = [k.sb("pTb", [128, 512], BF16, ph) for i in range(3)]
            tpT = [T(), T(), T()]
            rs = k.sb("rsb", [128, 1], F32, ph)
            trs = T()
            otok = [k.sb("otokb", [128, 128], BF16, ph) for i in range(2)]
            totok = [T(), T()]
            P.op("pool", lambda e: e.memset(vv[:, :, 128:129], 1.0), w=[tvv])
            ctr = dict(ip=0, ig=0, it=0)

            def do_head(h):
                srcs = [(win_kv, twin, r0, min(4, 15 - r0 // 128)) for r0 in range(0, 1920, 512)] + [(kvc_all, tkvc_all, 0, 2)]
                kc = 0
                for (srcT, tsrcT, r0, nch) in srcs:
                    b = ctr["ip"] % 2
                    ctr["ip"] += 1
                    P.dma("sp", lambda e, b=b, srcT=srcT, r0=r0, nch=nch: e.dma_start(
                        out=kin[b][:, 0:nch, :],
                        in_=srcT.ap()[r0:r0 + nch * 128, h * 128:(h + 1) * 128].rearrange("(c p) d -> p c d", p=128)),
                        r=[tsrcT], w=[tkin[b]])
                    pview = ps[b][:].bitcast(BF16)
                    for cc in range(nch):
                        P.op("pe", lambda e, b=b, cc=cc, pview=pview: e.transpose(pview[:, cc * 128:(cc + 1) * 128], kin[b][:, cc, :],
                                                                                ident[:]), r=[tkin[b], tid], w=[tps[b]])
                    P.op("act", lambda e, kc=kc, nch=nch, pview=pview: e.copy(out=kT[:, kc * 128:(kc + nch) * 128],
                                                                            in_=pview[:, 0:nch * 128]), r=[tps[b]], w=[tkT])
                    P.dma("sp", lambda e, srcT=srcT, r0=r0, nch=nch, kc=kc: e.dma_start(
                        out=vv[:, kc:kc + nch, 0:128],
                        in_=srcT.ap()[r0:r0 + nch * 128, 2048 + h * 128:2048 + (h + 1) * 128].rearrange("(c p) d -> p c d", p=128)),
                        r=[tsrcT], w=[tvv])
                    kc += nch
                for t in tiles:
                    if t == 8:
                        groups = [([15, 16], None)]
                    else:
                        groups = [([t, t + 1, t + 2, t + 3], 0), ([t + 4, t + 5, t + 6, t + 7], 4), ([15, 16], None)]
                    nall = sum(len(g_[0]) for g_ in groups)
                    done = 0
                    for (chs, boff) in groups:
                        ig = ctr["ig"]
                        ctr["ig"] += 1
                        sp_ = ig % 2
                        pb = ig % 3
                        n = len(chs)
                        for jj, kc in enumerate(chs):
                            P.op("pe", lambda e, jj=jj, kc=kc, t=t, sp_=sp_: e.matmul(
                                ps[sp_][:, jj * 128:(jj + 1) * 128], lhsT=kT[:, kc * 128:(kc + 1) * 128],
                                rhs=qT[:, h, t * 128:(t + 1) * 128], start=True, stop=True), r=[tkT, tqT[t]], w=[tps[sp_]])
                        if boff is not None:
                            bb = ig % 2
                            P.dma("sp", lambda e, t=t, boff=boff, bb=bb: e.dma_start(
                                out=bt[bb][:], in_=biasT_in.ap()[SLOT[t], h, boff:boff + 4].rearrange("c k q -> k c q")), w=[tbt[bb]])
                            P.op("dve", lambda e, sp_=sp_, bb=bb: e.scalar_tensor_tensor(
                                out=sbb[bb][:], in0=ps[sp_][:], scalar=SCALE, in1=bt[bb][:].rearrange("k c q -> k (c q)"),
                                op0=ALU.mult, op1=ALU.add), r=[tps[sp_], tbt[bb]], w=[tsbb[bb]])
                            P.op("act", lambda e, bb=bb, pb=pb: e.activation(out=pT[pb][:], in_=sbb[bb][:], func=AF.Exp),
                                 r=[tsbb[bb]], w=[tpT[pb]])
                        else:
                            P.op("act", lambda e, sp_=sp_, pb=pb, n=n: e.activation(out=pT[pb][:, 0:n * 128], in_=ps[sp_][:, 0:n * 128],
                                                                                  func=AF.Exp, scale=SCALE), r=[tps[sp_]], w=[tpT[pb]])
                        for jj, kc in enumerate(chs):
                            P.op("pe", lambda e, jj=jj, kc=kc, pb=pb, first=(done == 0), lastc=(done == nall - 1): e.matmul(
                                ps[2][:, 0:129], lhsT=pT[pb][:, jj * 128:(jj + 1) * 128], rhs=vv[:, kc, :],
                                start=first, stop=lastc), r=[tpT[pb], tvv], w=[tps[2]])
                            done += 1
                    ob = ctr["it"] % 2
                    ctr["it"] += 1
                    P.op("dve", lambda e: e.reciprocal(out=rs[:], in_=ps[2][:, 128:129]), r=[tps[2]], w=[trs])
                    P.op("dve", lambda e, ob=ob: e.tensor_scalar(out=otok[ob][:], in0=ps[2][:, 0:128], scalar1=rs[:, 0:1], scalar2=None,
                                                                 op0=ALU.mult), r=[tps[2], trs], w=[totok[ob]])
                    pview = ps[4 + ob][:].bitcast(BF16)
                    P.op("pe", lambda e, ob=ob, pview=pview: e.transpose(pview[:, 0:128], otok[ob][:], ident[:]),
                         r=[totok[ob], tid], w=[tps[4 + ob]])
                    P.op("act", lambda e, t=t, ob=ob, pview=pview: e.copy(out=hT[:, h, t * 128:(t + 1) * 128], in_=pview[:, 0:128]),
                         r=[tps[4 + ob]], w=[thT[t]])
            for h in range(16):
                do_head(h)
            ph.close()
            ph0.close()
            P.barrier()
            ph = contextlib.ExitStack()
            gates = load_gates(l, 2, ph)
            out_proj_residual(l, bwo_full[0], bwo_full[1], tiles, ph, gates)
            ph.close()
            P.barrier()

        for l in layers:
            last = (l == DEPTH - 1)
            tiles = list(range(8)) if last else list(range(NT))
            layer_vectors(l)
            if mixers:
                if l % 3 == 0:
                    norm_mod(0, list(range(NT)))
                    mixer_a(l, tiles)
                elif l % 3 == 1:
                    norm_mod(0, list(range(NT)))
                    mixer_b(l, tiles)
                else:
                    mixer_c(l, tiles)
            if do_peer:
                norm_mod(1, tiles)
                peer(l, tiles)

        fw = k.sb("fw", [128, D], F32)
        tfw = T()
        P.dma("sp", lambda e: e.dma_start(out=fw[:], in_=fnw.ap().partition_broadcast(128)), w=[tfw])
        ob = [k.sb("ob%d" % i, [128, D], F32) for i in range(2)]
        tob = [T(), T()]
        for t in range(8):
            b = t % 2
            P.op("act", lambda e, t=t, b=b: e.activation(out=xn[b][:], in_=xres[:, t, :], func=AF.Square, accum_out=ss[:, b:b + 1]),
                 r=[tx[t]], w=[txn[b], tss[b]])
            P.op("act", lambda e, b=b: e.activation(out=ss[:, b:b + 1], in_=ss[:, b:b + 1], func=AF.Sqrt, bias=eps[:], scale=1.0 / D),
                 r=[tss[b], teps], w=[tss[b]])
            P.op("dve", lambda e, b=b: e.reciprocal(out=ss[:, b:b + 1], in_=ss[:, b:b + 1]), r=[tss[b]], w=[tss[b]])
            P.op("dve", lambda e, t=t, b=b: e.scalar_tensor_tensor(out=ob[b][:], in0=xres[:, t, :], scalar=ss[:, b:b + 1], in1=fw[:],
                                                                   op0=ALU.mult, op1=ALU.mult), r=[tx[t], tss[b], tfw], w=[tob[b]])
            P.dma("sp", lambda e, t=t, b=b: e.dma_start(out=out.ap()[t], in_=ob[b][:]), r=[tob[b]])
        P.emit()
    return nc


class _V:
    def __init__(self, a):
        self._a = a

    def ap(self):
        return self._a


def _sub(t, l):
    class V:
        def __init__(self, a):
            self._a = a

        def ap(self):
            return self._a
    return V(t.ap()[l])


def host_inputs(inp, cfg):
    f = lambda a: np.ascontiguousarray(np.asarray(a, dtype=np.float32))
    x, c, ctx, c_ctx = f(inp["x"]), f(inp["c"]), f(inp["ctx"]), f(inp["c_ctx"])
    mod_w, mod_b = f(inp["mod_w"]), f(inp["mod_b"])
    maps = []
    mw5 = mod_w.reshape(DEPTH, D, 6, 8, 256)
    mb4 = mod_b.reshape(DEPTH, 6, 8, 256)
    skT = np.ascontiguousarray(f(inp["peer_sub_keys"]).transpose(0, 1, 3, 2))
    for cid in range(NC):
        b, kq = cid // 4, cid % 4
        xc = np.zeros((NT, 128, D), np.float32)
        xc[:8] = x[b, kq * 1024:(kq + 1) * 1024].reshape(8, 128, D)
        xc[8, :64] = ctx[b, kq * 64:(kq + 1) * 64]
        bsel = np.zeros((128, 2), np.float32)
        bsel[:, b] = 1.0
        m = {
            "x_c": xc,
            "c_all": np.stack([c[0], c[1], c_ctx]),
            "bsel": bsel,
            "modw": np.ascontiguousarray(mw5[:, :, :, cid, :]),
            "modb": np.ascontiguousarray(mb4[:, :, cid, :]).reshape(1, -1),
            "normw": f(inp["norm_w"]).reshape(DEPTH * 2, D),
            "fnw": f(inp["final_norm_w"]).reshape(1, D),
            "peer_wq": np.ascontiguousarray(f(inp["peer_wq"])[cfg["layers"], kq * 512:(kq + 1) * 512]),
            "skT": skT,
            "peer_u": np.ascontiguousarray(inp["peer_u"][cfg["layers"], kq * 4096:(kq + 1) * 4096], dtype=np.float32),
            "peer_v": np.ascontiguousarray(inp["peer_v"][cfg["layers"], kq * 4096:(kq + 1) * 4096], dtype=np.float32),
        }
        lays = cfg["layers"]
        if cfg.get("mixers", True) and any(l % 3 == 0 for l in lays):
            nA = sorted(set(l // 3 for l in lays if l % 3 == 0))
            m["a_wqkv"] = np.ascontiguousarray(f(inp["a_wqkv"])[nA, kq * 512:(kq + 1) * 512])
            m["a_wo"] = np.ascontiguousarray(f(inp["a_wo"])[nA, kq * 512:(kq + 1) * 512])
            m["a_gain"] = np.ascontiguousarray(np.stack([f(inp["a_q_gain"]), f(inp["a_k_gain"])], axis=1))
            m["rope"] = rope_tables(kq)
        if cfg.get("mixers", True) and any(l % 3 == 2 for l in lays):
            m["pool_w"] = np.ascontiguousarray(f(inp["pool_w"])[0, kq])
            m["pool_scale"] = f(inp["pool_scale"]).reshape(1, D)
            pm, phh, sel = pool_consts(kq)
            m["poolM"], m["poolH"], m["poolSel"] = pm, phh, sel
        if cfg.get("mixers", True) and any(l % 3 == 1 for l in lays):
            m["b_wqkv"] = np.ascontiguousarray(f(inp["b_wqkv"])[0, kq * 512:(kq + 1) * 512])
            m["b_wo"] = np.ascontiguousarray(f(inp["b_wo"])[0, kq * 512:(kq + 1) * 512])
            m["biasT"], m["idxB"] = nbr_consts(kq, f(inp["b_rpb"])[0])
        if not cfg.get("peer", True):
            for nm in ("peer_wq", "skT", "peer_u", "peer_v"):
                m.pop(nm)
        maps.append(m)
    return maps


def rope_tables(kq):
    tok = np.arange(kq * 1024, (kq + 1) * 1024)
    row = (tok // 64).astype(np.float32)
    col = (tok % 64).astype(np.float32)
    inv = (10000.0 ** (-np.arange(0, 64, 2, dtype=np.float32) / 64)).astype(np.float32)
    ar = row[:, None] * inv[None, :]
    ac = col[:, None] * inv[None, :]
    cos = np.concatenate([np.cos(ar), np.cos(ar), np.cos(ac), np.cos(ac)], axis=1)
    sin = np.concatenate([-np.sin(ar), np.sin(ar), -np.sin(ac), np.sin(ac)], axis=1)
    return np.ascontiguousarray(np.concatenate([cos, sin], axis=1).astype(np.float32).reshape(8, 128, 256))


CFG = dict(layers=[0, 1, 2, 3], peer=True, mixers=True)


def run(inp, cfg):
    nc = build(cfg)
    maps = host_inputs(inp, cfg)
    res = run_bass_kernel_spmd(nc, maps, core_ids=list(range(NC)))
    outp = np.zeros((2, 4096, D), np.float32)
    for cid in range(NC):
        b, kq = cid // 4, cid % 4
        outp[b, kq * 1024:(kq + 1) * 1024] = res.results[cid]["out"].reshape(1024, D)
    return outp


def kernel(**inputs):
    return run(inputs, CFG)


def pool_consts(kq):
    wins = (2, 4, 8, 16)

    def coef(tau, spos, w, L):
        lo = np.maximum(tau - w // 2, 0)
        hi = np.minimum(tau + w - w // 2, L)
        inwin = (spos[:, None] >= lo[None, :]) & (spos[:, None] < hi[None, :]) & (spos[:, None] >= 0) & (spos[:, None] < L)
        m = inwin / (hi - lo)[None, :].astype(np.float64)
        m = m - (spos[:, None] == tau[None, :])
        return m.astype(np.float32)
    PM = np.zeros((NT, 128, 12, 128), np.float32)
    PH = np.zeros((NT, 64, 4, 128), np.float32)
    for t in range(8):
        base = kq * 1024 + t * 128
        tau = base + np.arange(128)
        for g, w in enumerate(wins):
            PM[t, :, g, :] = coef(tau, base + np.arange(128), w, 4096)
            if t >= 1:
                PM[t, :, 4 + g, :] = coef(tau, base - 128 + np.arange(128), w, 4096)
            if t <= 6:
                PM[t, :, 8 + g, :] = coef(tau, base + 128 + np.arange(128), w, 4096)
            if t == 0:
                PH[t, 0:16, g, :] = coef(tau, base - 16 + np.arange(16), w, 4096)
            if t == 7:
                PH[t, 16:32, g, :] = coef(tau, base + 128 + np.arange(16), w, 4096)
    base = kq * 64
    tau = base + np.arange(128)
    valid = (np.arange(128) < 64)
    for g, w in enumerate(wins):
        m0 = coef(tau, base + np.arange(128), w, 256)
        m0[64:, :] = 0.0
        m0[:, ~valid] = 0.0
        PM[8, :, g, :] = m0
        mp = coef(tau, base - 16 + np.arange(16), w, 256)
        mn = coef(tau, base + 64 + np.arange(16), w, 256)
        mp[:, ~valid] = 0.0
        mn[:, ~valid] = 0.0
        PH[8, 32:48, g, :] = mp
        PH[8, 48:64, g, :] = mn
    sel = np.zeros((256, 64), np.float32)
    for i in range(16):
        if kq > 0:
            sel[(kq - 1) * 64 + 16 + i, i] = 1.0
            sel[(kq - 1) * 64 + 48 + i, 32 + i] = 1.0
        if kq < 3:
            sel[(kq + 1) * 64 + i, 16 + i] = 1.0
            sel[(kq + 1) * 64 + 32 + i, 48 + i] = 1.0
    return PM, PH, sel


def nbr_consts(kq, rpb):
    NEG = -30000.0
    out = np.full((5, 16, 8, 128, 128), NEG, np.float32)
    qr = np.arange(128) // 64
    qc = np.arange(128) % 64
    for slot, t in enumerate((0, 1, 2, 6, 7)):
        R0 = 16 * kq + 2 * t
        r = R0 + qr
        r0 = np.clip(r - 4, 0, 56)
        cs = np.clip(qc - 8, 0, 48)
        for jj in range(8):
            krow = (R0 - 7 + 2 * jj) + np.arange(128) // 64
            kcol = np.arange(128) % 64
            valid = ((krow[:, None] >= r0[None, :]) & (krow[:, None] < r0[None, :] + 8) &
                     (kcol[:, None] >= cs[None, :]) & (kcol[:, None] < cs[None, :] + 16) &
                     (krow[:, None] >= 0) & (krow[:, None] < 64))
            rr = np.clip(krow[:, None] - r[None, :] + 7, 0, 14)
            rc = np.clip(kcol[:, None] - qc[None, :] + 15, 0, 30)
            vals = rpb[:, rr, rc]
            out[slot, :, jj] = np.where(valid[None], vals, NEG)
    n = np.arange(1920)
    grow = np.clip(16 * kq - 7 + n // 64, 0, 63)
    idx = (grow * 64 + n % 64).astype(np.uint32).reshape(15, 128).T
    return out, np.ascontiguousarray(idx)
```

```python
import contextlib
import numpy as np
import concourse.bass as bass
import concourse.mybir as mybir
from concourse.bass_utils import run_bass_kernel_spmd

F32 = mybir.dt.float32
BF16 = mybir.dt.bfloat16
U32 = mybir.dt.uint32
AF = mybir.ActivationFunctionType
ALU = mybir.AluOpType
AX = mybir.AxisListType
GELU = AF.Gelu_apprx_tanh

NC = 8
D = 2048
NT = 9
DEPTH = 4
N_DMA_SEMS = 10
NCC = 4
QUEUES = ("sp", "act", "pool")
COMPUTE = ("pe", "act", "dve", "pool")
G4 = [[0, 1, 2, 3], [4, 5, 6, 7]]
G2 = [[0, 4], [1, 5], [2, 6], [3, 7]]


class T:
    __slots__ = ("w", "r")

    def __init__(self):
        self.w = None
        self.r = []


class Prog:
    def __init__(self, nc):
        self.nc = nc
        self.ops = []
        self.cnt = {e: 0 for e in ("pe", "act", "dve", "pool", "sp")}
        self.dma_cnt = {q: [0] * N_DMA_SEMS for q in QUEUES}
        self.dma_rr = {q: 0 for q in QUEUES}
        self.dma_last = {q: [None] * N_DMA_SEMS for q in QUEUES}
        self.cc_cnt = [0] * NCC
        self.cc_last = [None] * NCC
        self.cc_rr = 0
        self.bar = set()

    def barrier(self):
        last = {}
        for oid, o in enumerate(self.ops):
            key = o["dma"][:2] if o["dma"] is not None else ("c", o["eng"])
            last[key] = oid
        self.bar = set(last.values())

    def _deps(self, r, w):
        deps = set(self.bar)
        for t in r:
            if t.w is not None:
                deps.add(t.w)
        for t in w:
            if t.w is not None:
                deps.add(t.w)
            deps.update(t.r)
        return deps

    def _mark(self, oid, r, w):
        for t in r:
            t.r.append(oid)
        for t in w:
            t.w = oid
            t.r = []

    def op(self, eng, fn, r=(), w=()):
        deps = self._deps(r, w)
        oid = len(self.ops)
        self.cnt[eng] += 1
        self.ops.append(dict(eng=eng, fn=fn, deps=deps, dma=None, idx=self.cnt[eng]))
        self._mark(oid, r, w)
        return oid

    def dma(self, q, fn, r=(), w=()):
        deps = self._deps(r, w)
        oid = len(self.ops)
        j = self.dma_rr[q]
        self.dma_rr[q] = (j + 1) % N_DMA_SEMS
        if self.dma_last[q][j] is not None:
            deps.add(self.dma_last[q][j])
        self.dma_cnt[q][j] += 1
        self.dma_last[q][j] = oid
        self.ops.append(dict(eng=q, fn=fn, deps=deps, dma=(q, j, 16 * self.dma_cnt[q][j], 16), idx=None))
        self._mark(oid, r, w)
        return oid

    def cc(self, fn, r=(), w=()):
        deps = self._deps(r, w)
        oid = len(self.ops)
        j = self.cc_rr
        self.cc_rr = (j + 1) % NCC
        if self.cc_last[j] is not None:
            deps.add(self.cc_last[j])
        self.cc_cnt[j] += 1
        self.cc_last[j] = oid
        self.ops.append(dict(eng="pool", fn=fn, deps=deps, dma=("cc", j, self.cc_cnt[j], 1), idx=None))
        self._mark(oid, r, w)
        return oid

    def emit(self):
        nc = self.nc
        with contextlib.ExitStack() as st:
            csem = {e: st.enter_context(nc.semaphore("c_" + e)) for e in COMPUTE}
            dsem = {q: [st.enter_context(nc.semaphore("d_%s%d" % (q, j))) for j in range(N_DMA_SEMS)] for q in QUEUES}
            dsem["cc"] = [st.enter_context(nc.semaphore("ccs%d" % j)) for j in range(NCC)]
            block = st.enter_context(nc.Block())
            ops = self.ops

            def target(o):
                if o["dma"] is not None:
                    q, j, v, _ = o["dma"]
                    return dsem[q][j], v, ("d", q, j)
                return csem[o["eng"]], o["idx"], ("c", o["eng"])

            def run(engname, eng):
                seen = {}
                for o in ops:
                    if o["eng"] != engname:
                        continue
                    for d in sorted(o["deps"]):
                        od = ops[d]
                        if od["dma"] is None and od["eng"] == "pe" and engname == "pe" and o["dma"] is None:
                            continue
                        sem, val, key = target(od)
                        if seen.get(key, 0) >= val:
                            continue
                        eng.wait_ge(sem, val)
                        seen[key] = val
                    ins = o["fn"](eng)
                    if o["dma"] is not None:
                        q, j, v, inc = o["dma"]
                        ins.then_inc(dsem[q][j], inc)
                    else:
                        ins.then_inc(csem[engname], 1)
                if engname == "sp":
                    for q in QUEUES:
                        for j in range(N_DMA_SEMS):
                            if self.dma_cnt[q][j]:
                                eng.wait_ge(dsem[q][j], 16 * self.dma_cnt[q][j])

            @block.tensor
            def _(e):
                run("pe", e)

            @block.scalar
            def _(e):
                run("act", e)

            @block.vector
            def _(e):
                run("dve", e)

            @block.gpsimd
            def _(e):
                run("pool", e)

            @block.sync
            def _(e):
                run("sp", e)


def cprime(c):
    return (c % 2) * 8 + c // 2


class K:
    def __init__(self, cfg):
        self.cfg = cfg
        self.nc = bass.Bass("TRN2", target_bir_lowering=False)
        self.P = Prog(self.nc)
        self.st = contextlib.ExitStack()
        self.inputs = {}
        self.uid = 0

    def inp(self, name, shape, dt=F32):
        t = self.nc.dram_tensor(name, list(shape), dt, kind="ExternalInput")
        self.inputs[name] = t
        return t

    def idram(self, name, shape, dt):
        return self.nc.dram_tensor(name, list(shape), dt, kind="Internal")

    def sb(self, name, shape, dt, st=None):
        self.uid += 1
        return (st or self.st).enter_context(self.nc.sbuf_tensor("%s_%d" % (name, self.uid), list(shape), dt))

    def psum(self, name, shape, dt):
        return self.st.enter_context(self.nc.psum_tensor(name, list(shape), dt))

    NPOOL = 4

    def _pool(self, dt):
        if not hasattr(self, "pools"):
            self.pools = {}
            self.pool_rr = {}
        key = "bf16" if dt == BF16 else "f32"
        if key not in self.pools:
            ne = (1 << 20) // (2 if dt == BF16 else 4)
            self.pools[key] = [(self.idram("pc_a_%s%d" % (key, i), [ne], dt), self.idram("pc_b_%s%d" % (key, i), [4 * ne], dt),
                                T(), T()) for i in range(self.NPOOL)]
            self.pool_rr[key] = 0
        i = self.pool_rr[key]
        self.pool_rr[key] = (i + 1) % self.NPOOL
        return self.pools[key][i]

    def gather_full(self, name, shard, rows, cols, dt=F32, rdeps=(), cast=False, dq="sp"):
        P = self.P
        odt = BF16 if cast else dt
        full = self.idram(name + "_full", [4 * rows, cols], odt)
        tfull = T()
        rp = max(1, (1 << 20) // (cols * (2 if odt == BF16 else 4)))
        for pi, r0 in enumerate(range(0, rows, rp)):
            r1 = min(rows, r0 + rp)
            n = r1 - r0
            a0, a1, t0, t1 = self._pool(odt)
            s0 = a0.ap()[0:n * cols].rearrange("(r c) -> r c", c=cols)
            s1 = a1.ap()[0:4 * n * cols].rearrange("(r c) -> r c", c=cols)
            P.dma("pool" if cast else "sp", lambda e, r0=r0, r1=r1, s0=s0: e.dma_start(out=s0, in_=shard.ap()[r0:r1, :]),
                  r=list(rdeps), w=[t0])
            P.cc(lambda e, s0=s0, s1=s1: e.collective_compute("AllGather", ALU.bypass, replica_groups=G4, ins=[s0], outs=[s1]),
                 r=[t0], w=[t1])
            for rk in range(4):
                P.dma(dq, lambda e, rk=rk, r0=r0, n=n, s1=s1: e.dma_start(
                    out=full.ap()[rk * rows + r0: rk * rows + r0 + n, :], in_=s1[rk * n:(rk + 1) * n, :]),
                    r=[t1], w=[tfull])
        return full, tfull


def build(cfg):
    k = K(cfg)
    nc, P = k.nc, k.P
    layers = cfg["layers"]
    do_peer = cfg.get("peer", True)
    mixers = cfg.get("mixers", True)
    dbg = cfg.get("dbg", False)

    x_in = k.inp("x_c", [NT, 128, D])
    c_all = k.inp("c_all", [3, D])
    bsel = k.inp("bsel", [128, 2])
    modw = k.inp("modw", [DEPTH, D, 6, 256])
    modb = k.inp("modb", [1, DEPTH * 6 * 256])
    normw = k.inp("normw", [DEPTH * 2, D])
    fnw = k.inp("fnw", [1, D])
    NL = len(layers)
    if do_peer:
        wq_sh = k.inp("peer_wq", [NL, 512, D])
        skT_in = k.inp("skT", [DEPTH, 2, 128, 128])
        u_sh = k.inp("peer_u", [NL, 4096, D])
        v_sh = k.inp("peer_v", [NL, 4096, D])
    out = nc.dram_tensor("out", [8, 128, D], F32, kind="ExternalOutput")

    st = k.st
    with st:
        xres = k.sb("xres", [128, NT, D], F32)
        tx = [T() for _ in range(NT)]
        ident = k.sb("ident", [128, 128], BF16)
        identf = k.sb("identf", [128, 128], F32)
        tid = T()
        eps = k.sb("eps", [128, 1], F32)
        teps = T()
        bs = k.sb("bs", [128, 2], F32)
        tbs = T()
        ps = [k.psum("ps%d" % i, [128, 512], F32) for i in range(8)]
        tps = [T() for _ in range(8)]

        P.op("pool", lambda e: e.memset(ident[:], 0.0), w=[tid])
        P.op("pool", lambda e: e.affine_select(out=ident[:], in_=ident[:], pattern=[[-1, 128]], compare_op=ALU.not_equal,
                                               fill=1.0, base=0, channel_multiplier=1), r=[tid], w=[tid])
        P.op("pool", lambda e: e.memset(identf[:], 0.0), w=[tid])
        P.op("pool", lambda e: e.affine_select(out=identf[:], in_=identf[:], pattern=[[-1, 128]], compare_op=ALU.not_equal,
                                               fill=1.0, base=0, channel_multiplier=1), r=[tid], w=[tid])
        P.op("dve", lambda e: e.memset(eps[:], 1e-6), w=[teps])
        P.dma("sp", lambda e: e.dma_start(out=bs[:], in_=bsel.ap()), w=[tbs])
        for t in range(NT):
            P.dma("sp", lambda e, t=t: e.dma_start(out=xres[:, t, :], in_=x_in.ap()[t]), w=[tx[t]])

        wq_full, u_full, v_full = {}, {}, {}
        if do_peer:
            def gather_peer(li, dq):
                l = layers[li]
                wq_full[l] = k.gather_full("wq%d" % l, _sub(wq_sh, li), 512, D, cast=True, dq=dq)
                u_full[l] = k.gather_full("u%d" % l, _sub(u_sh, li), 4096, D, cast=True, dq=dq)
                v_full[l] = k.gather_full("v%d" % l, _sub(v_sh, li), 4096, D, cast=True, dq=dq)
            gather_peer(0, "sp")

        m_in = k.idram("m_in", [3, DEPTH * 6 * 256], F32)
        m_s1 = k.idram("m_s1", [12, DEPTH * 6 * 256], F32)
        m_all = k.idram("m_all", [24, DEPTH * 6 * 256], F32)
        tm_in, tm_s1, tm_all = T(), T(), T()
        MW = DEPTH * 6 * 256
        ph = contextlib.ExitStack()
        cs = k.sb("cs", [48, 128], F32, ph)
        tcs = T()
        sT = k.sb("sT", [128, 48], F32, ph)
        tsT = T()
        mwb = [k.sb("mwb%d" % i, [128, 16, 256], F32, ph) for i in range(2)]
        tmwb = [T(), T()]
        mbb = k.sb("mbb", [3, MW], F32, ph)
        tmbb = T()
        msb = k.sb("msb", [3, MW], F32, ph)
        tmsb = T()
        for r in range(3):
            P.dma("sp", lambda e, r=r: e.dma_start(out=cs[r * 16:(r + 1) * 16, :],
                                                   in_=c_all.ap()[r].rearrange("(c p) -> c p", p=128)), w=[tcs])
        P.op("act", lambda e: e.activation(out=cs[:], in_=cs[:], func=AF.Silu), r=[tcs], w=[tcs])
        P.op("pe", lambda e: e.transpose(ps[0][:, 0:48], cs[:], identf[0:48, 0:48]), r=[tcs, tid], w=[tps[0]])
        P.op("dve", lambda e: e.tensor_copy(out=sT[:], in_=ps[0][:, 0:48]), r=[tps[0]], w=[tsT])
        P.dma("sp", lambda e: e.dma_start(out=mbb[:], in_=modb.ap().partition_broadcast(3)), w=[tmbb])
        sT3 = sT[:].rearrange("p (r c) -> p c r", r=3)
        g = 0
        for l in range(DEPTH):
            for w6 in range(6):
                b = g % 2
                P.dma("sp", lambda e, l=l, w6=w6, b=b: e.dma_start(
                    out=mwb[b][:], in_=modw.ap()[l, :, w6, :].rearrange("(k p) n -> p k n", p=128)), w=[tmwb[b]])
                pp = 1 + (g % 2)
                for kk in range(16):
                    P.op("pe", lambda e, kk=kk, b=b, pp=pp: e.matmul(ps[pp][0:3, 0:256], lhsT=sT3[:, kk, :], rhs=mwb[b][:, kk, :],
                                                                   start=(kk == 0), stop=(kk == 15)),
                         r=[tsT, tmwb[b]], w=[tps[pp]])
                P.op("dve", lambda e, g=g, pp=pp: e.tensor_tensor(out=msb[:, g * 256:(g + 1) * 256], in0=ps[pp][0:3, 0:256],
                                                                  in1=mbb[:, g * 256:(g + 1) * 256], op=ALU.add),
                     r=[tps[pp], tmbb], w=[tmsb])
                g += 1
        P.dma("sp", lambda e: e.dma_start(out=m_in.ap(), in_=msb[:]), r=[tmsb], w=[tm_in])
        P.cc(lambda e: e.collective_compute("AllGather", ALU.bypass, replica_groups=G4, ins=[m_in.ap()], outs=[m_s1.ap()]),
             r=[tm_in], w=[tm_s1])
        P.cc(lambda e: e.collective_compute("AllGather", ALU.bypass, replica_groups=G2, ins=[m_s1.ap()], outs=[m_all.ap()]),
             r=[tm_s1], w=[tm_all])
        ph.close()
        P.barrier()
        hT = k.sb("hT", [128, 16, NT * 128], BF16)
        thT = [T() for _ in range(NT)]

        def mvec_ap(l, row, w6, jh=None, bcast=None):
            off = row * MW + (l * 6 + w6) * 256
            if bcast:
                return bass.AP(m_all, off, [[0, bcast], [3 * MW, 8], [1, 256]])
            return bass.AP(m_all, off + jh * 128, [[3 * MW, 8], [1, 128]])

        vrow = k.sb("vrow", [128, 2, 128], F32)
        tvrow = T()
        nrow = k.sb("nrow", [32, 128], F32)
        tnrow = T()
        vT = k.sb("vT", [128, 2, 128], F32)
        tvT = T()
        nT = k.sb("nT", [128, 32], F32)
        tnT = T()
        AB = k.sb("AB", [128, 8, 16], F32)
        tAB = T()

        def layer_vectors(l):
            vi = 0
            for row in (0, 1):
                for w6 in (0, 1, 3, 4):
                    for jh in (0, 1):
                        P.dma("sp", lambda e, row=row, w6=w6, jh=jh, vi=vi: e.dma_start(
                            out=vrow[vi * 16 + jh * 8: vi * 16 + jh * 8 + 8, 0, :], in_=mvec_ap(l, row, w6, jh=jh)),
                            r=[tm_all], w=[tvrow])
                    vi += 1
            vi = 0
            for w6 in (0, 1, 3, 4):
                for jh in (0, 1):
                    P.dma("sp", lambda e, w6=w6, jh=jh, vi=vi: e.dma_start(
                        out=vrow[vi * 16 + jh * 8: vi * 16 + jh * 8 + 8, 1, :], in_=mvec_ap(l, 2, w6, jh=jh)),
                        r=[tm_all], w=[tvrow])
                vi += 1
            for n2 in (0, 1):
                for jh in (0, 1):
                    P.dma("sp", lambda e, n2=n2, jh=jh: e.dma_start(
                        out=nrow[n2 * 16 + jh * 8: n2 * 16 + jh * 8 + 8, :],
                        in_=bass.AP(normw, (l * 2 + n2) * D + jh * 128, [[256, 8], [1, 128]])), w=[tnrow])
            P.op("pe", lambda e: e.transpose(ps[0][:, 0:128], vrow[:, 0, :], identf[:]), r=[tvrow, tid], w=[tps[0]])
            P.op("dve", lambda e: e.tensor_copy(out=vT[:, 0, :], in_=ps[0][:, 0:128]), r=[tps[0]], w=[tvT])
            P.op("pe", lambda e: e.transpose(ps[0][:, 0:64], vrow[0:64, 1, :], identf[0:64, 0:64]), r=[tvrow, tid], w=[tps[0]])
            P.op("dve", lambda e: e.tensor_copy(out=vT[:, 1, 0:64], in_=ps[0][:, 0:64]), r=[tps[0]], w=[tvT])
            P.op("pe", lambda e: e.transpose(ps[0][:, 0:32], nrow[:], identf[0:32, 0:32]), r=[tnrow, tid], w=[tps[0]])
            P.op("dve", lambda e: e.tensor_copy(out=nT[:], in_=ps[0][:, 0:32]), r=[tps[0]], w=[tnT])
            P.op("dve", lambda e: e.tensor_scalar(out=vT[:, 0, 0:64], in0=vT[:, 0, 0:64], scalar1=bs[:, 0:1], scalar2=None,
                                                  op0=ALU.mult), r=[tvT, tbs], w=[tvT])
            P.op("dve", lambda e: e.scalar_tensor_tensor(out=vT[:, 0, 0:64], in0=vT[:, 0, 64:128], scalar=bs[:, 1:2],
                                                         in1=vT[:, 0, 0:64], op0=ALU.mult, op1=ALU.add), r=[tvT, tbs], w=[tvT])
            for n2 in (0, 1):
                for grp in (0, 1):
                    ai = n2 * 4 + grp * 2
                    sh = vT[:, grp, (2 * n2) * 16:(2 * n2) * 16 + 16]
                    sc = vT[:, grp, (2 * n2 + 1) * 16:(2 * n2 + 1) * 16 + 16]
                    P.op("dve", lambda e, ai=ai, sc=sc, n2=n2: e.scalar_tensor_tensor(
                        out=AB[:, ai, :], in0=sc, scalar=1.0, in1=nT[:, n2 * 16:(n2 + 1) * 16], op0=ALU.add, op1=ALU.mult),
                        r=[tvT, tnT], w=[tAB])
                    P.op("dve", lambda e, ai=ai, sh=sh: e.tensor_copy(out=AB[:, ai + 1, :], in_=sh), r=[tvT], w=[tAB])

        def load_gates(l, w6, ph):
            g0 = k.sb("g0", [128, D], F32, ph)
            gl = k.sb("gl", [128, D], F32, ph)
            gc = k.sb("gc", [128, D], F32, ph)
            tg0, tgl, tgc = T(), T(), T()
            P.dma("sp", lambda e: e.dma_start(out=g0[:].rearrange("p (r j) -> p r j", r=8), in_=mvec_ap(l, 0, w6, bcast=128)),
                  r=[tm_all], w=[tg0])
            P.dma("sp", lambda e: e.dma_start(out=gl[:].rearrange("p (r j) -> p r j", r=8), in_=mvec_ap(l, 1, w6, bcast=128)),
                  r=[tm_all], w=[tgl])
            P.dma("sp", lambda e: e.dma_start(out=gc[:].rearrange("p (r j) -> p r j", r=8), in_=mvec_ap(l, 2, w6, bcast=128)),
                  r=[tm_all], w=[tgc])
            P.op("dve", lambda e: e.tensor_scalar(out=gl[:], in0=gl[:], scalar1=bs[:, 1:2], scalar2=None, op0=ALU.mult),
                 r=[tgl, tbs], w=[tgl])
            P.op("dve", lambda e: e.scalar_tensor_tensor(out=gl[:], in0=g0[:], scalar=bs[:, 0:1], in1=gl[:], op0=ALU.mult,
                                                         op1=ALU.add), r=[tg0, tgl, tbs], w=[tgl])
            return gl, tgl, gc, tgc

        xn = [k.sb("xn%d" % i, [128, D], BF16) for i in range(2)]
        txn = [T(), T()]
        ss = k.sb("ss", [128, 2], F32)
        tss = [T(), T()]

        def norm_mod(n2, tiles):
            for i, t in enumerate(tiles):
                b = i % 2
                P.op("act", lambda e, t=t, b=b: e.activation(out=xn[b][:], in_=xres[:, t, :], func=AF.Square,
                                                             accum_out=ss[:, b:b + 1]), r=[tx[t]], w=[txn[b], tss[b]])
                P.op("act", lambda e, b=b: e.activation(out=ss[:, b:b + 1], in_=ss[:, b:b + 1], func=AF.Sqrt, bias=eps[:],
                                                        scale=1.0 / D), r=[tss[b], teps], w=[tss[b]])
                P.op("dve", lambda e, b=b: e.reciprocal(out=ss[:, b:b + 1], in_=ss[:, b:b + 1]), r=[tss[b]], w=[tss[b]])
                P.op("dve", lambda e, t=t, b=b: e.tensor_scalar(out=xn[b][:], in0=xres[:, t, :], scalar1=ss[:, b:b + 1],
                                                                scalar2=None, op0=ALU.mult), r=[tx[t], tss[b]], w=[txn[b]])
                ai = n2 * 4 + (2 if t == 8 else 0)
                for q4 in range(4):
                    pp = 2 + (q4 % 2)
                    pview = ps[pp][:].bitcast(BF16)
                    for j in range(4):
                        c = q4 * 4 + j
                        P.op("pe", lambda e, c=c, j=j, b=b, pview=pview: e.transpose(pview[:, j * 128:(j + 1) * 128],
                                                                                    xn[b][:, c * 128:(c + 1) * 128], ident[:]),
                             r=[txn[b], tid], w=[tps[pp]])
                    for j in range(4):
                        c = q4 * 4 + j
                        P.op("dve", lambda e, c=c, j=j, t=t, ai=ai, pview=pview: e.tensor_scalar(
                            out=hT[:, c, t * 128:(t + 1) * 128], in0=pview[:, j * 128:(j + 1) * 128],
                            scalar1=AB[:, ai, cprime(c):cprime(c) + 1], scalar2=AB[:, ai + 1, cprime(c):cprime(c) + 1],
                            op0=ALU.mult, op1=ALU.add), r=[tps[pp], tAB], w=[thT[t]])

        NTOK = NT * 128
        Gd = k.idram("Gd", [NT, 128, 16384], BF16)
        tGd = [T() for _ in range(NT)]
        EC = 256
        NEC = 16384 // EC
        KB = 8
        NB = 128 // KB
        BE = KB * 128

        def peer(l, tiles):
            wqf, twqf = wq_full[l]
            uf, tuf = u_full[l]
            vf, tvf = v_full[l]
            ph = contextlib.ExitStack()
            qT = k.sb("qT", [128, 16, 128], BF16, ph)
            tqT = T()
            wqb = [k.sb("wqb", [128, 16, 128], BF16, ph) for i in range(2)]
            twqb = [T(), T()]
            skT = k.sb("skT", [128, 2, 128], BF16, ph)
            tskT = T()
            sall = k.sb("sall", [128, 16, 128], F32, ph)
            tsall = T()
            tmpm = k.sb("tmpm", [128, 256], F32, ph)
            ttmpm = T()
            vtop = k.sb("vtop", [128, 16, 16], F32, ph)
            tvtop = T()
            cand = k.sb("cand", [128, 8, 256], F32, ph)
            tcand = T()
            sc = k.sb("sc", [128, 8, 16], F32, ph)
            tsc = T()
            sm = k.sb("sm", [128, 4, 8], F32, ph)
            tsm = T()
            dd = k.sb("dd", [128, 8, 16], F32, ph)
            tdd = T()
            Dq = [k.sb("Dq", [128, BE], F32, ph) for i in range(2)]
            tDq = [T(), T()]
            Eq = [k.sb("Eq", [128, BE], BF16, ph) for i in range(2)]
            tEq = [T(), T()]
            Gq = [k.sb("Gq", [128, BE], BF16, ph) for i in range(2)]
            tGq = [T(), T()]
            Gb = [k.sb("Gb", [128, BE], BF16, ph) for i in range(2)]
            tGb = [T(), T()]
            for h in range(2):
                P.dma("pool", lambda e, h=h: e.dma_start(out=skT[:, h, :], in_=skT_in.ap()[l, h]), w=[tskT])
            for t in tiles:
                for j in range(16):
                    b = j % 2
                    pp = 4 + j % 2
                    P.dma("sp", lambda e, j=j, b=b: e.dma_start(
                        out=wqb[b][:], in_=wqf.ap()[:, j * 128:(j + 1) * 128].rearrange("(k p) n -> p k n", p=128)),
                        r=[twqf], w=[twqb[b]])
                    for kk in range(16):
                        P.op("pe", lambda e, kk=kk, b=b, t=t, pp=pp: e.matmul(
                            ps[pp][:, 0:128], lhsT=wqb[b][:, kk, :], rhs=hT[:, kk, t * 128:(t + 1) * 128],
                            start=(kk == 0), stop=(kk == 15)), r=[twqb[b], thT[t]], w=[tps[pp]])
                    P.op("act", lambda e, j=j, pp=pp: e.copy(out=qT[:, j, :], in_=ps[pp][:, 0:128]), r=[tps[pp]], w=[tqT])
                for q4 in range(4):
                    pp = q4 % 2
                    for jj in range(4):
                        j = q4 * 4 + jj
                        P.op("pe", lambda e, j=j, jj=jj, pp=pp: e.matmul(
                            ps[pp][:, jj * 128:(jj + 1) * 128], lhsT=qT[:, j, :], rhs=skT[:, j % 2, :],
                            start=True, stop=True), r=[tqT, tskT], w=[tps[pp]])
                    P.op("act", lambda e, q4=q4, pp=pp: e.copy(out=sall[:, q4 * 4:(q4 + 1) * 4, :].rearrange("p a b -> p (a b)"),
                                                               in_=ps[pp][:]), r=[tps[pp]], w=[tsall])
                for j in range(16):
                    P.op("dve", lambda e, j=j: e.max(out=vtop[:, j, 0:8], in_=sall[:, j, :]), r=[tsall], w=[tvtop])
                    P.op("dve", lambda e, j=j: e.match_replace(out=tmpm[:, 0:128], in_to_replace=vtop[:, j, 0:8],
                                                               in_values=sall[:, j, :], imm_value=-1e30),
                         r=[tsall, tvtop], w=[ttmpm])
                    P.op("dve", lambda e, j=j: e.max(out=vtop[:, j, 8:16], in_=tmpm[:, 0:128]), r=[ttmpm], w=[tvtop])
                vt4 = vtop[:].rearrange("p (h two) a -> p h two a", two=2)
                P.op("dve", lambda e, vt4=vt4: e.tensor_tensor(
                    out=cand[:].rearrange("p h (a b) -> p h a b", a=16),
                    in0=vt4[:, :, 0, :].unsqueeze(3).to_broadcast([128, 8, 16, 16]),
                    in1=vt4[:, :, 1, :].unsqueeze(2).to_broadcast([128, 8, 16, 16]), op=ALU.add), r=[tvtop], w=[tcand])
                for h in range(8):
                    P.op("dve", lambda e, h=h: e.max(out=sc[:, h, 0:8], in_=cand[:, h, :]), r=[tcand], w=[tsc])
                    P.op("dve", lambda e, h=h: e.match_replace(out=tmpm[:], in_to_replace=sc[:, h, 0:8], in_values=cand[:, h, :],
                                                               imm_value=-1e30), r=[tcand, tsc], w=[ttmpm])
                    P.op("dve", lambda e, h=h: e.max(out=sc[:, h, 8:16], in_=tmpm[:]), r=[ttmpm], w=[tsc])
                P.op("dve", lambda e: e.tensor_scalar(out=sm[:, 0, :], in0=sc[:, :, 15], scalar1=-1.0, scalar2=None, op0=ALU.mult),
                     r=[tsc], w=[tsm])
                P.op("dve", lambda e: e.tensor_tensor(out=dd[:], in0=sc[:], in1=sm[:, 0, :].unsqueeze(2).to_broadcast([128, 8, 16]),
                                                      op=ALU.add), r=[tsc, tsm], w=[tdd])
                P.op("act", lambda e: e.activation(out=dd[:], in_=dd[:], func=AF.Exp), r=[tdd], w=[tdd])
                P.op("dve", lambda e: e.tensor_reduce(out=sm[:, 1, :], in_=dd[:], axis=AX.X, op=ALU.add), r=[tdd], w=[tsm])
                P.op("act", lambda e: e.activation(out=sm[:, 2, :], in_=sm[:, 1, :], func=AF.Ln), r=[tsm], w=[tsm])
                P.op("dve", lambda e: e.tensor_scalar(out=sm[:, 2, :], in0=sm[:, 2, :], scalar1=-1.0, scalar2=None, op0=ALU.mult),
                     r=[tsm], w=[tsm])
                it = 0
                for qq in range(NB):
                    gb = qq % 2
                    for h in range(8):
                        b = it % 2
                        it += 1
                        P.op("dve", lambda e, h=h, qq=qq, b=b: e.scalar_tensor_tensor(
                            out=Dq[b][:].rearrange("p (a c) -> p a c", a=KB),
                            in0=sall[:, 2 * h, qq * KB:(qq + 1) * KB].unsqueeze(2).to_broadcast([128, KB, 128]),
                            scalar=sm[:, 0, h:h + 1],
                            in1=sall[:, 2 * h + 1, :].unsqueeze(1).to_broadcast([128, KB, 128]),
                            op0=ALU.add, op1=ALU.add), r=[tsall, tsm], w=[tDq[b]])
                        P.op("act", lambda e, h=h, b=b: e.activation(out=Eq[b][:], in_=Dq[b][:], func=AF.Exp,
                                                                     bias=sm[:, 2, h:h + 1], scale=1.0),
                             r=[tDq[b], tsm], w=[tEq[b]])
                        if h == 0:
                            P.op("dve", lambda e, b=b, gb=gb: e.scalar_tensor_tensor(
                                out=Gb[gb][:], in0=Dq[b][:], scalar=-1e-5, in1=Eq[b][:], op0=ALU.is_ge, op1=ALU.mult),
                                r=[tDq[b], tEq[b]], w=[tGb[gb]])
                        else:
                            P.op("dve", lambda e, b=b: e.scalar_tensor_tensor(
                                out=Gq[b][:], in0=Dq[b][:], scalar=-1e-5, in1=Eq[b][:], op0=ALU.is_ge, op1=ALU.mult),
                                r=[tDq[b], tEq[b]], w=[tGq[b]])
                            P.op("pool", lambda e, b=b, gb=gb: e.tensor_tensor(out=Gb[gb][:], in0=Gb[gb][:], in1=Gq[b][:],
                                                                             op=ALU.add), r=[tGq[b], tGb[gb]], w=[tGb[gb]])
                    P.dma("sp", lambda e, t=t, qq=qq, gb=gb: e.dma_start(out=Gd.ap()[t, :, qq * BE:(qq + 1) * BE], in_=Gb[gb][:]),
                          r=[tGb[gb]], w=[tGd[t]])
            ph.close()
            P.barrier()
            li = layers.index(l)
            if li + 1 < len(layers):
                gather_peer(li + 1, "pool")
            ph = contextlib.ExitStack()
            usb = k.sb("usb", [128, 2, D], BF16, ph)
            tusb = T()
            vsb = [k.sb("vsb", [128, 2, D], BF16, ph) for i in range(2)]
            tvsb = [T(), T()]
            uT = k.sb("uT", [128, 16, EC], BF16, ph)
            tuT = T()
            gsb = [k.sb("gsb", [128, NT, EC], BF16, ph) for i in range(2)]
            tgsb = [T(), T()]
            asb = [k.sb("asb", [128, EC], BF16, ph) for i in range(2)]
            tasb = [T(), T()]
            wsb = [k.sb("wsb", [128, EC], BF16, ph) for i in range(2)]
            twsb = [T(), T()]
            wT = [k.sb("wT", [128, 2, 128], BF16, ph) for i in range(2)]
            twT = [T(), T()]
            acc = [k.sb("acc", [128, 512], F32, ph) for i in range(2)]
            tacc = [T(), T()]
            gl, tgl, gc, tgc = load_gates(l, 5, ph)
            ia = 0
            for ec in range(NEC):
                b = ec % 2
                e0 = ec * EC
                P.dma("sp", lambda e, e0=e0: e.dma_start(
                    out=usb[:], in_=uf.ap()[e0:e0 + EC, :].rearrange("(s p) f -> p s f", p=128)), r=[tuf], w=[tusb])
                P.dma("sp", lambda e, b=b, e0=e0: e.dma_start(
                    out=vsb[b][:], in_=vf.ap()[e0:e0 + EC, :].rearrange("(s p) f -> p s f", p=128)), r=[tvf], w=[tvsb[b]])
                P.dma("sp", lambda e, b=b, e0=e0: e.dma_start(
                    out=gsb[b][:], in_=Gd.ap()[:, :, e0:e0 + EC].rearrange("t p e -> p t e")), r=tGd, w=[tgsb[b]])
                it = 0
                for s in range(2):
                    for k4 in range(4):
                        pp = it % 2
                        it += 1
                        pview = ps[pp][:].bitcast(BF16)
                        for jj in range(4):
                            kk = k4 * 4 + jj
                            P.op("pe", lambda e, s=s, kk=kk, jj=jj, pview=pview: e.transpose(
                                pview[:, jj * 128:(jj + 1) * 128], usb[:, s, kk * 128:(kk + 1) * 128], ident[:]),
                                r=[tusb, tid], w=[tps[pp]])
                        P.op("act", lambda e, s=s, k4=k4, pview=pview: e.copy(
                            out=uT[:, k4 * 4:(k4 + 1) * 4, s * 128:(s + 1) * 128],
                            in_=pview[:, 0:512].rearrange("p (j n) -> p j n", j=4)), r=[tps[pp]], w=[tuT])
                def act_mm(i, b=b):
                    t = tiles[i]
                    pa = 2 + i % 2
                    for kk in range(16):
                        P.op("pe", lambda e, kk=kk, t=t, pa=pa: e.matmul(ps[pa][:, 0:EC], lhsT=hT[:, kk, t * 128:(t + 1) * 128],
                                                                        rhs=uT[:, kk, :], start=(kk == 0), stop=(kk == 15)),
                             r=[thT[t], tuT], w=[tps[pa]])
                act_mm(0)
                for i, t in enumerate(tiles):
                    b2 = i % 2
                    pa = 2 + i % 2
                    P.op("act", lambda e, b2=b2, pa=pa: e.activation(out=asb[b2][:], in_=ps[pa][:, 0:EC], func=GELU),
                         r=[tps[pa]], w=[tasb[b2]])
                    P.op("dve", lambda e, b2=b2, b=b, t=t: e.tensor_tensor(out=wsb[b2][:], in0=asb[b2][:], in1=gsb[b][:, t, :],
                                                                         op=ALU.mult), r=[tasb[b2], tgsb[b]], w=[twsb[b2]])
                    if i + 1 < len(tiles):
                        act_mm(i + 1)
                    pw = i % 2
                    pview = ps[pw][:].bitcast(BF16)
                    for s in range(2):
                        P.op("pe", lambda e, s=s, b2=b2, pview=pview, pw=pw: e.transpose(pview[:, s * 128:(s + 1) * 128],
                                                                                       wsb[b2][:, s * 128:(s + 1) * 128], ident[:]),
                             r=[twsb[b2], tid], w=[tps[pw]])
                    P.op("act", lambda e, b2=b2, pview=pview, pw=pw: e.copy(out=wT[b2][:].rearrange("p s n -> p (s n)"),
                                                                          in_=pview[:, 0:256]), r=[tps[pw]], w=[twT[b2]])
                    gt, tg = (gc, tgc) if t == 8 else (gl, tgl)
                    for fc in range(4):
                        for s in range(2):
                            P.op("pe", lambda e, fc=fc, s=s, b=b, b2=b2: e.matmul(
                                ps[4 + fc][:], lhsT=wT[b2][:, s, :], rhs=vsb[b][:, s, fc * 512:(fc + 1) * 512],
                                start=(s == 0), stop=(s == 1)), r=[twT[b2], tvsb[b]], w=[tps[4 + fc]])
                    for fc in range(4):
                        a2 = ia % 2
                        ia += 1
                        P.op("dve", lambda e, fc=fc, a2=a2, gt=gt: e.tensor_tensor(
                            out=acc[a2][:], in0=ps[4 + fc][:], in1=gt[:, fc * 512:(fc + 1) * 512], op=ALU.mult),
                            r=[tps[4 + fc], tg], w=[tacc[a2]])
                        P.op("dve", lambda e, t=t, fc=fc, a2=a2: e.tensor_tensor(
                            out=xres[:, t, fc * 512:(fc + 1) * 512], in0=xres[:, t, fc * 512:(fc + 1) * 512], in1=acc[a2][:],
                            op=ALU.add), r=[tacc[a2], tx[t]], w=[tx[t]])
            ph.close()
            P.barrier()

        def proj(Wf, tWf, col0, ncols, tiles, consume, ph, bw=256, nk=16, krow0=0, src=None, tsrc=None, kofs=0):
            src = hT if src is None else src
            tsrc = thT if tsrc is None else tsrc
            wb = [k.sb("wblk", [128, nk, bw], BF16, ph) for i in range(2)]
            twb = [T(), T()]
            it = 0
            for bi, c0 in enumerate(range(col0, col0 + ncols, bw)):
                b = bi % 2
                P.dma("sp" if Wf.ap().dtype == BF16 else "pool", lambda e, b=b, c0=c0: e.dma_start(
                    out=wb[b][:], in_=Wf.ap()[krow0:krow0 + nk * 128, c0:c0 + bw].rearrange("(k p) n -> p k n", p=128)),
                    r=[tWf], w=[twb[b]])
                for t in tiles:
                    pp = 6 + it % 2
                    it += 1
                    for kk in range(nk):
                        P.op("pe", lambda e, kk=kk, b=b, t=t, pp=pp: e.matmul(
                            ps[pp][:, 0:bw], lhsT=src[:, kofs + kk, t * 128:(t + 1) * 128], rhs=wb[b][:, kk, :],
                            start=(kk == 0), stop=(kk == nk - 1)), r=[tsrc[t], twb[b]], w=[tps[pp]])
                    consume(t, c0 // bw, ps[pp][:, 0:bw], tps[pp])

        def out_proj_residual(l, Wf, tWf, tiles, ph, gates):
            gl, tgl, gc, tgc = gates
            tmp = [k.sb("optmp", [128, 256], F32, ph) for i in range(2)]
            ttmp = [T(), T()]
            cnt = [0]

            def consume(t, bi, pap, tpp):
                a2 = cnt[0] % 2
                cnt[0] += 1
                gt, tg = (gc, tgc) if t == 8 else (gl, tgl)
                c0 = bi * 256
                P.op("dve", lambda e: e.tensor_tensor(out=tmp[a2][:], in0=pap, in1=gt[:, c0:c0 + 256], op=ALU.mult),
                     r=[tpp, tg], w=[ttmp[a2]])
                P.op("pool", lambda e: e.tensor_tensor(out=xres[:, t, c0:c0 + 256], in0=xres[:, t, c0:c0 + 256], in1=tmp[a2][:],
                                                       op=ALU.add), r=[ttmp[a2], tx[t]], w=[tx[t]])
            proj(Wf, tWf, 0, D, tiles, consume, ph)

        if mixers and any(l % 3 == 0 for l in layers):
            nA = sorted(set(l // 3 for l in layers if l % 3 == 0))
            awqkv_sh = k.inp("a_wqkv", [len(nA), 512, 3072])
            awo_sh = k.inp("a_wo", [len(nA), 512, D])
            again = k.inp("a_gain", [2, 2, 128])
            rope_in = k.inp("rope", [8, 128, 256])
            awqkv_full, awo_full = {}, {}
            for ji, j in enumerate(nA):
                awqkv_full[j] = k.gather_full("awqkv%d" % j, _sub(awqkv_sh, ji), 512, 3072)
                awo_full[j] = k.gather_full("awo%d" % j, _sub(awo_sh, ji), 512, D, cast=True)
            kvl_in = k.idram("kvl_in", [1024, 1024], BF16)
            kvc_in = k.idram("kvc_in", [64, 1024], BF16)
        SCALE = 128 ** -0.5

        def mixer_a(l, tiles):
            j = l // 3
            last = (l == DEPTH - 1)
            Wf, tWf = awqkv_full[j]
            ph0 = contextlib.ExitStack()
            qT = k.sb("qTa", [128, 16, NT * 128], BF16, ph0)
            tqT = [T() for _ in range(NT)]
            ph = contextlib.ExitStack()
            gq = k.sb("gq", [128, 2, 128], F32, ph)
            tgq = T()
            for i2 in range(2):
                P.dma("sp", lambda e, i2=i2: e.dma_start(out=gq[:, i2, :], in_=again.ap()[j, i2:i2 + 1, :].partition_broadcast(128)),
                      w=[tgq])
            rp_ = [k.sb("ropeb", [128, 256], F32, ph) for i in range(2)]
            trp = [T(), T()]
            sq = [k.sb("sq", [128, 256], F32, ph) for i in range(2)]
            tsq = [T(), T()]
            s2 = [k.sb("s2", [128, 2], F32, ph) for i in range(2)]
            ts2 = [T(), T()]
            qn = [k.sb("qn", [128, 256], F32, ph) for i in range(2)]
            tqn = [T(), T()]
            qc = [k.sb("qc", [128, 256], F32, ph) for i in range(2)]
            tqc = [T(), T()]
            qf = [k.sb("qf", [128, 256], BF16, ph) for i in range(2)]
            tqf = [T(), T()]
            tkvl, tkvc = T(), T()
            cnt = [0]
            ropetile = [None, None]

            def consume(t, bi, pap, tpp):
                b = cnt[0] % 2
                cnt[0] += 1
                c0 = bi * 256
                isq, isk, isv = c0 < 2048, 2048 <= c0 < 2560, c0 >= 2560
                if isv:
                    P.op("act", lambda e: e.copy(out=qf[b][:], in_=pap), r=[tpp], w=[tqf[b]])
                else:
                    gi = 0 if isq else 1
                    P.op("act", lambda e: e.activation(out=sq[b][:], in_=pap, func=AF.Square), r=[tpp], w=[tsq[b]])
                    P.op("dve", lambda e: e.tensor_reduce(out=s2[b][:], in_=sq[b][:].rearrange("p (h d) -> p h d", h=2), axis=AX.X,
                                                          op=ALU.add), r=[tsq[b]], w=[ts2[b]])
                    P.op("act", lambda e: e.activation(out=s2[b][:], in_=s2[b][:], func=AF.Sqrt, bias=eps[:], scale=1.0 / 128),
                         r=[ts2[b], teps], w=[ts2[b]])
                    P.op("dve", lambda e: e.reciprocal(out=s2[b][:], in_=s2[b][:]), r=[ts2[b]], w=[ts2[b]])
                    P.op("dve", lambda e: e.tensor_tensor(out=qn[b][:].rearrange("p (h d) -> p h d", h=2),
                                                          in0=pap.rearrange("p (h d) -> p h d", h=2),
                                                          in1=s2[b][:].unsqueeze(2).to_broadcast([128, 2, 128]), op=ALU.mult),
                         r=[tpp, ts2[b]], w=[tqn[b]])
                    if t == 8:
                        P.op("pool", lambda e: e.tensor_tensor(out=qf[b][:].rearrange("p (h d) -> p h d", h=2),
                                                               in0=qn[b][:].rearrange("p (h d) -> p h d", h=2),
                                                               in1=gq[:, gi, :].unsqueeze(1).to_broadcast([128, 2, 128]), op=ALU.mult),
                             r=[tqn[b], tgq], w=[tqf[b]])
                    else:
                        P.op("pool", lambda e: e.tensor_tensor(out=qn[b][:].rearrange("p (h d) -> p h d", h=2),
                                                               in0=qn[b][:].rearrange("p (h d) -> p h d", h=2),
                                                               in1=gq[:, gi, :].unsqueeze(1).to_broadcast([128, 2, 128]), op=ALU.mult),
                             r=[tqn[b], tgq], w=[tqn[b]])
                        rb = t % 2
                        if ropetile[rb] != t:
                            ropetile[rb] = t
                            P.dma("sp", lambda e: e.dma_start(out=rp_[rb][:], in_=rope_in.ap()[t]), w=[trp[rb]])
                        cosv = rp_[rb][:, 0:128]
                        sinv = rp_[rb][:, 128:256].rearrange("p (a b f) -> p a b f", a=2, b=2)
                        q5 = qn[b][:].rearrange("p (h a b f) -> p h a b f", h=2, a=2, b=2)
                        c5 = qc[b][:].rearrange("p (h a b f) -> p h a b f", h=2, a=2, b=2)
                        for hh in range(2):
                            P.op("dve", lambda e, hh=hh: e.tensor_tensor(
                                out=c5[:, :, :, hh, :], in0=q5[:, :, :, 1 - hh, :],
                                in1=sinv[:, :, hh, :].unsqueeze(1).to_broadcast([128, 2, 2, 32]), op=ALU.mult),
                                r=[tqn[b], trp[rb]], w=[tqc[b]])
                        P.op("pool", lambda e: e.tensor_tensor(out=qn[b][:].rearrange("p (h d) -> p h d", h=2),
                                                               in0=qn[b][:].rearrange("p (h d) -> p h d", h=2),
                                                               in1=cosv.unsqueeze(1).to_broadcast([128, 2, 128]), op=ALU.mult),
                             r=[tqn[b], trp[rb]], w=[tqn[b]])
                        P.op("pool", lambda e: e.tensor_tensor(out=qf[b][:], in0=qn[b][:], in1=qc[b][:], op=ALU.add),
                             r=[tqn[b], tqc[b]], w=[tqf[b]])
                if isq:
                    pview = ps[2 + bi % 2][:].bitcast(BF16)
                    tp_ = tps[2 + bi % 2]
                    for hh in range(2):
                        P.op("pe", lambda e, hh=hh: e.transpose(pview[:, hh * 128:(hh + 1) * 128], qf[b][:, hh * 128:(hh + 1) * 128],
                                                                ident[:]), r=[tqf[b], tid], w=[tp_])
                    P.op("act", lambda e: e.copy(out=qT[:, 2 * bi:2 * bi + 2, t * 128:(t + 1) * 128],
                                                 in_=pview[:, 0:256].rearrange("p (h n) -> p h n", h=2)), r=[tp_], w=[tqT[t]])
                else:
                    cc0 = c0 - 2048
                    if t == 8:
                        P.dma("sp", lambda e: e.dma_start(out=kvc_in.ap()[:, cc0:cc0 + 256], in_=qf[b][0:64, :]),
                              r=[tqf[b]], w=[tkvc])
                    else:
                        P.dma("sp", lambda e: e.dma_start(out=kvl_in.ap()[t * 128:(t + 1) * 128, cc0:cc0 + 256], in_=qf[b][:]),
                              r=[tqf[b]], w=[tkvl])

            qtiles = tiles
            proj(Wf, tWf, 0, 2048, qtiles, consume, ph)
            proj(Wf, tWf, 2048, 1024, list(range(NT)), consume, ph)
            ph.close()
            P.barrier()
            kvl_all, tkvl_all = k.gather_full("kvl%d" % l, _V(kvl_in.ap()), 1024, 1024, BF16, rdeps=[tkvl])
            kvc_all, tkvc_all = k.gather_full("kvc%d" % l, _V(kvc_in.ap()), 64, 1024, BF16, rdeps=[tkvc])
            ph = contextlib.ExitStack()
            NKC = 34
            kT = k.sb("kTa", [128, NKC * 128], BF16, ph)
            tkT = T()
            vv = k.sb("vva", [128, NKC, 129], BF16, ph)
            tvv = T()
            kin = [k.sb("kin", [128, 4, 128], BF16, ph) for i in range(2)]
            tkin = [T(), T()]
            pT = [k.sb("pTa", [128, 512], BF16, ph) for i in range(3)]
            tpT = [T(), T(), T()]
            rs = k.sb("rsa", [128, 4], F32, ph)
            trs = T()
            otok = [k.sb("otok", [128, 512], BF16, ph) for i in range(2)]
            totok = [T(), T()]
            P.op("pool", lambda e: e.memset(vv[:, :, 128:129], 1.0), w=[tvv])

            def keyrows(c4):
                if c4 * 4 * 128 < 256:
                    return kvc_all, tkvc_all, c4 * 512
                return kvl_all, tkvl_all, c4 * 512 - 256
            ipc = [0]

            def do_group(g):
                ip = ipc[0]
                srcs = [(kvc_all, tkvc_all, 0, 2)] + [(kvl_all, tkvl_all, r0, 4) for r0 in range(0, 4096, 512)]
                kc = 0
                for (srcT, tsrcT, r0, nch) in srcs:
                    b = ip % 2
                    ip += 1
                    P.dma("sp", lambda e, b=b, srcT=srcT, r0=r0, nch=nch: e.dma_start(
                        out=kin[b][:, 0:nch, :],
                        in_=srcT.ap()[r0:r0 + nch * 128, g * 128:(g + 1) * 128].rearrange("(c p) d -> p c d", p=128)),
                        r=[tsrcT], w=[tkin[b]])
                    pview = ps[b][:].bitcast(BF16)
                    for cc in range(nch):
                        P.op("pe", lambda e, b=b, cc=cc, pview=pview: e.transpose(pview[:, cc * 128:(cc + 1) * 128], kin[b][:, cc, :],
                                                                                ident[:]), r=[tkin[b], tid], w=[tps[b]])
                    P.op("act", lambda e, kc=kc, nch=nch, pview=pview: e.copy(out=kT[:, kc * 128:(kc + nch) * 128],
                                                                            in_=pview[:, 0:nch * 128]), r=[tps[b]], w=[tkT])
                    P.dma("sp", lambda e, srcT=srcT, r0=r0, nch=nch, kc=kc: e.dma_start(
                        out=vv[:, kc:kc + nch, 0:128],
                        in_=srcT.ap()[r0:r0 + nch * 128, 512 + g * 128:512 + (g + 1) * 128].rearrange("(c p) d -> p c d", p=128)),
                        r=[tsrcT], w=[tvv])
                    kc += nch
                for ti, t in enumerate(tiles):
                    chunks = [0, 1] if t == 8 else list(range(NKC))
                    def s_mm(ci, t=t, chunks=chunks):
                        kc = chunks[ci]
                        sp_ = ci % 2
                        P.op("pe", lambda e, kc=kc, t=t, sp_=sp_: e.matmul(
                            ps[sp_][:], lhsT=kT[:, kc * 128:(kc + 1) * 128], rhs=qT[:, 4 * g:4 * g + 4, t * 128:(t + 1) * 128],
                            start=True, stop=True), r=[tkT, tqT[t]], w=[tps[sp_]])
                    s_mm(0)
                    for ci, kc in enumerate(chunks):
                        sp_ = ci % 2
                        pb = ci % 3
                        if ci + 1 < len(chunks):
                            s_mm(ci + 1)
                        P.op("act", lambda e, sp_=sp_, pb=pb: e.activation(out=pT[pb][:], in_=ps[sp_][:], func=AF.Exp, scale=SCALE),
                             r=[tps[sp_]], w=[tpT[pb]])
                        for hh in range(4):
                            po = ps[2 + hh // 2]
                            off = (hh % 2) * 129
                            P.op("pe", lambda e, hh=hh, kc=kc, pb=pb, po=po, off=off, ci=ci, n=len(chunks): e.matmul(
                                po[:, off:off + 129], lhsT=pT[pb][:, hh * 128:(hh + 1) * 128], rhs=vv[:, kc, :],
                                start=(ci == 0), stop=(ci == n - 1)), r=[tpT[pb], tvv], w=[tps[2 + hh // 2]])
                    ob = ti % 2
                    for hh in range(4):
                        po = ps[2 + hh // 2]
                        off = (hh % 2) * 129
                        P.op("dve", lambda e, hh=hh, po=po, off=off: e.reciprocal(out=rs[:, hh:hh + 1], in_=po[:, off + 128:off + 129]),
                             r=[tps[2 + hh // 2]], w=[trs])
                        P.op("dve", lambda e, hh=hh, po=po, off=off, ob=ob: e.tensor_scalar(
                            out=otok[ob][:, hh * 128:(hh + 1) * 128], in0=po[:, off:off + 128], scalar1=rs[:, hh:hh + 1],
                            scalar2=None, op0=ALU.mult), r=[tps[2 + hh // 2], trs], w=[totok[ob]])
                    pview = ps[4 + ti % 2][:].bitcast(BF16)
                    tp_ = tps[4 + ti % 2]
                    for hh in range(4):
                        P.op("pe", lambda e, hh=hh, ob=ob, pview=pview: e.transpose(pview[:, hh * 128:(hh + 1) * 128],
                                                                                  otok[ob][:, hh * 128:(hh + 1) * 128], ident[:]),
                             r=[totok[ob], tid], w=[tp_])
                    P.op("act", lambda e, t=t, pview=pview: e.copy(out=hT[:, 4 * g:4 * g + 4, t * 128:(t + 1) * 128],
                                                                   in_=pview[:, 0:512].rearrange("p (h n) -> p h n", h=4)),
                         r=[tp_], w=[thT[t]])
                ipc[0] = ip
            for g in range(4):
                do_group(g)
            ph.close()
            ph0.close()
            P.barrier()
            ph = contextlib.ExitStack()
            gates = load_gates(l, 2, ph)
            out_proj_residual(l, awo_full[j][0], awo_full[j][1], tiles, ph, gates)
            ph.close()
            P.barrier()

        if mixers and any(l % 3 == 2 for l in layers):
            poolw_sh = k.inp("pool_w", [512, 512])
            pools_in = k.inp("pool_scale", [1, D])
            poolM_in = k.inp("poolM", [NT, 128, 12, 128])
            poolH_in = k.inp("poolH", [NT, 64, 4, 128])
            sel_in = k.inp("poolSel", [256, 64])
            poolw_full = k.gather_full("poolw", poolw_sh, 512, 512)
            xe_in = k.idram("xe_in", [64, D], BF16)

        def mixer_c(l, tiles):
            ph = contextlib.ExitStack()
            xna = k.sb("xna", [128, NT, D], BF16, ph)
            txna = [T() for _ in range(NT)]
            txe = T()
            for i, t in enumerate(range(NT)):
                b = i % 2
                P.op("act", lambda e, t=t, b=b: e.activation(out=xna[:, t, :], in_=xres[:, t, :], func=AF.Square,
                                                             accum_out=ss[:, b:b + 1]), r=[tx[t]], w=[txna[t], tss[b]])
                P.op("act", lambda e, b=b: e.activation(out=ss[:, b:b + 1], in_=ss[:, b:b + 1], func=AF.Sqrt, bias=eps[:],
                                                        scale=1.0 / D), r=[tss[b], teps], w=[tss[b]])
                P.op("dve", lambda e, b=b: e.reciprocal(out=ss[:, b:b + 1], in_=ss[:, b:b + 1]), r=[tss[b]], w=[tss[b]])
                P.op("dve", lambda e, t=t, b=b: e.tensor_scalar(out=xna[:, t, :], in0=xres[:, t, :], scalar1=ss[:, b:b + 1],
                                                                scalar2=None, op0=ALU.mult), r=[tx[t], tss[b]], w=[txna[t]])
            for (r0, p0, t) in ((0, 0, 0), (16, 112, 7), (32, 0, 8), (48, 48, 8)):
                P.dma("sp", lambda e, r0=r0, p0=p0, t=t: e.dma_start(out=xe_in.ap()[r0:r0 + 16, :], in_=xna[p0:p0 + 16, t, :]),
                      r=[txna[t]], w=[txe])
            xe_all, txe_all = k.gather_full("xe%d" % l, _V(xe_in.ap()), 64, D, BF16, rdeps=[txe])
            xes = k.sb("xes", [128, 2, D], BF16, ph)
            txes = T()
            sels = k.sb("sels", [128, 2, 64], BF16, ph)
            tsels = T()
            hal = k.sb("hal", [64, D], BF16, ph)
            thal = T()
            P.dma("sp", lambda e: e.dma_start(out=xes[:], in_=xe_all.ap().rearrange("(c p) f -> p c f", p=128)), r=[txe_all], w=[txes])
            P.dma("pool", lambda e: e.dma_start(out=sels[:], in_=sel_in.ap().rearrange("(c p) f -> p c f", p=128)), w=[tsels])
            for fb in range(4):
                pp = fb % 2
                for c in range(2):
                    P.op("pe", lambda e, fb=fb, c=c, pp=pp: e.matmul(ps[pp][0:64, :], lhsT=sels[:, c, :],
                                                                    rhs=xes[:, c, fb * 512:(fb + 1) * 512], start=(c == 0), stop=(c == 1)),
                         r=[tsels, txes], w=[tps[pp]])
                P.op("act", lambda e, fb=fb, pp=pp: e.copy(out=hal[:, fb * 512:(fb + 1) * 512], in_=ps[pp][0:64, :]), r=[tps[pp]], w=[thal])
            Mt = [k.sb("Mt", [128, 12, 128], BF16, ph) for i in range(2)]
            tMt = [T(), T()]
            Mh = [k.sb("Mh", [64, 4, 128], BF16, ph) for i in range(2)]
            tMh = [T(), T()]
            it = 0
            for i, t in enumerate(tiles):
                b = i % 2
                P.dma("pool", lambda e, t=t, b=b: e.dma_start(out=Mt[b][:], in_=poolM_in.ap()[t]), w=[tMt[b]])
                P.dma("pool", lambda e, t=t, b=b: e.dma_start(out=Mh[b][:], in_=poolH_in.ap()[t]), w=[tMh[b]])
                ai = 2 if t == 8 else 0
                for c in range(16):
                    g = c // 4
                    pp = 2 + it % 4
                    it += 1
                    srcs = [(xna[:, t, c * 128:(c + 1) * 128], Mt[b][:, g, :], [txna[t], tMt[b]])]
                    if 1 <= t <= 7:
                        srcs.append((xna[:, t - 1, c * 128:(c + 1) * 128], Mt[b][:, 4 + g, :], [txna[t - 1], tMt[b]]))
                    if t <= 6:
                        srcs.append((xna[:, t + 1, c * 128:(c + 1) * 128], Mt[b][:, 8 + g, :], [txna[t + 1], tMt[b]]))
                    if t in (0, 7):
                        srcs.append((hal[0:32, c * 128:(c + 1) * 128], Mh[b][0:32, g, :], [thal, tMh[b]]))
                    if t == 8:
                        srcs.append((hal[32:64, c * 128:(c + 1) * 128], Mh[b][32:64, g, :], [thal, tMh[b]]))
                    for si, (lh, rh, deps) in enumerate(srcs):
                        P.op("pe", lambda e, lh=lh, rh=rh, pp=pp, si=si, n=len(srcs): e.matmul(
                            ps[pp][:, 0:128], lhsT=lh, rhs=rh, start=(si == 0), stop=(si == n - 1)), r=deps, w=[tps[pp]])
                    P.op("dve", lambda e, c=c, t=t, ai=ai, pp=pp: e.tensor_scalar(
                        out=hT[:, c, t * 128:(t + 1) * 128], in0=ps[pp][:, 0:128], scalar1=AB[:, ai, cprime(c):cprime(c) + 1],
                        scalar2=None, op0=ALU.mult), r=[tps[pp], tAB], w=[thT[t]])
            ph.close()
            P.barrier()
            ph = contextlib.ExitStack()
            gl, tgl, gc, tgc = load_gates(l, 2, ph)
            lsb = k.sb("lsb", [128, D], F32, ph)
            tlsb = T()
            P.dma("sp", lambda e: e.dma_start(out=lsb[:], in_=pools_in.ap().partition_broadcast(128)), w=[tlsb])
            P.op("dve", lambda e: e.tensor_tensor(out=gl[:], in0=gl[:], in1=lsb[:], op=ALU.mult), r=[tgl, tlsb], w=[tgl])
            P.op("pool", lambda e: e.tensor_tensor(out=gc[:], in0=gc[:], in1=lsb[:], op=ALU.mult), r=[tgc, tlsb], w=[tgc])
            tmp = [k.sb("pctmp", [128, 256], F32, ph) for i in range(2)]
            ttmp = [T(), T()]
            cnt = [0]
            for g in range(4):
                def consume(t, bi, pap, tpp, g=g):
                    a2 = cnt[0] % 2
                    cnt[0] += 1
                    gt, tg = (gc, tgc) if t == 8 else (gl, tgl)
                    c0 = g * 512 + bi * 256
                    P.op("dve", lambda e: e.tensor_tensor(out=tmp[a2][:], in0=pap, in1=gt[:, c0:c0 + 256], op=ALU.mult),
                         r=[tpp, tg], w=[ttmp[a2]])
                    P.op("pool", lambda e: e.tensor_tensor(out=xres[:, t, c0:c0 + 256], in0=xres[:, t, c0:c0 + 256], in1=tmp[a2][:],
                                                           op=ALU.add), r=[ttmp[a2], tx[t]], w=[tx[t]])
                proj(poolw_full[0], poolw_full[1], 0, 512, tiles, consume, ph, nk=4, krow0=g * 512, kofs=4 * g)
            ph.close()
            P.barrier()

        if mixers and any(l % 3 == 1 for l in layers):
            bwqkv_sh = k.inp("b_wqkv", [512, 6144])
            bwo_sh = k.inp("b_wo", [512, D])
            biasT_in = k.inp("biasT", [5, 16, 8, 128, 128])
            idxB_in = k.inp("idxB", [128, 15], U32)
            bwqkv_full = k.gather_full("bwqkv", bwqkv_sh, 512, 6144)
            bwo_full = k.gather_full("bwo", bwo_sh, 512, D, cast=True)
            kvbl_in = k.idram("kvbl_in", [1024, 4096], BF16)
            kvbc_in = k.idram("kvbc_in", [64, 4096], BF16)
            win_kv = k.idram("win_kv", [1920, 4096], BF16)
        SLOT = [0, 1, 2, 2, 2, 2, 3, 4]

        def mixer_b(l, tiles):
            Wf, tWf = bwqkv_full
            ph0 = contextlib.ExitStack()
            qT = k.sb("qTb", [128, 16, NT * 128], BF16, ph0)
            tqT = [T() for _ in range(NT)]
            ph = contextlib.ExitStack()
            qf = [k.sb("qfb", [128, 256], BF16, ph) for i in range(2)]
            tqf = [T(), T()]
            tkvl, tkvc = T(), T()
            cnt = [0]

            def consume(t, bi, pap, tpp):
                b = cnt[0] % 2
                cnt[0] += 1
                c0 = bi * 256
                P.op("act", lambda e: e.copy(out=qf[b][:], in_=pap), r=[tpp], w=[tqf[b]])
                if c0 < 2048:
                    pview = ps[2 + bi % 2][:].bitcast(BF16)
                    tp_ = tps[2 + bi % 2]
                    for hh in range(2):
                        P.op("pe", lambda e, hh=hh: e.transpose(pview[:, hh * 128:(hh + 1) * 128], qf[b][:, hh * 128:(hh + 1) * 128],
                                                                ident[:]), r=[tqf[b], tid], w=[tp_])
                    P.op("act", lambda e: e.copy(out=qT[:, 2 * bi:2 * bi + 2, t * 128:(t + 1) * 128],
                                                 in_=pview[:, 0:256].rearrange("p (h n) -> p h n", h=2)), r=[tp_], w=[tqT[t]])
                else:
                    cc0 = c0 - 2048
                    if t == 8:
                        P.dma("sp", lambda e: e.dma_start(out=kvbc_in.ap()[:, cc0:cc0 + 256], in_=qf[b][0:64, :]), r=[tqf[b]], w=[tkvc])
                    else:
                        P.dma("sp", lambda e: e.dma_start(out=kvbl_in.ap()[t * 128:(t + 1) * 128, cc0:cc0 + 256], in_=qf[b][:]),
                              r=[tqf[b]], w=[tkvl])
            proj(Wf, tWf, 0, 6144, list(range(NT)), consume, ph)
            ph.close()
            P.barrier()
            kvl_all, tkvl_all = k.gather_full("kvbl%d" % l, _V(kvbl_in.ap()), 1024, 4096, BF16, rdeps=[tkvl])
            kvc_all, tkvc_all = k.gather_full("kvbc%d" % l, _V(kvbc_in.ap()), 64, 4096, BF16, rdeps=[tkvc])
            ph = contextlib.ExitStack()
            idxs = k.sb("idxs", [128, 15], U32, ph)
            tidx = T()
            wbuf = [k.sb("wbuf", [128, 4096], BF16, ph) for i in range(2)]
            twbuf = [T(), T()]
            twin = T()
            P.dma("sp", lambda e: e.dma_start(out=idxs[:], in_=idxB_in.ap()), w=[tidx])
            for wc in range(15):
                b = wc % 2
                P.dma("pool", lambda e, wc=wc, b=b: e.indirect_dma_start(
                    out=wbuf[b][:], out_offset=None, in_=kvl_all.ap(),
                    in_offset=bass.IndirectOffsetOnAxis(ap=idxs[:, wc:wc + 1], axis=0)), r=[tidx, tkvl_all], w=[twbuf[b]])
                P.dma("sp", lambda e, wc=wc, b=b: e.dma_start(out=win_kv.ap()[wc * 128:(wc + 1) * 128, :], in_=wbuf[b][:]),
                      r=[twbuf[b]], w=[twin])
            ph.close()
            P.barrier()
            ph = contextlib.ExitStack()
            NKC = 17
            kT = k.sb("kTb", [128, NKC * 128], BF16, ph)
            tkT = T()
            vv = k.sb("vvb", [128, NKC, 129], BF16, ph)
            tvv = T()
            kin = [k.sb("kinb", [128, 4, 128], BF16, ph) for i in range(2)]
            tkin = [T(), T()]
            bt = [k.sb("btb", [128, 4, 128], F32, ph) for i in range(2)]
            tbt = [T(), T()]
            sbb = [k.sb("sbb", [128, 512], F32, ph) for i in range(2)]
            tsbb = [T(), T()]
            pT = [k.sb("pTb", [128, 512], BF16, ph) for i in range(3)]
            tpT = [T(), T(), T()]
            rs = k.sb("rsb", [128, 1], F32, ph)
            trs = T()
            otok = [k.sb("otokb", [128, 128], BF16, ph) for i in range(2)]
            totok = [T(), T()]
            P.op("pool", lambda e: e.memset(vv[:, :, 128:129], 1.0), w=[tvv])
            ctr = dict(ip=0, ig=0, it=0)

            def do_head(h):
                srcs = [(win_kv, twin, r0, min(4, 15 - r0 // 128)) for r0 in range(0, 1920, 512)] + [(kvc_all, tkvc_all, 0, 2)]
                kc = 0
                for (srcT, tsrcT, r0, nch) in srcs:
                    b = ctr["ip"] % 2
                    ctr["ip"] += 1
                    P.dma("sp", lambda e, b=b, srcT=srcT, r0=r0, nch=nch: e.dma_start(
                        out=kin[b][:, 0:nch, :],
                        in_=srcT.ap()[r0:r0 + nch * 128, h * 128:(h + 1) * 128].rearrange("(c p) d -> p c d", p=128)),
                        r=[tsrcT], w=[tkin[b]])
                    pview = ps[b][:].bitcast(BF16)
                    for cc in range(nch):
                        P.op("pe", lambda e, b=b, cc=cc, pview=pview: e.transpose(pview[:, cc * 128:(cc + 1) * 128], kin[b][:, cc, :],
                                                                                ident[:]), r=[tkin[b], tid], w=[tps[b]])
                    P.op("act", lambda e, kc=kc, nch=nch, pview=pview: e.copy(out=kT[:, kc * 128:(kc + nch) * 128],
                                                                            in_=pview[:, 0:nch * 128]), r=[tps[b]], w=[tkT])
                    P.dma("sp", lambda e, srcT=srcT, r0=r0, nch=nch, kc=kc: e.dma_start(
                        out=vv[:, kc:kc + nch, 0:128],
                        in_=srcT.ap()[r0:r0 + nch * 128, 2048 + h * 128:2048 + (h + 1) * 128].rearrange("(c p) d -> p c d", p=128)),
                        r=[tsrcT], w=[tvv])
                    kc += nch
                for t in tiles:
                    if t == 8:
                        groups = [([15, 16], None)]
                    else:
                        groups = [([t, t + 1, t + 2, t + 3], 0), ([t + 4, t + 5, t + 6, t + 7], 4), ([15, 16], None)]
                    nall = sum(len(g_[0]) for g_ in groups)
                    done = 0
                    for (chs, boff) in groups:
                        ig = ctr["ig"]
                        ctr["ig"] += 1
                        sp_ = ig % 2
                        pb = ig % 3
                        n = len(chs)
                        for jj, kc in enumerate(chs):
                            P.op("pe", lambda e, jj=jj, kc=kc, t=t, sp_=sp_: e.matmul(
                                ps[sp_][:, jj * 128:(jj + 1) * 128], lhsT=kT[:, kc * 128:(kc + 1) * 128],
                                rhs=qT[:, h, t * 128:(t + 1) * 128], start=True, stop=True), r=[tkT, tqT[t]], w=[tps[sp_]])
                        if boff is not None:
                            bb = ig % 2
                            P.dma("sp", lambda e, t=t, boff=boff, bb=bb: e.dma_start(
                                out=bt[bb][:], in_=biasT_in.ap()[SLOT[t], h, boff:boff + 4].rearrange("c k q -> k c q")), w=[tbt[bb]])
                            P.op("dve", lambda e, sp_=sp_, bb=bb: e.scalar_tensor_tensor(
                                out=sbb[bb][:], in0=ps[sp_][:], scalar=SCALE, in1=bt[bb][:].rearrange("k c q -> k (c q)"),
                                op0=ALU.mult, op1=ALU.add), r=[tps[sp_], tbt[bb]], w=[tsbb[bb]])
                            P.op("act", lambda e, bb=bb, pb=pb: e.activation(out=pT[pb][:], in_=sbb[bb][:], func=AF.Exp),
                                 r=[tsbb[bb]], w=[tpT[pb]])
                        else:
                            P.op("act", lambda e, sp_=sp_, pb=pb, n=n: e.activation(out=pT[pb][:, 0:n * 128], in_=ps[sp_][:, 0:n * 128],
                                                                                  func=AF.Exp, scale=SCALE), r=[tps[sp_]], w=[tpT[pb]])
                        for jj, kc in enumerate(chs):
                            P.op("pe", lambda e, jj=jj, kc=kc, pb=pb, first=(done == 0), lastc=(done == nall - 1): e.matmul(
                                ps[2][:, 0:129], lhsT=pT[pb][:, jj * 128:(jj + 1) * 128], rhs=vv[:, kc, :],
                                start=first, stop=lastc), r=[tpT[pb], tvv], w=[tps[2]])
                            done += 1
                    ob = ctr["it"] % 2
                    ctr["it"] += 1
                    P.op("dve", lambda e: e.reciprocal(out=rs[:], in_=ps[2][:, 128:129]), r=[tps[2]], w=[trs])
                    P.op("dve", lambda e, ob=ob: e.tensor_scalar(out=otok[ob][:], in0=ps[2][:, 0:128], scalar1=rs[:, 0:1], scalar2=None,
                                                                 op0=ALU.mult), r=[tps[2], trs], w=[totok[ob]])
                    pview = ps[4 + ob][:].bitcast(BF16)
                    P.op("pe", lambda e, ob=ob, pview=pview: e.transpose(pview[:, 0:128], otok[ob][:], ident[:]),
                         r=[totok[ob], tid], w=[tps[4 + ob]])
                    P.op("act", lambda e, t=t, ob=ob, pview=pview: e.copy(out=hT[:, h, t * 128:(t + 1) * 128], in_=pview[:, 0:128]),
                         r=[tps[4 + ob]], w=[thT[t]])
            for h in range(16):
                do_head(h)
            ph.close()
            ph0.close()
            P.barrier()
            ph = contextlib.ExitStack()
            gates = load_gates(l, 2, ph)
            out_proj_residual(l, bwo_full[0], bwo_full[1], tiles, ph, gates)
            ph.close()
            P.barrier()

        for l in layers:
            last = (l == DEPTH - 1)
            tiles = list(range(8)) if last else list(range(NT))
            layer_vectors(l)
            if mixers:
                if l % 3 == 0:
                    norm_mod(0, list(range(NT)))
                    mixer_a(l, tiles)
                elif l % 3 == 1:
                    norm_mod(0, list(range(NT)))
                    mixer_b(l, tiles)
                else:
                    mixer_c(l, tiles)
            if do_peer:
                norm_mod(1, tiles)
                peer(l, tiles)

        fw = k.sb("fw", [128, D], F32)
        tfw = T()
        P.dma("sp", lambda e: e.dma_start(out=fw[:], in_=fnw.ap().partition_broadcast(128)), w=[tfw])
        ob = [k.sb("ob%d" % i, [128, D], F32) for i in range(2)]
        tob = [T(), T()]
        for t in range(8):
            b = t % 2
            P.op("act", lambda e, t=t, b=b: e.activation(out=xn[b][:], in_=xres[:, t, :], func=AF.Square, accum_out=ss[:, b:b + 1]),
                 r=[tx[t]], w=[txn[b], tss[b]])
            P.op("act", lambda e, b=b: e.activation(out=ss[:, b:b + 1], in_=ss[:, b:b + 1], func=AF.Sqrt, bias=eps[:], scale=1.0 / D),
                 r=[tss[b], teps], w=[tss[b]])
            P.op("dve", lambda e, b=b: e.reciprocal(out=ss[:, b:b + 1], in_=ss[:, b:b + 1]), r=[tss[b]], w=[tss[b]])
            P.op("dve", lambda e, t=t, b=b: e.scalar_tensor_tensor(out=ob[b][:], in0=xres[:, t, :], scalar=ss[:, b:b + 1], in1=fw[:],
                                                                   op0=ALU.mult, op1=ALU.mult), r=[tx[t], tss[b], tfw], w=[tob[b]])
            P.dma("sp", lambda e, t=t, b=b: e.dma_start(out=out.ap()[t], in_=ob[b][:]), r=[tob[b]])
        P.emit()
    return nc


class _V:
    def __init__(self, a):
        self._a = a

    def ap(self):
        return self._a


def _sub(t, l):
    class V:
        def __init__(self, a):
            self._a = a

        def ap(self):
            return self._a
    return V(t.ap()[l])


def host_inputs(inp, cfg):
    f = lambda a: np.ascontiguousarray(np.asarray(a, dtype=np.float32))
    x, c, ctx, c_ctx = f(inp["x"]), f(inp["c"]), f(inp["ctx"]), f(inp["c_ctx"])
    mod_w, mod_b = f(inp["mod_w"]), f(inp["mod_b"])
    maps = []
    mw5 = mod_w.reshape(DEPTH, D, 6, 8, 256)
    mb4 = mod_b.reshape(DEPTH, 6, 8, 256)
    skT = np.ascontiguousarray(f(inp["peer_sub_keys"]).transpose(0, 1, 3, 2))
    for cid in range(NC):
        b, kq = cid // 4, cid % 4
        xc = np.zeros((NT, 128, D), np.float32)
        xc[:8] = x[b, kq * 1024:(kq + 1) * 1024].reshape(8, 128, D)
        xc[8, :64] = ctx[b, kq * 64:(kq + 1) * 64]
        bsel = np.zeros((128, 2), np.float32)
        bsel[:, b] = 1.0
        m = {
            "x_c": xc,
            "c_all": np.stack([c[0], c[1], c_ctx]),
            "bsel": bsel,
            "modw": np.ascontiguousarray(mw5[:, :, :, cid, :]),
            "modb": np.ascontiguousarray(mb4[:, :, cid, :]).reshape(1, -1),
            "normw": f(inp["norm_w"]).reshape(DEPTH * 2, D),
            "fnw": f(inp["final_norm_w"]).reshape(1, D),
            "peer_wq": np.ascontiguousarray(f(inp["peer_wq"])[cfg["layers"], kq * 512:(kq + 1) * 512]),
            "skT": skT,
            "peer_u": np.ascontiguousarray(inp["peer_u"][cfg["layers"], kq * 4096:(kq + 1) * 4096], dtype=np.float32),
            "peer_v": np.ascontiguousarray(inp["peer_v"][cfg["layers"], kq * 4096:(kq + 1) * 4096], dtype=np.float32),
        }
        lays = cfg["layers"]
        if cfg.get("mixers", True) and any(l % 3 == 0 for l in lays):
            nA = sorted(set(l // 3 for l in lays if l % 3 == 0))
            m["a_wqkv"] = np.ascontiguousarray(f(inp["a_wqkv"])[nA, kq * 512:(kq + 1) * 512])
            m["a_wo"] = np.ascontiguousarray(f(inp["a_wo"])[nA, kq * 512:(kq + 1) * 512])
            m["a_gain"] = np.ascontiguousarray(np.stack([f(inp["a_q_gain"]), f(inp["a_k_gain"])], axis=1))
            m["rope"] = rope_tables(kq)
        if cfg.get("mixers", True) and any(l % 3 == 2 for l in lays):
            m["pool_w"] = np.ascontiguousarray(f(inp["pool_w"])[0, kq])
            m["pool_scale"] = f(inp["pool_scale"]).reshape(1, D)
            pm, phh, sel = pool_consts(kq)
            m["poolM"], m["poolH"], m["poolSel"] = pm, phh, sel
        if cfg.get("mixers", True) and any(l % 3 == 1 for l in lays):
            m["b_wqkv"] = np.ascontiguousarray(f(inp["b_wqkv"])[0, kq * 512:(kq + 1) * 512])
            m["b_wo"] = np.ascontiguousarray(f(inp["b_wo"])[0, kq * 512:(kq + 1) * 512])
            m["biasT"], m["idxB"] = nbr_consts(kq, f(inp["b_rpb"])[0])
        if not cfg.get("peer", True):
            for nm in ("peer_wq", "skT", "peer_u", "peer_v"):
                m.pop(nm)
        maps.append(m)
    return maps


def rope_tables(kq):
    tok = np.arange(kq * 1024, (kq + 1) * 1024)
    row = (tok // 64).astype(np.float32)
    col = (tok % 64).astype(np.float32)
    inv = (10000.0 ** (-np.arange(0, 64, 2, dtype=np.float32) / 64)).astype(np.float32)
    ar = row[:, None] * inv[None, :]
    ac = col[:, None] * inv[None, :]
    cos = np.concatenate([np.cos(ar), np.cos(ar), np.cos(ac), np.cos(ac)], axis=1)
    sin = np.concatenate([-np.sin(ar), np.sin(ar), -np.sin(ac), np.sin(ac)], axis=1)
    return np.ascontiguousarray(np.concatenate([cos, sin], axis=1).astype(np.float32).reshape(8, 128, 256))


CFG = dict(layers=[0, 1, 2, 3], peer=True, mixers=True)


def run(inp, cfg):
    nc = build(cfg)
    maps = host_inputs(inp, cfg)
    res = run_bass_kernel_spmd(nc, maps, core_ids=list(range(NC)))
    outp = np.zeros((2, 4096, D), np.float32)
    for cid in range(NC):
        b, kq = cid // 4, cid % 4
        outp[b, kq * 1024:(kq + 1) * 1024] = res.results[cid]["out"].reshape(1024, D)
    return outp


def kernel(**inputs):
    return run(inputs, CFG)


def pool_consts(kq):
    wins = (2, 4, 8, 16)

    def coef(tau, spos, w, L):
        lo = np.maximum(tau - w // 2, 0)
        hi = np.minimum(tau + w - w // 2, L)
        inwin = (spos[:, None] >= lo[None, :]) & (spos[:, None] < hi[None, :]) & (spos[:, None] >= 0) & (spos[:, None] < L)
        m = inwin / (hi - lo)[None, :].astype(np.float64)
        m = m - (spos[:, None] == tau[None, :])
        return m.astype(np.float32)
    PM = np.zeros((NT, 128, 12, 128), np.float32)
    PH = np.zeros((NT, 64, 4, 128), np.float32)
    for t in range(8):
        base = kq * 1024 + t * 128
        tau = base + np.arange(128)
        for g, w in enumerate(wins):
            PM[t, :, g, :] = coef(tau, base + np.arange(128), w, 4096)
            if t >= 1:
                PM[t, :, 4 + g, :] = coef(tau, base - 128 + np.arange(128), w, 4096)
            if t <= 6:
                PM[t, :, 8 + g, :] = coef(tau, base + 128 + np.arange(128), w, 4096)
            if t == 0:
                PH[t, 0:16, g, :] = coef(tau, base - 16 + np.arange(16), w, 4096)
            if t == 7:
                PH[t, 16:32, g, :] = coef(tau, base + 128 + np.arange(16), w, 4096)
    base = kq * 64
    tau = base + np.arange(128)
    valid = (np.arange(128) < 64)
    for g, w in enumerate(wins):
        m0 = coef(tau, base + np.arange(128), w, 256)
        m0[64:, :] = 0.0
        m0[:, ~valid] = 0.0
        PM[8, :, g, :] = m0
        mp = coef(tau, base - 16 + np.arange(16), w, 256)
        mn = coef(tau, base + 64 + np.arange(16), w, 256)
        mp[:, ~valid] = 0.0
        mn[:, ~valid] = 0.0
        PH[8, 32:48, g, :] = mp
        PH[8, 48:64, g, :] = mn
    sel = np.zeros((256, 64), np.float32)
    for i in range(16):
        if kq > 0:
            sel[(kq - 1) * 64 + 16 + i, i] = 1.0
            sel[(kq - 1) * 64 + 48 + i, 32 + i] = 1.0
        if kq < 3:
            sel[(kq + 1) * 64 + i, 16 + i] = 1.0
            sel[(kq + 1) * 64 + 32 + i, 48 + i] = 1.0
    return PM, PH, sel


def nbr_consts(kq, rpb):
    NEG = -30000.0
    out = np.full((5, 16, 8, 128, 128), NEG, np.float32)
    qr = np.arange(128) // 64
    qc = np.arange(128) % 64
    for slot, t in enumerate((0, 1, 2, 6, 7)):
        R0 = 16 * kq + 2 * t
        r = R0 + qr
        r0 = np.clip(r - 4, 0, 56)
        cs = np.clip(qc - 8, 0, 48)
        for jj in range(8):
            krow = (R0 - 7 + 2 * jj) + np.arange(128) // 64
            kcol = np.arange(128) % 64
            valid = ((krow[:, None] >= r0[None, :]) & (krow[:, None] < r0[None, :] + 8) &
                     (kcol[:, None] >= cs[None, :]) & (kcol[:, None] < cs[None, :] + 16) &
                     (krow[:, None] >= 0) & (krow[:, None] < 64))
            rr = np.clip(krow[:, None] - r[None, :] + 7, 0, 14)
            rc = np.clip(kcol[:, None] - qc[None, :] + 15, 0, 30)
            vals = rpb[:, rr, rc]
            out[slot, :, jj] = np.where(valid[None], vals, NEG)
    n = np.arange(1920)
    grow = np.clip(16 * kq - 7 + n // 64, 0, 63)
    idx = (grow * 64 + n % 64).astype(np.uint32).reshape(15, 128).T
    return out, np.ascontiguousarray(idx)
```
